# Optimizing a Trainium2 kernel written in Bass

```python
import math
import jax
import jax.numpy as jnp
from jax import lax
import numpy as np

D_MODEL = 1024
BATCH = 2
SEQ = 8192
DEPTH = 2

N_MIXERS = 2
S5_WIDTH = D_MODEL
S5_GROUP = 16
S5_GROUPS = S5_WIDTH // S5_GROUP
S5_STATE = 64
S5_DT_MIN = 1e-3
S5_DT_MAX = 1e-1
N_HEADS = 8
HEAD_DIM = D_MODEL // N_HEADS
MOBA_BLOCK = 256
MOBA_TOPK = 3
Q_CHUNK = 32
ROPE_THETA = 10000.0
N_EXPERTS = 32
TOP_K = 4
D_FF = D_MODEL
SWIGLU_LIMIT = 7.0
SWIGLU_ALPHA = 1.702
EPS = 1e-6

kernel_name = "hybrid_s5_moba_moe_adaln"


def rms_norm(x, g):
    xf = x.astype(jnp.float32)
    y = xf * lax.rsqrt(jnp.mean(xf * xf, axis=-1, keepdims=True) + EPS)
    return y.astype(x.dtype) * g


def modulate(h, shift, scale):
    return h * (1.0 + scale[:, None, :]) + shift[:, None, :]


def rope_tables(positions):
    inv = ROPE_THETA ** (-jnp.arange(0, HEAD_DIM, 2, dtype=jnp.float32) / HEAD_DIM)
    ang = positions.astype(jnp.float32)[..., None] * inv
    ang = jnp.concatenate([ang, ang], axis=-1)
    return jnp.cos(ang), jnp.sin(ang)


def apply_rope(t, cos, sin):
    t1, t2 = jnp.split(t, 2, axis=-1)
    rot = jnp.concatenate([-t2, t1], axis=-1)
    return (t * cos[:, None] + rot * sin[:, None]).astype(t.dtype)


def _ssm_combine(e1, e2):
    a1r, a1i, b1r, b1i = e1
    a2r, a2i, b2r, b2i = e2
    return (a2r * a1r - a2i * a1i,
            a2r * a1i + a2i * a1r,
            a2r * b1r - a2i * b1i + b2r,
            a2r * b1i + a2i * b1r + b2i)


def s5_mixer(h, w_in, b_re, b_im, c_re, c_im, lam_re, lam_im, log_dt, d_skip, w_glu, w_out):
    bsz, seq, _ = h.shape
    u = h @ w_in
    ug = u.reshape(bsz, seq, S5_GROUPS, S5_GROUP)
    dt = jnp.exp(log_dt)[:, None]
    mag = jnp.exp(lam_re * dt)
    a_re = mag * jnp.cos(lam_im * dt)
    a_im = mag * jnp.sin(lam_im * dt)
    den = lam_re * lam_re + lam_im * lam_im
    f_re = ((a_re - 1.0) * lam_re + a_im * lam_im) / den
    f_im = (a_im * lam_re - (a_re - 1.0) * lam_im) / den
    bb_re = f_re[..., None] * b_re - f_im[..., None] * b_im
    bb_im = f_re[..., None] * b_im + f_im[..., None] * b_re
    bu_re = jnp.einsum('bsgc,gpc->bsgp', ug, bb_re)
    bu_im = jnp.einsum('bsgc,gpc->bsgp', ug, bb_im)
    ar = jnp.broadcast_to(a_re.astype(bu_re.dtype), bu_re.shape)
    ai = jnp.broadcast_to(a_im.astype(bu_re.dtype), bu_re.shape)
    _, _, x_re, x_im = lax.associative_scan(_ssm_combine, (ar, ai, bu_re, bu_im), axis=1)
    y = jnp.einsum('bsgp,gcp->bsgc', x_re, c_re) - jnp.einsum('bsgp,gcp->bsgc', x_im, c_im)
    y = y.reshape(bsz, seq, S5_WIDTH) + d_skip * u
    z = jax.nn.gelu(y)
    z = z * jax.nn.sigmoid(z @ w_glu)
    return z @ w_out


def moba_mixer(h, cos, sin, w_qkv, w_o):
    bsz, seq, _ = h.shape
    qkv = (h @ w_qkv).reshape(bsz, seq, 3, N_HEADS, HEAD_DIM)
    q = apply_rope(qkv[:, :, 0].transpose(0, 2, 1, 3), cos, sin)
    k = apply_rope(qkv[:, :, 1].transpose(0, 2, 1, 3), cos, sin)
    v = qkv[:, :, 2].transpose(0, 2, 1, 3)
    scale = HEAD_DIM ** -0.5
    n_blocks = -(-seq // MOBA_BLOCK)
    pad = n_blocks * MOBA_BLOCK - seq
    k_p = jnp.pad(k, ((0, 0), (0, 0), (0, pad), (0, 0)))
    v_p = jnp.pad(v, ((0, 0), (0, 0), (0, pad), (0, 0)))
    k_blocks = k_p.reshape(bsz, N_HEADS, n_blocks, MOBA_BLOCK, HEAD_DIM)
    v_blocks = v_p.reshape(bsz, N_HEADS, n_blocks, MOBA_BLOCK, HEAD_DIM)
    k_mean = jnp.mean(k_blocks, axis=3)
    n_sel = min(MOBA_TOPK, n_blocks)
    bi = jnp.arange(bsz)[:, None, None, None]
    hi = jnp.arange(N_HEADS)[None, :, None, None]
    blk_ids = jnp.arange(n_blocks)
    n_chunks = seq // Q_CHUNK

    def chunk(ci):
        q0 = ci * Q_CHUNK
        qc = lax.dynamic_slice_in_dim(q, q0, Q_CHUNK, axis=2)
        j = q0 // MOBA_BLOCK
        qpos = q0 + jnp.arange(Q_CHUNK)
        gate_s = jnp.einsum('bhqd,bhnd->bhqn', qc, k_mean).astype(jnp.float32)
        gate_s = jnp.where(blk_ids < j, gate_s, -jnp.inf)
        _, idx = lax.top_k(gate_s, n_sel)
        valid = idx < j
        kg = k_blocks[bi, hi, idx]
        vg = v_blocks[bi, hi, idx]
        s_sel = jnp.einsum('bhqd,bhqnkd->bhqnk', qc, kg).astype(jnp.float32) * scale
        s_sel = jnp.where(valid[..., None], s_sel, -jnp.inf)
        s_sel = s_sel.reshape(bsz, N_HEADS, Q_CHUNK, n_sel * MOBA_BLOCK)
        k_own = lax.dynamic_slice_in_dim(k_p, j * MOBA_BLOCK, MOBA_BLOCK, axis=2)
        v_own = lax.dynamic_slice_in_dim(v_p, j * MOBA_BLOCK, MOBA_BLOCK, axis=2)
        s_own = jnp.einsum('bhqd,bhkd->bhqk', qc, k_own).astype(jnp.float32) * scale
        kpos = j * MOBA_BLOCK + jnp.arange(MOBA_BLOCK)
        s_own = jnp.where(kpos[None, :] <= qpos[:, None], s_own, -jnp.inf)
        p = jax.nn.softmax(jnp.concatenate([s_sel, s_own], axis=-1), axis=-1).astype(v.dtype)
        p_sel = p[..., :n_sel * MOBA_BLOCK].reshape(bsz, N_HEADS, Q_CHUNK, n_sel, MOBA_BLOCK)
        p_own = p[..., n_sel * MOBA_BLOCK:]
        return (jnp.einsum('bhqnk,bhqnkd->bhqd', p_sel, vg)
                + jnp.einsum('bhqk,bhkd->bhqd', p_own, v_own))

    o = lax.map(chunk, jnp.arange(n_chunks))
    o = o.transpose(1, 0, 3, 2, 4).reshape(bsz, seq, N_HEADS * HEAD_DIM)
    return o @ w_o


def moe(h, w_router, b_router, w_gate_up, b_gate_up, w_down, b_down):
    bsz, seq, d = h.shape
    xf = h.reshape(-1, d)
    t = xf.shape[0]
    logits = (xf @ w_router + b_router).astype(jnp.float32)
    top_v, top_i = lax.top_k(logits, TOP_K)
    gates = jax.nn.softmax(top_v, axis=-1)
    flat_e = top_i.reshape(-1)
    order = jnp.argsort(flat_e)
    e_sorted = flat_e[order]
    tok = order // TOP_K
    xs = xf[tok]
    sizes = jnp.bincount(flat_e, length=N_EXPERTS).astype(jnp.int32)
    hu = lax.ragged_dot(xs, w_gate_up, sizes) + b_gate_up[e_sorted]
    g = jnp.minimum(hu[:, :D_FF], SWIGLU_LIMIT)
    up = jnp.clip(hu[:, D_FF:], -SWIGLU_LIMIT, SWIGLU_LIMIT)
    act = (up + 1.0) * g * jax.nn.sigmoid(SWIGLU_ALPHA * g)
    out = lax.ragged_dot(act, w_down, sizes) + b_down[e_sorted]
    wts = gates.reshape(-1)[order].astype(out.dtype)
    y = jnp.zeros((t, d), out.dtype).at[tok].add(out * wts[:, None])
    return y.reshape(bsz, seq, d)


def setup_inputs(seed: int = 0) -> dict:
    key = jax.random.key(seed)
    ks = iter(jax.random.split(key, 64))

    def nrm(shape, std):
        return std * jax.random.normal(next(ks), shape, jnp.float32)

    p = {}
    p['x'] = nrm((BATCH, SEQ, D_MODEL), 1.0)
    p['c'] = nrm((BATCH, D_MODEL), 1.0)
    offset = jax.random.randint(next(ks), (BATCH, 1), 0, 4096, dtype=jnp.int32)
    p['positions'] = jnp.arange(SEQ, dtype=jnp.int32)[None, :] + offset
    for i in range(DEPTH):
        pre = 'l%d_' % i
        p[pre + 'norm1_g'] = 1.0 + nrm((D_MODEL,), 0.02)
        p[pre + 'ada_w'] = nrm((D_MODEL, 6 * D_MODEL), 0.02)
        p[pre + 'ada_b'] = nrm((6 * D_MODEL,), 0.01)
        if i % N_MIXERS == 0:
            p[pre + 's5_w_in'] = nrm((D_MODEL, S5_WIDTH), D_MODEL ** -0.5)
            p[pre + 's5_b_re'] = nrm((S5_GROUPS, S5_STATE, S5_GROUP), (2 * S5_GROUP) ** -0.5)
            p[pre + 's5_b_im'] = nrm((S5_GROUPS, S5_STATE, S5_GROUP), (2 * S5_GROUP) ** -0.5)
            p[pre + 's5_c_re'] = nrm((S5_GROUPS, S5_GROUP, S5_STATE), S5_STATE ** -0.5)
            p[pre + 's5_c_im'] = nrm((S5_GROUPS, S5_GROUP, S5_STATE), S5_STATE ** -0.5)
            p[pre + 's5_lam_re'] = -0.5 + nrm((S5_GROUPS, S5_STATE), 0.01)
            p[pre + 's5_lam_im'] = (math.pi * jnp.arange(S5_STATE, dtype=jnp.float32))[None, :] + nrm((S5_GROUPS, S5_STATE), 0.01)
            p[pre + 's5_log_dt'] = jax.random.uniform(next(ks), (S5_GROUPS,), jnp.float32, math.log(S5_DT_MIN), math.log(S5_DT_MAX))
            p[pre + 's5_d'] = nrm((S5_WIDTH,), 1.0)
            p[pre + 's5_w_glu'] = nrm((S5_WIDTH, S5_WIDTH), S5_WIDTH ** -0.5)
            p[pre + 's5_w_out'] = nrm((S5_WIDTH, D_MODEL), S5_WIDTH ** -0.5)
        else:
            p[pre + 'moba_w_qkv'] = nrm((D_MODEL, 3 * N_HEADS * HEAD_DIM), D_MODEL ** -0.5)
            p[pre + 'moba_w_o'] = nrm((N_HEADS * HEAD_DIM, D_MODEL), (N_HEADS * HEAD_DIM) ** -0.5)
        p[pre + 'norm2_g'] = 1.0 + nrm((D_MODEL,), 0.02)
        p[pre + 'moe_w_router'] = nrm((D_MODEL, N_EXPERTS), D_MODEL ** -0.5)
        p[pre + 'moe_b_router'] = nrm((N_EXPERTS,), 0.01)
        p[pre + 'moe_w_gate_up'] = nrm((N_EXPERTS, D_MODEL, 2 * D_FF), D_MODEL ** -0.5)
        p[pre + 'moe_b_gate_up'] = nrm((N_EXPERTS, 2 * D_FF), 0.01)
        p[pre + 'moe_w_down'] = nrm((N_EXPERTS, D_FF, D_MODEL), D_FF ** -0.5)
        p[pre + 'moe_b_down'] = nrm((N_EXPERTS, D_MODEL), 0.01)
    p['final_norm_g'] = 1.0 + nrm((D_MODEL,), 0.02)
    return p


def reference(x, c, positions,
              l0_norm1_g, l0_ada_w, l0_ada_b,
              l0_s5_w_in, l0_s5_b_re, l0_s5_b_im, l0_s5_c_re, l0_s5_c_im,
              l0_s5_lam_re, l0_s5_lam_im, l0_s5_log_dt, l0_s5_d, l0_s5_w_glu, l0_s5_w_out,
              l0_norm2_g, l0_moe_w_router, l0_moe_b_router, l0_moe_w_gate_up, l0_moe_b_gate_up,
              l0_moe_w_down, l0_moe_b_down,
              l1_norm1_g, l1_ada_w, l1_ada_b,
              l1_moba_w_qkv, l1_moba_w_o,
              l1_norm2_g, l1_moe_w_router, l1_moe_b_router, l1_moe_w_gate_up, l1_moe_b_gate_up,
              l1_moe_w_down, l1_moe_b_down,
              final_norm_g):
    cos, sin = rope_tables(positions)
    c_act = jax.nn.silu(c)
    layers = (
        (l0_norm1_g, l0_ada_w, l0_ada_b,
         (l0_s5_w_in, l0_s5_b_re, l0_s5_b_im, l0_s5_c_re, l0_s5_c_im,
          l0_s5_lam_re, l0_s5_lam_im, l0_s5_log_dt, l0_s5_d, l0_s5_w_glu, l0_s5_w_out),
         l0_norm2_g,
         (l0_moe_w_router, l0_moe_b_router, l0_moe_w_gate_up, l0_moe_b_gate_up, l0_moe_w_down, l0_moe_b_down)),
        (l1_norm1_g, l1_ada_w, l1_ada_b,
         (l1_moba_w_qkv, l1_moba_w_o),
         l1_norm2_g,
         (l1_moe_w_router, l1_moe_b_router, l1_moe_w_gate_up, l1_moe_b_gate_up, l1_moe_w_down, l1_moe_b_down)),
    )
    for i in range(DEPTH):
        norm1_g, ada_w, ada_b, mix_p, norm2_g, moe_p = layers[i]
        ada = c_act @ ada_w + ada_b
        sh1, sc1, g1, sh2, sc2, g2 = jnp.split(ada, 6, axis=-1)
        h = modulate(rms_norm(x, norm1_g), sh1, sc1)
        if i % N_MIXERS == 0:
            m = s5_mixer(h, *mix_p)
        else:
            m = moba_mixer(h, cos, sin, *mix_p)
        x = x + g1[:, None, :] * m
        h = modulate(rms_norm(x, norm2_g), sh2, sc2)
        x = x + g2[:, None, :] * moe(h, *moe_p)
    return rms_norm(x, final_norm_g)
```

```python
import math
import numpy as np
from contextlib import ExitStack
import concourse.bass as bass
import concourse.mybir as mybir
from concourse.bass_utils import run_bass_kernel_spmd

F32 = mybir.dt.float32
BF16 = mybir.dt.bfloat16
I32 = mybir.dt.int32
AF = mybir.ActivationFunctionType
ALU = mybir.AluOpType
AX = mybir.AxisListType

QUEUES = ("pe", "act", "dve", "pool", "sp")
DMA_SLOTS = 8
SAME_Q_SYNC = True


class _Op:
    __slots__ = ("q", "fn", "dma", "deps", "sig", "cnt", "slot", "slotcnt", "inc", "semq")


class Sched:
    def __init__(self):
        self.ops = []
        self.lastw = {}
        self.readers = {}
        self.ndma = {q: 0 for q in QUEUES}
        self.ncoll = 0

    def op(self, q, fn, reads=(), writes=(), dma=False, coll=False):
        o = _Op()
        if coll:
            dma = True
        o.q, o.fn, o.dma, o.sig, o.cnt = q, fn, dma, False, 0
        o.inc, o.semq = 16, q
        deps = set()
        raw = set()
        for k in reads:
            w = self.lastw.get(k)
            if w is not None:
                deps.add(w)
                raw.add(w)
        for k in writes:
            w = self.lastw.get(k)
            if w is not None:
                deps.add(w)
            for r in self.readers.get(k, ()):
                deps.add(r)
        o.deps = []
        for d in deps:
            if d.q == q and not d.dma:
                if not (SAME_Q_SYNC and d in raw and q != "pe"):
                    continue
            d.sig = True
            o.deps.append(d)
        for k in reads:
            self.readers.setdefault(k, []).append(o)
        for k in writes:
            self.lastw[k] = o
            self.readers[k] = []
        if coll:
            o.inc, o.semq = 1, "cc"
            o.slot = self.ncoll
            o.slotcnt = 1
            self.ncoll += 1
        elif dma:
            o.slot = self.ndma[q] % DMA_SLOTS
            o.slotcnt = self.ndma[q] // DMA_SLOTS + 1
            self.ndma[q] += 1
        self.ops.append(o)
        return o

    def barrier(self):
        last = {}
        for o in self.ops:
            if o.fn is not None and not o.dma:
                last[o.q] = o
        dmas = []
        for q in QUEUES:
            dq = [o for o in self.ops if o.dma and o.q == q and o.semq == q]
            dmas += dq[-DMA_SLOTS:]
        dmas += [o for o in self.ops if o.dma and o.semq == "cc"]
        for q in QUEUES:
            o = self.op(q, None)
            for d in list(last.values()) + dmas:
                if d.q == q and not d.dma:
                    continue
                d.sig = True
                o.deps.append(d)
        self.lastw.clear()
        self.readers.clear()

    def emit(self, sems, dsems, block):
        cnt = {q: 0 for q in QUEUES}
        for o in self.ops:
            if o.dma:
                continue
            if o.sig and o.fn is not None:
                cnt[o.q] += 1
            o.cnt = cnt[o.q]
        byq = {q: [o for o in self.ops if o.q == q] for q in QUEUES}
        engs = {"pe": "tensor", "act": "scalar", "dve": "vector", "pool": "gpsimd", "sp": "sync"}

        def make(q):
            def body(e):
                waited = {}
                for o in byq[q]:
                    need = {}
                    for d in o.deps:
                        if d.dma:
                            key = ("d", d.semq, d.slot)
                            v = d.inc * d.slotcnt
                        else:
                            key = ("e", d.q)
                            v = d.cnt
                        if need.get(key, 0) < v:
                            need[key] = v
                    if o.dma and o.slotcnt > 1 and o.semq == q:
                        key = ("d", q, o.slot)
                        v = 16 * (o.slotcnt - 1)
                        if need.get(key, 0) < v:
                            need[key] = v
                    for key, v in need.items():
                        if waited.get(key, 0) >= v:
                            continue
                        waited[key] = v
                        if key[0] == "d":
                            e.wait_ge(dsems[key[1]][key[2]], v)
                        else:
                            e.wait_ge(sems[key[1]], v)
                    if o.fn is None:
                        continue
                    ins = o.fn(e)
                    if o.dma:
                        ins.then_inc(dsems[o.semq][o.slot], o.inc)
                    elif o.sig:
                        ins.then_inc(sems[q], 1)
                n = self.ndma[q]
                for s in range(min(n, DMA_SLOTS)):
                    c = (n - 1 - s) // DMA_SLOTS + 1
                    e.wait_ge(dsems[q][s], 16 * c)
            return body

        for q in QUEUES:
            getattr(block, engs[q])(make(q))


def _prod(s):
    r = 1
    for v in s:
        r *= v
    return r


ARENA_WORDS = 52224


class KB:
    def __init__(self):
        self.nc = bass.Bass("TRN2", target_bir_lowering=False)
        self.es = ExitStack()
        self.S = Sched()
        nc = self.nc
        self.arena = self.es.enter_context(nc.sbuf_tensor("arena", [128, ARENA_WORDS], F32))
        self.off = 0
        self.ps = [self.es.enter_context(nc.psum_tensor("ps%d" % i, [128, 512], F32)) for i in range(8)]
        self.sems = {q: self.es.enter_context(nc.semaphore("s_" + q)) for q in QUEUES}
        self.dsems = {q: [self.es.enter_context(nc.semaphore("d_%s%d" % (q, i))) for i in range(DMA_SLOTS)]
                      for q in QUEUES}
        self.dsems["cc"] = [self.es.enter_context(nc.semaphore("cc%d" % i)) for i in range(20)]
        self.nbank = 0
        self.uid = 0

    def alloc(self, free_shape, dt):
        n = _prod(free_shape)
        esz = 4 if dt in (F32, I32) else 2
        words = (n * esz + 3) // 4
        words = (words + 7) // 8 * 8
        assert self.off + words <= ARENA_WORDS, ("arena overflow", self.off, words)
        a = self.arena[:, self.off:self.off + words]
        self.off += words
        if esz == 2:
            a = a.bitcast(dt)
        elif dt != F32:
            a = a.bitcast(dt)
        a = a[:, 0:n]
        if len(free_shape) > 1:
            names = ["a%d" % i for i in range(len(free_shape))]
            kw = {nm: v for nm, v in zip(names, free_shape)}
            a = a.rearrange("p (%s) -> p %s" % (" ".join(names), " ".join(names)), **kw)
        return a

    def mark(self):
        return self.off

    def release(self, m):
        self.off = m

    def bank(self):
        i = self.nbank % 8
        self.nbank += 1
        return i

    def din(self, name, shape, dt=F32):
        return self.nc.dram_tensor(name, list(shape), dt, kind="ExternalInput").ap()

    def dout(self, name, shape, dt=F32):
        return self.nc.dram_tensor(name, list(shape), dt, kind="ExternalOutput").ap()

    def dtmp(self, name, shape, dt=F32):
        return self.nc.dram_tensor(name, list(shape), dt, kind="Internal").ap()

    def finish(self):
        with self.nc.Block() as block:
            self.S.emit(self.sems, self.dsems, block)
        self.es.close()
        return self.nc

    def MM(self, out, lhsT, rhs, st, sp, r, w):
        self.S.op("pe", lambda e: e.matmul(out, lhsT=lhsT, rhs=rhs, start=st, stop=sp), r, w)

    def TR(self, out, in_, idn, r, w):
        self.S.op("pe", lambda e: e.transpose(out=out, in_=in_, identity=idn), r, w)

    def ACT(self, out, in_, func, r, w, **kw):
        self.S.op("act", lambda e: e.activation(out=out, in_=in_, func=func, **kw), r, w)

    def TT(self, q, out, a, b, op, r, w):
        self.S.op(q, lambda e: e.tensor_tensor(out=out, in0=a, in1=b, op=op), r, w)

    def TS(self, q, out, a, s1, op0, r, w, s2=None, op1=None, accum=None):
        if op1 is None:
            self.S.op(q, lambda e: e.tensor_scalar(out=out, in0=a, scalar1=s1, scalar2=None, op0=op0), r, w)
        elif accum is None:
            self.S.op(q, lambda e: e.tensor_scalar(out=out, in0=a, scalar1=s1, scalar2=s2, op0=op0, op1=op1), r, w)
        else:
            self.S.op(q, lambda e: e.tensor_scalar(out=out, in0=a, scalar1=s1, scalar2=s2, op0=op0, op1=op1,
                                                   accum_out=accum), r, w)

    def STT(self, out, a, sc, b, op0, op1, r, w, accum=None):
        if accum is None:
            self.S.op("dve", lambda e: e.scalar_tensor_tensor(out=out, in0=a, scalar=sc, in1=b, op0=op0, op1=op1), r, w)
        else:
            self.S.op("dve", lambda e: e.scalar_tensor_tensor(out=out, in0=a, scalar=sc, in1=b, op0=op0, op1=op1,
                                                              accum_out=accum), r, w)

    def CP(self, q, out, in_, r, w):
        if q == "act":
            self.S.op(q, lambda e: e.copy(out=out, in_=in_), r, w)
        else:
            self.S.op(q, lambda e: e.tensor_copy(out=out, in_=in_), r, w)

    def DMA(self, q, out, in_, r, w):
        self.S.op(q, lambda e: e.dma_start(out=out, in_=in_), r, w, dma=True)

    def MEMSET(self, q, out, val, w):
        self.S.op(q, lambda e: e.memset(out, val), (), w)

    def RECIP(self, out, in_, r, w):
        self.S.op("dve", lambda e: e.reciprocal(out=out, in_=in_), r, w)

    def key(self, base):
        self.uid += 1
        return (base, self.uid)

    def consts(self):
        self.identf = self.alloc((128,), F32)
        self.identb = self.alloc((128,), BF16)
        self.eps = self.alloc((1,), F32)
        self.MEMSET("pool", self.identf, 0.0, ["identf"])
        idf = self.identf
        self.S.op("pool", lambda e: e.affine_select(out=idf, in_=idf, pattern=[[1, 128]], compare_op=ALU.not_equal,
                                                    fill=1.0, base=0, channel_multiplier=-1), ["identf"], ["identf"])
        self.CP("dve", self.identb, self.identf, ["identf"], ["identb"])
        self.MEMSET("pool", self.eps, 1e-6, ["eps"])


def ada_phase(K, crep, ada_w, ada_b, ada_dram):
    m = K.mark()
    ada = K.alloc((6144,), F32)
    csil = K.alloc((8, 128), F32)
    K.DMA("sp", csil, crep, [], ["csil"])
    K.ACT(csil, csil, AF.Silu, ["csil"], ["csil"])
    wch = [K.alloc((8, 512), F32) for _ in range(2)]
    wv = ada_w.rearrange("(kc p) n -> p kc n", p=128)
    for j in range(12):
        sl = slice(j * 512, (j + 1) * 512)
        K.DMA("sp", ada[:, sl], ada_b[sl].partition_broadcast(128), [], [("ada", j)])
    for j in range(12):
        wb = wch[j % 2]
        wk = ("adaw", j % 2)
        K.DMA("sp", wb, wv[:, :, j * 512:(j + 1) * 512], [], [wk])
        b = K.bank()
        for kc in range(8):
            K.MM(K.ps[b][:], csil[:, kc, :], wb[:, kc, :], kc == 0, kc == 7, ["csil", wk], [("ps", b)])
        sl = slice(j * 512, (j + 1) * 512)
        K.TT("dve", ada[:, sl], K.ps[b][:], ada[:, sl], ALU.add, [("ps", b), ("ada", j)], [("ada", j)])
    K.DMA("sp", ada_dram, ada, [("ada", j) for j in range(12)], ["ada_dram"])
    K.S.barrier()
    K.release(m)


def load_mod(K, ada_dram, which, norm_g, want_gate=True):
    base = 0 if which == 0 else 3
    k = K.key("mod")
    gt = None
    if want_gate:
        gt = K.alloc((1024,), F32)
        K.DMA("sp", gt, ada_dram[:, (base + 2) * 1024:(base + 3) * 1024], ["ada_dram"], [(k, "g")])
    return gt, (k, "g"), base, k


def load_mod2(K, ada_dram, base, k, norm_g):
    sh = K.alloc((1024,), F32)
    gs = K.alloc((1024,), F32)
    tmp = K.alloc((1024,), F32)
    K.DMA("sp", sh, ada_dram[:, (base + 0) * 1024:(base + 1) * 1024], ["ada_dram"], [(k, "sh")])
    K.DMA("sp", tmp, ada_dram[:, (base + 1) * 1024:(base + 2) * 1024], ["ada_dram"], [(k, "sc")])
    K.DMA("sp", gs, norm_g.partition_broadcast(128), [], [(k, "gs")])
    K.STT(gs, tmp, 1.0, gs, ALU.add, ALU.mult, [(k, "sc"), (k, "gs")], [(k, "gs")])
    return gs, sh, (k, "gs"), (k, "sh")


def moe_phase(K, x, xkey, ada_dram, P, nexp=32, dbg_gates=None):
    S = K.S
    m0 = K.mark()
    g2, kg2, mbase, mk = load_mod(K, ada_dram, 1, P["norm2_g"])
    h2T = K.alloc((8, 2048), BF16)
    gates = K.alloc((16, 32), F32)
    wtsT = K.alloc((2048,), BF16)
    bgu = K.alloc((32, 16), F32)
    bdb = K.alloc((1024,), BF16)
    K.DMA("sp", bgu, P["bguT"], [], ["bgu"])
    m1 = K.mark()
    gs2, sh2, kgs, ksh = load_mod2(K, ada_dram, mbase, mk, P["norm2_g"])
    wr = K.alloc((8, 32), F32)
    brt = K.alloc((32,), F32)
    bdf = K.alloc((1024,), F32)
    stat = K.alloc((16,), F32)
    junk = K.alloc((1024,), F32)
    K.DMA("sp", wr, P["w_router"].rearrange("(kc p) e -> p kc e", p=128), [], ["wr"])
    K.DMA("sp", brt, P["b_router"].partition_broadcast(128), [], ["brt"])
    K.DMA("sp", bdf[0:32, :], P["b_down"], [], ["bdf"])
    K.TT("dve", bdb[0:32, :], bdf[0:32, :], g2[0:32, :], ALU.mult, ["bdf", kg2], ["bdb"])
    for s in range(16):
        K.ACT(junk, x[:, s, :], AF.Square, [xkey(s)], ["junk", "stat"], accum_out=stat[:, s:s + 1])
    K.ACT(stat, stat, AF.Sqrt, ["stat"], ["stat"], scale=1.0 / 1024, bias=K.eps[:, 0:1])
    K.RECIP(stat, stat, ["stat"], ["stat"])
    tmpf = [K.alloc((1024,), F32) for _ in range(2)]
    h2f = [K.alloc((1024,), F32) for _ in range(2)]
    hTs = [K.alloc((8, 128), F32) for _ in range(2)]
    gt = [K.alloc((160,), F32) for _ in range(2)]
    for s in range(16):
        i = s % 2
        K.STT(tmpf[i], x[:, s, :], stat[:, s:s + 1], gs2, ALU.mult, ALU.mult, [xkey(s), "stat", kgs], [("tmpf", i)])
        K.TT("pool", h2f[i], tmpf[i], sh2, ALU.add, [("tmpf", i), ksh], [("h2f", i)])
        b0 = K.bank()
        b1 = K.bank()
        for kc in range(8):
            b = b0 if kc < 4 else b1
            K.TR(K.ps[b][:, (kc % 4) * 128:(kc % 4 + 1) * 128], h2f[i][:, kc * 128:(kc + 1) * 128], K.identf,
                 [("h2f", i), "identf"], [("ps", b)])
        for hh, b in enumerate((b0, b1)):
            src = K.ps[b][:].rearrange("p (a n) -> p a n", a=4)
            K.CP("dve", hTs[i][:, hh * 4:(hh + 1) * 4, :], src, [("ps", b)], [("hTs", i)])
        K.CP("act", h2T[:, :, s * 128:(s + 1) * 128], hTs[i], [("hTs", i)], [("h2T", s)])
        bl = K.bank()
        for kc in range(8):
            K.MM(K.ps[bl][:, 0:32], hTs[i][:, kc, :], wr[:, kc, :], kc == 0, kc == 7, [("hTs", i), "wr"], [("ps", bl)])
        g = gt[i]
        gk = ("gt", i)
        lg, m8, ex, em = g[:, 0:32], g[:, 32:40], g[:, 64:96], g[:, 96:128]
        negm, ssum, mask = g[:, 40:41], g[:, 41:42], g[:, 128:160]
        K.TT("dve", lg, K.ps[bl][:, 0:32], brt, ALU.add, [("ps", bl), "brt"], [gk])
        S.op("dve", lambda e, m8=m8, lg=lg: e.max(out=m8, in_=lg), [gk], [gk])
        K.TS("dve", negm, m8[:, 0:1], -1.0, ALU.mult, [gk], [gk])
        K.TS("dve", mask, lg, m8[:, 3:4], ALU.is_ge, [gk], [gk])
        K.ACT(ex, lg, AF.Exp, [gk], [gk], bias=negm, scale=1.0)
        K.STT(em, ex, 1.0, mask, ALU.mult, ALU.mult, [gk], [gk], accum=ssum)
        K.RECIP(ssum, ssum, [gk], [gk])
        K.TS("dve", gates[:, s, :], em, ssum, ALU.mult, [gk], [("gates", s)])
    for q4 in range(4):
        b = K.bank()
        for j in range(4):
            s = q4 * 4 + j
            K.TR(K.ps[b][0:32, j * 128:(j + 1) * 128], gates[:, s, :], K.identf, [("gates", s), "identf"], [("ps", b)])
        K.CP("act", wtsT[0:32, q4 * 512:(q4 + 1) * 512], K.ps[b][0:32, :], [("ps", b)], ["wtsT"])
    if dbg_gates is not None:
        K.DMA("sp", dbg_gates, gates, [("gates", s) for s in range(16)], ["dbg_gates"])
    K.S.barrier()
    K.release(m1)
    actT = K.alloc((8, 2048), BF16)
    NW = 2
    wgu_t = [K.alloc((8, 2, 256), BF16) for _ in range(NW)]
    wdn_t = [K.alloc((8, 512), BF16) for _ in range(2)]
    gsb = [K.alloc((512,), F32) for _ in range(2)]
    ssb = [K.alloc((512,), BF16) for _ in range(2)]
    usb = [K.alloc((512,), F32) for _ in range(2)]
    tdn = [K.alloc((512,), F32) for _ in range(2)]
    wguv = P["w_gate_up"]
    wdv = P["w_down"]
    chunks = []
    for e in range(nexp):
        for q in range(4):
            chunks.append(("gu", e, q))
        for dh in range(2):
            chunks.append(("dn", e, dh))
    cnt = {"gu": 0, "dn": 0}
    slot_of = {}

    def issue(ci):
        kind, e, q = chunks[ci]
        i = cnt[kind]
        cnt[kind] += 1
        if kind == "gu":
            wb = wgu_t[i % NW]
            wk = ("wgu", i % NW)
            srcv = wguv[e].rearrange("(kc p) f -> p kc f", p=128)
            K.DMA("pool", wb[:, :, 0, :], srcv[:, :, q * 256:(q + 1) * 256], [], [wk])
            K.DMA("pool", wb[:, :, 1, :], srcv[:, :, 1024 + q * 256:1024 + (q + 1) * 256], [], [wk])
            slot_of[ci] = (wb, wk)
            return
        if True:
            wb = wdn_t[i % 2]
            wk = ("wdn", i % 2)
            src = wdv[e].rearrange("(fc p) d -> p fc d", p=128)[:, :, q * 512:(q + 1) * 512]
        K.DMA("pool", wb, src, [], [wk])
        slot_of[ci] = (wb, wk)

    if chunks:
        issue(0)
    ep = 0
    dcnt = 0
    for ci, (kind, e, q) in enumerate(chunks):
        if ci + 1 < len(chunks):
            issue(ci + 1)
        wb, wk = slot_of.pop(ci)
        if kind == "gu":
            for ft in range(2):
                fcol = q * 2 + ft
                for tt in range(4):
                    bg = K.bank()
                    bu = K.bank()
                    rk = [wk] + [("h2T", s) for s in range(tt * 4, tt * 4 + 4)]
                    for kc in range(8):
                        K.MM(K.ps[bg][:], wb[:, kc, 0, ft * 128:(ft + 1) * 128], h2T[:, kc, tt * 512:(tt + 1) * 512],
                             kc == 0, kc == 7, rk, [("ps", bg)])
                    for kc in range(8):
                        K.MM(K.ps[bu][:], wb[:, kc, 1, ft * 128:(ft + 1) * 128], h2T[:, kc, tt * 512:(tt + 1) * 512],
                             kc == 0, kc == 7, rk, [("ps", bu)])
                    i = ep % 2
                    ep += 1
                    K.TS("dve", gsb[i], K.ps[bg][:], bgu[:, e, fcol:fcol + 1], ALU.add, [("ps", bg), "bgu"],
                         [("gsb", i)], s2=7.0, op1=ALU.min)
                    K.ACT(ssb[i], gsb[i], AF.Sigmoid, [("gsb", i)], [("ssb", i)], scale=1.702)
                    K.TS("dve", usb[i], K.ps[bu][:], bgu[:, e, 8 + fcol:9 + fcol], ALU.add, [("ps", bu), "bgu"],
                         [("usb", i)], s2=7.0, op1=ALU.min)
                    K.TS("dve", usb[i], usb[i], -7.0, ALU.max, [("usb", i)], [("usb", i)], s2=1.0, op1=ALU.add)
                    K.TT("pool", gsb[i], gsb[i], ssb[i], ALU.mult, [("gsb", i), ("ssb", i)], [("gsb", i)])
                    K.TT("pool", actT[:, fcol, tt * 512:(tt + 1) * 512], usb[i], gsb[i], ALU.mult,
                         [("usb", i), ("gsb", i)], [("actT", fcol, tt)])
        else:
            dh = q
            for ts in range(16):
                b = K.bank()
                for fc in range(8):
                    K.MM(K.ps[b][:], actT[:, fc, ts * 128:(ts + 1) * 128], wb[:, fc, :], fc == 0, fc == 7,
                         [("actT", fc, ts // 4), wk], [("ps", b)])
                i = dcnt % 2
                dcnt += 1
                K.STT(tdn[i], K.ps[b][:], gates[:, ts, e:e + 1], g2[:, dh * 512:(dh + 1) * 512], ALU.mult, ALU.mult,
                      [("ps", b), ("gates", ts), kg2], [("tdn", i)])
                xs = x[:, ts, dh * 512:(dh + 1) * 512]
                K.TT("pool", xs, xs, tdn[i], ALU.add, [("tdn", i), xkey(ts)], [xkey(ts)])
    for ts in range(16):
        for dh in range(2):
            b = K.bank()
            K.MM(K.ps[b][:], wtsT[0:32, ts * 128:(ts + 1) * 128], bdb[0:32, dh * 512:(dh + 1) * 512], True, True,
                 ["wtsT", "bdb"], [("ps", b)])
            xs = x[:, ts, dh * 512:(dh + 1) * 512]
            K.TT("dve", xs, K.ps[b][:], xs, ALU.add, [("ps", b), xkey(ts)], [xkey(ts)])
    K.S.barrier()
    K.release(m0)


TWO_PI = 2.0 * math.pi
CW1 = 6.28125
CW2 = TWO_PI - 6.28125


def sincos(K, theta, sin_o, cos_o, shape, kin, kout):
    tf = K.alloc(shape, F32)
    ti = K.alloc(shape, I32)
    kf = K.alloc(shape, F32)
    r = K.alloc(shape, F32)
    y = K.alloc(shape, F32)
    mk = K.alloc(shape, F32)
    k = K.key("sc")
    K.TS("dve", tf, theta, 1.0 / TWO_PI, ALU.mult, [kin], [(k, 0)], s2=0.5, op1=ALU.add)
    K.CP("dve", ti, tf, [(k, 0)], [(k, 1)])
    K.CP("dve", kf, ti, [(k, 1)], [(k, 2)])
    K.STT(r, kf, -CW1, theta, ALU.mult, ALU.add, [(k, 2), kin], [(k, 3)])
    K.STT(r, kf, -CW2, r, ALU.mult, ALU.add, [(k, 2), (k, 3)], [(k, 3)])
    for shift, outp in ((0.0, sin_o), (math.pi / 2, cos_o)):
        K.TS("dve", y, r, shift, ALU.add, [(k, 3)], [(k, 4)])
        K.TS("dve", mk, y, math.pi, ALU.is_gt, [(k, 4)], [(k, 5)])
        K.STT(y, mk, -TWO_PI, y, ALU.mult, ALU.add, [(k, 5), (k, 4)], [(k, 4)])
        K.TS("dve", mk, y, -math.pi, ALU.is_lt, [(k, 4)], [(k, 5)])
        K.STT(y, mk, TWO_PI, y, ALU.mult, ALU.add, [(k, 5), (k, 4)], [(k, 4)])
        K.ACT(outp, y, AF.Sin, [(k, 4)], [kout])


def bc_last(ap, n):
    return bass.AP(tensor=ap.tensor, offset=ap.offset, ap=[list(d) for d in ap.ap] + [[0, n]])


def s5_tables(K, P, Cs, Mt, Bs, C1, C2):
    m = K.mark()
    kk = "s5t"
    lam_r = K.alloc((64,), F32)
    lam_i = K.alloc((64,), F32)
    ldt = K.alloc((64,), F32)
    bPr = K.alloc((64, 16), F32)
    bPi = K.alloc((64, 16), F32)
    cPr = K.alloc((64, 16), F32)
    cPi = K.alloc((64, 16), F32)
    dS = K.alloc((64,), F32)
    for t, nm in ((lam_r, "lamP_re"), (lam_i, "lamP_im"), (ldt, "logdtP"), (bPr, "bP_re"), (bPi, "bP_im"),
                  (cPr, "cP_re"), (cPi, "cP_im"), (dS, "dS")):
        K.DMA("sp", t, P[nm], [], [kk])
    dt = K.alloc((64,), F32)
    lr = K.alloc((64,), F32)
    li = K.alloc((64,), F32)
    mag = K.alloc((64,), F32)
    imag = K.alloc((64,), F32)
    sn = K.alloc((64,), F32)
    cs = K.alloc((64,), F32)
    t1 = K.alloc((64,), F32)
    t2 = K.alloc((64,), F32)
    K.ACT(dt, ldt, AF.Exp, [kk], [kk])
    K.TT("dve", lr, lam_r, dt, ALU.mult, [kk], [kk])
    K.TT("dve", li, lam_i, dt, ALU.mult, [kk], [kk])
    for outp, sg in ((mag, 1.0), (imag, -1.0)):
        K.TS("dve", outp, lr, sg / 6.0, ALU.mult, [kk], [kk], s2=1.0, op1=ALU.add)
        for dnm in (5.0, 4.0, 3.0, 2.0, 1.0):
            K.TT("dve", outp, outp, lr, ALU.mult, [kk], [kk])
            K.TS("dve", outp, outp, sg / dnm, ALU.mult, [kk], [kk], s2=1.0, op1=ALU.add)
    sincos(K, li, sn, cs, (64,), kk, kk)
    apr = K.alloc((9, 64), F32)
    api = K.alloc((9, 64), F32)
    ivr = K.alloc((8, 64), F32)
    ivi = K.alloc((8, 64), F32)
    K.MEMSET("dve", apr[:, 0, :], 1.0, [kk])
    K.MEMSET("dve", api[:, 0, :], 0.0, [kk])
    K.MEMSET("dve", ivr[:, 0, :], 1.0, [kk])
    K.MEMSET("dve", ivi[:, 0, :], 0.0, [kk])
    K.TT("dve", apr[:, 1, :], mag, cs, ALU.mult, [kk], [kk])
    K.TT("dve", api[:, 1, :], mag, sn, ALU.mult, [kk], [kk])
    K.TT("dve", ivr[:, 1, :], imag, cs, ALU.mult, [kk], [kk])
    K.TT("dve", t1, imag, sn, ALU.mult, [kk], [kk])
    K.TS("dve", ivi[:, 1, :], t1, -1.0, ALU.mult, [kk], [kk])

    def cmul(orr, oi, ar, ai, br, bi, ta, tb, q="dve"):
        K.TT(q, ta, ar, br, ALU.mult, [kk], [kk])
        K.TT(q, tb, ai, bi, ALU.mult, [kk], [kk])
        K.TT(q, orr, ta, tb, ALU.subtract, [kk], [kk])
        K.TT(q, ta, ar, bi, ALU.mult, [kk], [kk])
        K.TT(q, tb, ai, br, ALU.mult, [kk], [kk])
        K.TT(q, oi, ta, tb, ALU.add, [kk], [kk])

    for t in range(2, 9):
        cmul(apr[:, t, :], api[:, t, :], apr[:, t - 1, :], api[:, t - 1, :], apr[:, 1, :], api[:, 1, :], t1, t2)
    for t in range(2, 8):
        cmul(ivr[:, t, :], ivi[:, t, :], ivr[:, t - 1, :], ivi[:, t - 1, :], ivr[:, 1, :], ivi[:, 1, :], t1, t2)
    for h in range(2):
        rows = slice(64 * h, 64 * h + 64)
        gsl = slice(32 * h, 32 * h + 32)
        K.CP("dve", C1[rows, 0, :], apr[rows, 8, gsl], [kk], [kk])
        K.CP("dve", C1[rows, 1, :], apr[rows, 8, gsl], [kk], [kk])
        K.TS("dve", C2[rows, 0, :], api[rows, 8, gsl], -1.0, ALU.mult, [kk], [kk])
        K.CP("dve", C2[rows, 1, :], api[rows, 8, gsl], [kk], [kk])
    den = K.alloc((64,), F32)
    fr = K.alloc((64,), F32)
    fi = K.alloc((64,), F32)
    am1 = K.alloc((64,), F32)
    K.TT("dve", den, lam_r, lam_r, ALU.mult, [kk], [kk])
    K.TT("dve", t1, lam_i, lam_i, ALU.mult, [kk], [kk])
    K.TT("dve", den, den, t1, ALU.add, [kk], [kk])
    K.RECIP(den, den, [kk], [kk])
    K.TS("dve", am1, apr[:, 1, :], -1.0, ALU.add, [kk], [kk])
    K.TT("dve", t1, am1, lam_r, ALU.mult, [kk], [kk])
    K.TT("dve", t2, api[:, 1, :], lam_i, ALU.mult, [kk], [kk])
    K.TT("dve", t1, t1, t2, ALU.add, [kk], [kk])
    K.TT("dve", fr, t1, den, ALU.mult, [kk], [kk])
    K.TT("dve", t1, api[:, 1, :], lam_r, ALU.mult, [kk], [kk])
    K.TT("dve", t2, am1, lam_i, ALU.mult, [kk], [kk])
    K.TT("dve", t1, t1, t2, ALU.subtract, [kk], [kk])
    K.TT("dve", fi, t1, den, ALU.mult, [kk], [kk])
    bbr = K.alloc((64, 16), F32)
    bbi = K.alloc((64, 16), F32)
    u1 = K.alloc((64, 16), F32)
    u2 = K.alloc((64, 16), F32)
    frb, fib = bc_last(fr, 16), bc_last(fi, 16)
    cmul(bbr, bbi, frb, fib, bPr, bPi, u1, u2)
    for h in range(2):
        rows = slice(64 * h, 64 * h + 64)
        gsl = slice(32 * h, 32 * h + 32)
        for tau in range(8):
            pr = bc_last(apr[rows, tau + 1, gsl], 16)
            pi = bc_last(api[rows, tau + 1, gsl], 16)
            a1 = u1[rows, 0:32, :]
            a2 = u2[rows, 0:32, :]
            K.TT("dve", a1, cPr[rows, gsl, :], pr, ALU.mult, [kk], [kk])
            K.TT("dve", a2, cPi[rows, gsl, :], pi, ALU.mult, [kk], [kk])
            K.TT("dve", Cs[rows, 0, :, tau, :], a1, a2, ALU.subtract, [kk], ["Cs"])
            K.TT("dve", a1, cPr[rows, gsl, :], pi, ALU.mult, [kk], [kk])
            K.TT("dve", a2, cPi[rows, gsl, :], pr, ALU.mult, [kk], [kk])
            K.STT(Cs[rows, 1, :, tau, :], a1, -1.0, a2, ALU.mult, ALU.subtract, [kk], ["Cs"])
    Kk2f = K.alloc((64, 128), F32)
    Q2f = K.alloc((64, 128), F32)
    BsPf = K.alloc((64, 128), F32)
    Kk2 = Kk2f.rearrange("p g (s c) -> p g s c", s=8)
    Q2 = Q2f.rearrange("p g (s c) -> p g s c", s=8)
    BsP = BsPf.rearrange("p g (s c) -> p g s c", s=8)
    lo = slice(0, 64)
    hi = slice(64, 128)
    for s in range(8):
        for (dst, pw_r, pw_i) in ((Kk2, ivr[:, s, :], ivi[:, s, :]), (BsP, apr[:, 7 - s, :], api[:, 7 - s, :])):
            K.TT("dve", u1[lo], bbr[lo], bc_last(pw_r[lo], 16), ALU.mult, [kk], [kk])
            K.TT("dve", u2[lo], bbi[lo], bc_last(pw_i[lo], 16), ALU.mult, [kk], [kk])
            K.TT("dve", dst[lo, :, s, :], u1[lo], u2[lo], ALU.subtract, [kk], [kk])
            K.TT("dve", u1[hi], bbi[hi], bc_last(pw_r[hi], 16), ALU.mult, [kk], [kk])
            K.TT("dve", u2[hi], bbr[hi], bc_last(pw_i[hi], 16), ALU.mult, [kk], [kk])
            K.TT("dve", dst[hi, :, s, :], u1[hi], u2[hi], ALU.add, [kk], [kk])
        pw_r, pw_i = apr[:, s, :], api[:, s, :]
        K.TT("dve", u1[lo], cPr[lo], bc_last(pw_r[lo], 16), ALU.mult, [kk], [kk])
        K.TT("dve", u2[lo], cPi[lo], bc_last(pw_i[lo], 16), ALU.mult, [kk], [kk])
        K.TT("dve", Q2[lo, :, s, :], u1[lo], u2[lo], ALU.subtract, [kk], [kk])
        K.TT("dve", u1[hi], cPr[hi], bc_last(pw_i[hi], 16), ALU.mult, [kk], [kk])
        K.TT("dve", u2[hi], cPi[hi], bc_last(pw_r[hi], 16), ALU.mult, [kk], [kk])
        K.TT("dve", u1[hi], u1[hi], u2[hi], ALU.add, [kk], [kk])
        K.TS("dve", Q2[hi, :, s, :], u1[hi], -1.0, ALU.mult, [kk], [kk])
    msk = K.alloc((8, 16), F32)
    K.MEMSET("pool", msk, 1.0, [kk])
    K.S.op("pool", lambda e: e.affine_select(out=msk, in_=msk, pattern=[[16, 8], [0, 16]], compare_op=ALU.is_ge,
                                             fill=0.0, base=15, channel_multiplier=-1), [kk], [kk])
    mtmp = K.alloc((4, 128), F32)
    for gq in range(16):
        b = K.bank()
        for j in range(4):
            g = gq * 4 + j
            K.MM(K.ps[b][:, j * 128:(j + 1) * 128], Kk2f[:, g, :], Q2f[:, g, :], True, True, [kk], [("ps", b)])
        mskb = bass.AP(tensor=msk.tensor, offset=msk.offset, ap=[list(msk.ap[0]), [0, 4], [1, 128]])
        K.TT("dve", mtmp, K.ps[b][:].rearrange("p (j n) -> p j n", j=4), mskb, ALU.mult, [("ps", b), kk], ["mtmp"])
        for j in range(4):
            g = gq * 4 + j
            K.STT(Mt[:, g, :], K.identf, dS[:, g:g + 1], mtmp[:, j, :], ALU.mult, ALU.add, ["mtmp", kk, "identf"],
                  ["Mt"])
    for gq in range(16):
        b = K.bank()
        for j in range(4):
            g = gq * 4 + j
            K.TR(K.ps[b][:, j * 128:(j + 1) * 128], BsPf[:, g, :], K.identf, [kk, "identf"], [("ps", b)])
        K.CP("act", Bs[:, gq * 4:(gq + 1) * 4, :, :], K.ps[b][:].rearrange("p (j r q) -> p j r q", j=4, r=2),
             [("ps", b)], ["Bs"])
    K.S.barrier()
    K.release(m)


def s5_exchange(K, Z, C1, C2, selm, st_l, st_g, groups):
    kk = "xch"
    m = K.mark()
    K.DMA("sp", st_l, Z.rearrange("p a b -> p (a b)"), [("Zf", 0), ("Zf", 1)], ["st_l"])
    K.S.op("pool", lambda e: e.collective_compute("AllGather", ALU.bypass, replica_groups=groups, ins=[st_l],
                                                  outs=[st_g]), ["st_l"], ["st_g"], coll=True)
    G = K.alloc((4, 64), F32)
    K.DMA("sp", G, st_g.rearrange("(r p) c -> p r c", r=4), ["st_g"], [kk])
    Pk1 = K.alloc((3, 64), F32)
    Pk2 = K.alloc((3, 64), F32)
    t1 = K.alloc((64,), F32)
    t2 = K.alloc((64,), F32)
    c1f = C1.rearrange("p a b -> p (a b)")
    c2f = C2.rearrange("p a b -> p (a b)")
    K.MEMSET("dve", Pk1[:, 0, :], 1.0, [kk])
    K.MEMSET("dve", Pk2[:, 0, :], 0.0, [kk])
    K.CP("dve", Pk1[:, 1, :], c1f, [kk], [kk])
    K.CP("dve", Pk2[:, 1, :], c2f, [kk], [kk])

    def sq(a1, a2, o1, o2):
        K.TT("dve", t1, a1, a1, ALU.mult, [kk], [kk])
        K.TT("dve", t2, a2, a2, ALU.mult, [kk], [kk])
        K.TT("dve", t2, t1, t2, ALU.subtract, [kk], [kk])
        K.TT("dve", t1, a1, a2, ALU.mult, [kk], [kk])
        K.TS("dve", o2, t1, 2.0, ALU.mult, [kk], [kk])
        K.CP("dve", o1, t2, [kk], [kk])

    for _ in range(8):
        sq(Pk1[:, 1, :], Pk2[:, 1, :], Pk1[:, 1, :], Pk2[:, 1, :])
    sq(Pk1[:, 1, :], Pk2[:, 1, :], Pk1[:, 2, :], Pk2[:, 2, :])
    acc = K.alloc((64,), F32)
    cc1 = K.alloc((64,), F32)
    cc2 = K.alloc((64,), F32)
    K.MEMSET("dve", acc, 0.0, [kk])
    for r in range(4):
        K.TS("dve", cc1, Pk1[:, 0, :], selm[:, r * 3:r * 3 + 1], ALU.mult, [kk], [kk])
        K.TS("dve", cc2, Pk2[:, 0, :], selm[:, r * 3:r * 3 + 1], ALU.mult, [kk], [kk])
        for mm in (1, 2):
            K.STT(cc1, Pk1[:, mm, :], selm[:, r * 3 + mm:r * 3 + mm + 1], cc1, ALU.mult, ALU.add, [kk], [kk])
            K.STT(cc2, Pk2[:, mm, :], selm[:, r * 3 + mm:r * 3 + mm + 1], cc2, ALU.mult, ALU.add, [kk], [kk])
        g = G[:, r, :]
        gsw = bass.AP(tensor=g.tensor, offset=g.offset + 32, ap=[list(g.ap[0]), [-32, 2], [1, 32]])
        K.TT("dve", t1, cc1, g, ALU.mult, [kk], [kk])
        K.TT("dve", t2.rearrange("p (a b) -> p a b", a=2), cc2.rearrange("p (a b) -> p a b", a=2), gsw, ALU.mult,
             [kk], [kk])
        K.TT("dve", t1, t1, t2, ALU.add, [kk], [kk])
        K.TT("dve", acc, acc, t1, ALU.add, [kk], [kk])
    K.CP("dve", Z.rearrange("p a b -> p (a b)"), acc, [kk], [("Zf", 0), ("Zf", 1)])
    K.S.barrier()
    K.release(m)


def phase_S5(K, P, xmid, ada_dram, st_l, st_g, groups):
    S = K.S
    mT = K.mark()
    Csf = K.alloc((2, 32, 128), BF16)
    Cs = Csf.rearrange("p r g (t c) -> p r g t c", t=8)
    Mt = K.alloc((64, 128), BF16)
    Bs = K.alloc((64, 2, 64), BF16)
    C1 = K.alloc((2, 32), F32)
    C2 = K.alloc((2, 32), F32)
    mS5 = K.mark()
    ada_phase(K, P["crep"], P["ada_w"], P["ada_b"], ada_dram)
    s5_tables(K, P, Cs, Mt, Bs, C1, C2)
    w_in = K.alloc((8, 1024), BF16)
    w_glu = K.alloc((8, 1024), BF16)
    w_out = K.alloc((8, 1024), BF16)
    K.DMA("pool", w_in, P["w_in"].rearrange("(kc p) n -> p kc n", p=128), [], ["w_in"])
    K.DMA("pool", w_glu, P["w_glu"].rearrange("(kc p) n -> p kc n", p=128), [], ["w_glu"])
    mw = K.mark()
    g1, kg1, mbase, mk = load_mod(K, ada_dram, 0, P["norm1_g"])
    wst = K.alloc((8, 1024), F32)
    K.DMA("sp", wst, P["w_out"].rearrange("(kc p) n -> p kc n", p=128), [], ["wst"])
    for kc in range(8):
        K.TT("dve" if kc % 2 else "pool", w_out[:, kc, :], wst[:, kc, :], g1, ALU.mult, ["wst", kg1], ["w_out"])
    K.S.barrier()
    K.release(mw)
    gs1, sh1, kgs, ksh = load_mod2(K, ada_dram, mbase, mk, P["norm1_g"])
    selm = K.alloc((12,), F32)
    K.DMA("sp", selm, P["selm"], [], ["xch"])
    xrow = [K.alloc((1024,), F32) for _ in range(2)]
    xr2 = xrow
    tmpf = K.alloc((1024,), F32)
    junk = tmpf
    hrow = [K.alloc((1024,), BF16) for _ in range(2)]
    hTt = [K.alloc((8, 128), BF16) for _ in range(2)]
    zTt = [K.alloc((8, 128), BF16) for _ in range(2)]
    zgTt = [K.alloc((8, 128), BF16) for _ in range(2)]
    sgt = [K.alloc((512,), BF16) for _ in range(2)]
    xo = [K.alloc((512,), F32) for _ in range(2)]
    u = K.alloc((8, 1024), BF16)
    ug = u.rearrange("p t (g c) -> p (t g c)", g=64).rearrange("p (g s c) -> p g s c", g=64, s=8)
    ug2 = u.rearrange("p t c -> p (t c)").rearrange("p (g k) -> p g k", g=64)
    UT = K.alloc((64, 128), BF16)
    Sb = K.alloc((2, 32, 129), BF16)
    Zf = [K.alloc((2, 32), F32) for _ in range(2)]
    T1 = K.alloc((2, 32), F32)
    T2 = K.alloc((2, 32), F32)
    st = K.alloc((2,), F32)
    K.MEMSET("dve", Zf[0], 0.0, [("Zf", 0)])
    xsv = P["xs"].rearrange("(H n t) d -> H t n d", H=2, t=8)
    xmv = xmid.rearrange("(H n t) d -> H t n d", H=2, t=8)
    zcur = 0
    ukeys = [("u", t) for t in range(8)]
    for hs in range(4):
        pss, H = divmod(hs, 2)
        own = pss == 1
        if hs == 2:
            s5_exchange(K, Zf[zcur], C1, C2, selm, st_l, st_g, groups)
        for tau in range(8):
            i = tau % 2
            xr = xrow[i]
            K.DMA("sp", xr, xsv[H, tau], [], [("xrow", i)])
            K.ACT(junk, xr, AF.Square, [("xrow", i)], ["tmpf", "st"], accum_out=st[:, 0:1])
            K.ACT(st[:, 1:2], st[:, 0:1], AF.Sqrt, ["st"], ["st2"], scale=1.0 / 1024, bias=K.eps[:, 0:1])
            K.RECIP(st[:, 1:2], st[:, 1:2], ["st2"], ["st2"])
            K.STT(tmpf, xr, st[:, 1:2], gs1, ALU.mult, ALU.mult, [("xrow", i), "st2", kgs], ["tmpf"])
            K.TT("pool", hrow[i], tmpf, sh1, ALU.add, ["tmpf", ksh], [("hrow", i)])
            b = K.bank()
            psb = K.ps[b].bitcast(BF16)
            for kc in range(8):
                K.TR(psb[:, kc * 128:(kc + 1) * 128], hrow[i][:, kc * 128:(kc + 1) * 128], K.identb,
                     [("hrow", i), "identb"], [("ps", b)])
            K.CP("act", hTt[i], psb[:, 0:1024].rearrange("p (a n) -> p a n", a=8), [("ps", b)], [("hTt", i)])
            for ch in range(2):
                b = K.bank()
                for kc in range(8):
                    K.MM(K.ps[b][:], hTt[i][:, kc, :], w_in[:, kc, ch * 512:(ch + 1) * 512], kc == 0, kc == 7,
                         [("hTt", i), "w_in"], [("ps", b)])
                K.CP("dve" if ch else "act", ug[:, ch * 32:(ch + 1) * 32, tau, :],
                     K.ps[b][:].rearrange("p (g c) -> p g c", g=32), [("ps", b)], ukeys)
        for g8 in range(8):
            b = K.bank()
            psb = K.ps[b].bitcast(BF16)
            for j in range(8):
                g = g8 * 8 + j
                K.TR(psb[:, j * 128:(j + 1) * 128], ug2[:, g, :], K.identb, ukeys + ["identb"],
                     [("ps", b)])
            K.CP("act" if g8 % 2 else "dve", UT[:, g8 * 8:(g8 + 1) * 8, :],
                 psb[:, 0:1024].rearrange("p (a n) -> p a n", a=8), [("ps", b)], [("UT", g8)])
        for q in range(16):
            b = K.bank()
            for ri in range(2):
                for gp in range(2):
                    for h in range(2):
                        g = h * 32 + 2 * q + gp
                        c0 = (ri * 2 + gp) * 128
                        K.MM(K.ps[b][64 * h:64 * h + 64, c0:c0 + 128], Bs[:, g, ri, :], UT[:, g, :], True, True,
                             ["Bs", ("UT", g // 8)], [("ps", b)])
            K.ACT(Sb[:, :, 2 * q:2 * q + 2, 1:129], K.ps[b][:].rearrange("p (r g n) -> p r g n", r=2, g=2),
                  AF.Identity, [("ps", b)], ["Sb", "Sbx"])
        for n in range(128):
            zc = Zf[zcur]
            zn = Zf[1 - zcur]
            if own:
                K.CP("act", Sb[:, :, :, n], zc, [("Zf", zcur)], ["Sbx"])
            zsw = bass.AP(tensor=zc.tensor, offset=zc.offset + 32, ap=[list(zc.ap[0]), [-32, 2], [1, 32]])
            K.TT("dve", T1, C1, zc, ALU.mult, [("Zf", zcur)], ["T1"])
            K.TT("pool", T2, C2, zsw, ALU.mult, [("Zf", zcur)], ["T2"])
            K.TT("dve", T1, T1, T2, ALU.add, ["T1", "T2"], ["T1"])
            K.TT("dve", zn, T1, Sb[:, :, :, n + 1], ALU.add, ["T1", "Sb"], [("Zf", 1 - zcur)])
            zcur = 1 - zcur
        if not own:
            continue
        for gq in range(16):
            b = K.bank()
            for j in range(4):
                g = gq * 4 + j
                h, g32 = divmod(g, 32)
                rows = slice(64 * h, 64 * h + 64)
                o = K.ps[b][:, j * 128:(j + 1) * 128]
                K.MM(o, UT[:, g, :], Mt[:, g, :], True, False, [("UT", g // 8), "Mt"], [("ps", b)])
                K.MM(o, Sb[rows, 0, g32, 0:128], Csf[rows, 0, g32, :], False, False, ["Sbx", "Cs"], [("ps", b)])
                K.MM(o, Sb[rows, 1, g32, 0:128], Csf[rows, 1, g32, :], False, True, ["Sbx", "Cs"], [("ps", b)])
            zo = u[:, :, gq * 64:(gq + 1) * 64].rearrange("p t (j c) -> p j t c", j=4)
            K.ACT(zo, K.ps[b][:].rearrange("p (j t c) -> p j t c", j=4, t=8), AF.Gelu_apprx_tanh, [("ps", b)], ukeys)
        for tau in range(8):
            i = tau % 2
            b = K.bank()
            psb = K.ps[b].bitcast(BF16)
            for kc in range(8):
                K.TR(psb[:, kc * 128:(kc + 1) * 128], u[:, tau, kc * 128:(kc + 1) * 128], K.identb,
                     [("u", tau), "identb"], [("ps", b)])
            K.CP("act", zTt[i], psb[:, 0:1024].rearrange("p (a n) -> p a n", a=8), [("ps", b)], [("zTt", i)])
            for half in range(2):
                b = K.bank()
                for c4 in range(4):
                    co = half * 4 + c4
                    for kc in range(8):
                        K.MM(K.ps[b][:, c4 * 128:(c4 + 1) * 128], w_glu[:, kc, co * 128:(co + 1) * 128], zTt[i][:, kc, :],
                             kc == 0, kc == 7, ["w_glu", ("zTt", i)], [("ps", b)])
                K.ACT(sgt[half], K.ps[b][:], AF.Sigmoid, [("ps", b)], [("sgt", half)])
                K.TT("pool", zgTt[i][:, half * 4:(half + 1) * 4, :], zTt[i][:, half * 4:(half + 1) * 4, :],
                     sgt[half].rearrange("p (a n) -> p a n", a=4), ALU.mult, [("zTt", i), ("sgt", half)],
                     [("zgTt", i, half)])
            K.DMA("sp", xr2[i], xsv[H, tau], [], [("xrow", i)])
            for dh in range(2):
                b = K.bank()
                for kc in range(8):
                    K.MM(K.ps[b][:], zgTt[i][:, kc, :], w_out[:, kc, dh * 512:(dh + 1) * 512], kc == 0, kc == 7,
                         [("zgTt", i, kc // 4), "w_out"], [("ps", b)])
                K.TT("dve", xo[dh], K.ps[b][:], xr2[i][:, dh * 512:(dh + 1) * 512], ALU.add, [("ps", b), ("xrow", i)],
                     [("xo", dh)])
                K.DMA("sp", xmv[H, tau][:, dh * 512:(dh + 1) * 512], xo[dh], [("xo", dh)], ["xmid"])
    K.S.barrier()
    K.release(mT)
    return


SCALE = 128.0 ** -0.5
NEGB = -30000.0


def norm_slots(K, x, xkey, stat, junk):
    for s in range(16):
        K.ACT(junk, x[:, s, :], AF.Square, [xkey(s)], ["junk", "stat"], accum_out=stat[:, s:s + 1])
    K.ACT(stat, stat, AF.Sqrt, ["stat"], ["stat"], scale=1.0 / 1024, bias=K.eps[:, 0:1])
    K.RECIP(stat, stat, ["stat"], ["stat"])


def phase_L2(K, P, x, xkey, ada_dram, pos, qT_o, kT_o, v_o, km_o, after_kv=None):
    S = K.S
    mL2 = K.mark()
    cosT = K.alloc((2048,), F32)
    sinT = K.alloc((2048,), F32)
    Rm = K.alloc((128,), BF16)
    K.TS("dve", Rm[:, 0:64], K.identf[:, 64:128], -1.0, ALU.mult, ["identf"], ["Rm"])
    K.CP("dve", Rm[:, 64:128], K.identf[:, 0:64], ["identf"], ["Rm"])
    mr = K.mark()
    posi = K.alloc((2048,), I32)
    ang = K.alloc((2048,), F32)
    invf = K.alloc((1,), F32)
    K.DMA("sp", posi, pos.partition_broadcast(128), [], ["posi"])
    K.DMA("sp", invf, P["invf"], [], ["invf"])
    K.CP("dve", ang, posi, ["posi"], ["ang"])
    K.TS("dve", ang, ang, invf[:, 0:1], ALU.mult, ["ang", "invf"], ["ang"])
    sincos(K, ang, sinT, cosT, (2048,), "ang", "rope")
    K.S.barrier()
    K.release(mr)
    g1, kg1, mbase, mk = load_mod(K, ada_dram, 0, P["norm1_g"], want_gate=False)
    gs1, sh1, kgs, ksh = load_mod2(K, ada_dram, mbase, mk, P["norm1_g"])
    hT = K.alloc((8, 2048), BF16)
    wqb = [K.alloc((8, 1024), BF16) for _ in range(2)]
    wqv = P["w_qkv"].rearrange("(kc p) n -> p kc n", p=128)
    K.DMA("pool", wqb[1], wqv[:, :, 1024:2048], [], [("wq", 1)])
    K.DMA("pool", wqb[0], wqv[:, :, 2048:3072], [], [("wq", 0)])
    stat = K.alloc((16,), F32)
    tmpf = K.alloc((1024,), F32)
    hrow = [K.alloc((1024,), BF16) for _ in range(2)]
    for s in range(16):
        i = s % 2
        K.ACT(tmpf, x[:, s, :], AF.Square, [xkey(s)], ["junk", "st"], accum_out=stat[:, 0:1])
        K.ACT(stat[:, 1:2], stat[:, 0:1], AF.Sqrt, ["st"], ["st2"], scale=1.0 / 1024, bias=K.eps[:, 0:1])
        K.RECIP(stat[:, 1:2], stat[:, 1:2], ["st2"], ["st2"])
        K.STT(tmpf, x[:, s, :], stat[:, 1:2], gs1, ALU.mult, ALU.mult, [xkey(s), "st2", kgs], ["junk"])
        K.TT("pool", hrow[i], tmpf, sh1, ALU.add, ["junk", ksh], [("hrow", i)])
        b = K.bank()
        psb = K.ps[b].bitcast(BF16)
        for kc in range(8):
            K.TR(psb[:, kc * 128:(kc + 1) * 128], hrow[i][:, kc * 128:(kc + 1) * 128], K.identb,
                 [("hrow", i), "identb"], [("ps", b)])
        K.CP("act", hT[:, :, s * 128:(s + 1) * 128], psb[:, 0:1024].rearrange("p (a n) -> p a n", a=8),
             [("ps", b)], [("hT", s)])
    tf = [K.alloc((512,), F32) for _ in range(2)]
    tb = [K.alloc((512,), BF16) for _ in range(2)]
    ta = [K.alloc((512,), F32) for _ in range(2)]
    tq = [K.alloc((512,), F32) for _ in range(2)]
    tk = [K.alloc((512,), BF16) for _ in range(2)]
    km = K.alloc((8, 8), F32)
    it = [0]

    def qk_pass(which):
        for h in range(8):
            for tt in range(4):
                i = it[0] % 2
                it[0] += 1
                b = K.bank()
                tsl = slice(tt * 512, (tt + 1) * 512)
                c0 = h * 128
                for kc in range(8):
                    K.MM(K.ps[b][:], wqb[which][:, kc, c0:c0 + 128], hT[:, kc, tsl], kc == 0, kc == 7,
                         [("wq", which)] + [("hT", s) for s in range(tt * 4, tt * 4 + 4)], [("ps", b)])
                K.CP("act", tf[i], K.ps[b][:], [("ps", b)], [("tf", i)])
                K.CP("dve", tb[i], tf[i], [("tf", i)], [("tb", i)])
                b2 = K.bank()
                K.MM(K.ps[b2][:], Rm, tb[i], True, True, ["Rm", ("tb", i)], [("ps", b2)])
                K.TT("dve", ta[i], K.ps[b2][:], sinT[:, tsl], ALU.mult, [("ps", b2), "rope"], [("ta", i)])
                K.TT("dve", tf[i], tf[i], cosT[:, tsl], ALU.mult, [("tf", i), "rope"], [("tf", i)])
                if which == 0:
                    K.TT("dve", tq[i], tf[i], ta[i], ALU.add, [("tf", i), ("ta", i)], [("tq", i)])
                    K.ACT(tq[i], tq[i], AF.Copy, [("tq", i)], [("tq", i)], scale=SCALE)
                    K.DMA("sp", qT_o[h][:, tsl], tq[i], [("tq", i)], ["qTo"])
                else:
                    K.TT("dve", tq[i], tf[i], ta[i], ALU.add, [("tf", i), ("ta", i)], [("tq", i)])
                    K.CP("act", tk[i], tq[i], [("tq", i)], [("tk", i)])
                    K.DMA("sp", kT_o[h][:, tsl], tk[i], [("tk", i)], [("kTo", h)])
                    K.S.op("dve", lambda e, o=km[:, h, tt * 2:tt * 2 + 2], a=tq[i].rearrange("p (b n) -> p b n", b=2):
                           e.tensor_reduce(out=o, in_=a, axis=AX.X, op=ALU.add), [("tq", i)], ["km"])

    qk_pass(1)
    K.TS("dve", km, km, 1.0 / 256, ALU.mult, ["km"], ["km"])
    K.DMA("sp", km_o.rearrange("(h d) n -> d h n", h=8), km, ["km"], ["kmo"])
    vb = [K.alloc((512,), BF16) for _ in range(2)]
    for s in range(16):
        for dh in range(2):
            i = (s * 2 + dh) % 2
            b = K.bank()
            for kc in range(8):
                K.MM(K.ps[b][:], hT[:, kc, s * 128:(s + 1) * 128], wqb[0][:, kc, dh * 512:(dh + 1) * 512],
                     kc == 0, kc == 7, [("hT", s), ("wq", 0)], [("ps", b)])
            K.CP("act" if dh else "dve", vb[i], K.ps[b][:], [("ps", b)], [("vb", i)])
            for h4 in range(4):
                hh = dh * 4 + h4
                K.DMA("sp", v_o[hh][s * 128:(s + 1) * 128, :], vb[i][:, h4 * 128:(h4 + 1) * 128], [("vb", i)], [("vo", hh)])
    if after_kv is not None:
        after_kv()
    K.DMA("pool", wqb[0], wqv[:, :, 0:1024], [], [("wq", 0)])
    qk_pass(0)
    K.S.barrier()
    K.release(mL2)


def phase_L3(K, P, x, xkey, ada_dram, qT_l, kT_l, v_l, kT_g, v_g, km_g, out, nexp=32):
    S = K.S
    dbg = False
    mO = K.mark()
    oT = K.alloc((8, 2048), BF16)
    mA = K.mark()
    onesb = K.alloc((128,), BF16)
    K.MEMSET("pool", onesb, 1.0, ["onesb"])
    cmask = K.alloc((4, 512), BF16)
    K.MEMSET("pool", cmask, 0.0, ["cmask"])
    for i in range(4):
        bk = i // 2
        cm = cmask[:, i, bk * 256:(bk + 1) * 256]
        S.op("pool", lambda e, cm=cm, i=i: e.affine_select(out=cm, in_=cm, pattern=[[1, 256]], compare_op=ALU.is_ge,
                                                           fill=NEGB, base=-128 * (i % 2), channel_multiplier=-1),
             ["cmask"], ["cmask"])
    NT = 64
    Ind = K.alloc((40, 128), BF16)
    K.MEMSET("pool", Ind, 0.0, ["Ind"])
    K.DMA("sp", Ind[0:32], P["Ind"], ["Ind"], ["Ind"])
    kmT = K.alloc((8, 32), F32)
    for r in range(4):
        K.DMA("sp", kmT[:, :, r * 8:(r + 1) * 8], km_g[r * 1024:(r + 1) * 1024, :].rearrange("(h d) n -> d h n", h=8),
              ["km_g"], ["kmT"])
    bm_lt = K.alloc((16, 32), F32)
    bm_eq = K.alloc((16, 32), F32)
    bm_pen = K.alloc((16, 32), F32)
    bm_no = K.alloc((16, 32), F32)
    K.DMA("sp", bm_lt, P["bm_lt"], [], ["bm"])
    K.DMA("sp", bm_eq, P["bm_eq"], [], ["bm"])
    K.DMA("sp", bm_pen, P["bm_pen"], [], ["bm"])
    K.DMA("sp", bm_no, P["bm_no"], [], ["bm"])
    kTh = K.alloc((NT * 128,), BF16)
    vh = K.alloc((NT, 128), BF16)
    qf = K.alloc((2048,), F32)
    qb = K.alloc((2048,), BF16)
    BiasT2 = [K.alloc((2048,), BF16) for _ in range(2)]
    BiasG2 = [K.alloc((2048,), BF16) for _ in range(2)]
    for hb_ in range(2):
        K.MEMSET("pool", BiasT2[hb_], 0.0, [("Bias", hb_)])
        K.MEMSET("pool", BiasG2[hb_], 0.0, [("Bias", hb_)])
    NPT = 7
    pT = [K.alloc((512,), BF16) for _ in range(NPT)]
    rec = K.alloc((512,), F32)
    LaccD = K.alloc((512,), F32)
    LaccP = K.alloc((512,), F32)
    onesf = K.alloc((128,), F32)
    K.MEMSET("pool", onesf, 1.0, ["onesf"])
    gtmp = [K.alloc((48,), F32) for _ in range(2)]
    gal = K.alloc((16, 64), F32)
    SB = [0, 1, 2, 3]
    BL, BT, BG = 6, 6, 7
    nsb = 0
    npt = 0

    def gating_front(h):
        K.DMA("sp", qf, qT_l[h], ["qT_l"], ["qf"])
        for s in range(16):
            K.MM(K.ps[BG][:, s * 32:(s + 1) * 32], qf[:, s * 128:(s + 1) * 128], kmT[:, h, :], True, True,
                 ["qf", "kmT"], [("ps", BG)])
        for s in range(16):
            i = s % 2
            g = gtmp[i]
            gk = ("gtmp", i)
            gm, m8 = g[:, 0:32], g[:, 32:40]
            al, al2 = gal[:, s, 0:32], gal[:, s, 32:64]
            ak = ("gal", s)
            K.TT("dve", gm, K.ps[BG][:, s * 32:(s + 1) * 32], bm_lt[:, s, :], ALU.mult, [("ps", BG), "bm"], [gk])
            K.TT("dve", gm, gm, bm_pen[:, s, :], ALU.add, [gk, "bm"], [gk])
            S.op("dve", lambda e, m8=m8, gm=gm: e.max(out=m8, in_=gm), [gk], [gk])
            K.TS("dve", al, gm, m8[:, 2:3], ALU.is_ge, [gk], [ak])
            K.TT("dve", al, al, bm_lt[:, s, :], ALU.mult, [ak, "bm"], [ak])
            K.TT("dve", al, al, bm_eq[:, s, :], ALU.add, [ak, "bm"], [ak])
            K.TT("dve", al2, al, bm_no[:, s, :], ALU.mult, [ak, "bm"], [ak])
            K.TS("dve", al, al, -1.0, ALU.add, [ak], [ak], s2=-NEGB, op1=ALU.mult)
            K.TS("dve", al2, al2, -1.0, ALU.add, [ak], [ak], s2=-NEGB, op1=ALU.mult)

    def gating_back(h):
        hb = h % 2
        for which, dst in ((0, BiasT2[hb]), (1, BiasG2[hb])):
            for s4 in range(4):
                for j in range(4):
                    s = s4 * 4 + j
                    K.TR(K.ps[BT][0:32, j * 128:(j + 1) * 128], gal[:, s, which * 32:(which + 1) * 32], K.identf,
                         [("gal", s), "identf"], [("ps", BT)])
                K.CP("act", dst[0:32, s4 * 512:(s4 + 1) * 512], K.ps[BT][0:32, :], [("ps", BT)], [("Bias", hb)])

    gating_front(0)
    gating_back(0)
    for h in range(8):
        hb = h % 2
        BiasT, BiasG = BiasT2[hb], BiasG2[hb]
        K.DMA("sp", kTh[:, 0:2048], kT_l[h], [("kTo", h)], ["kTh"])
        K.DMA("sp", vh[:, 0:16, :], v_l[h].rearrange("(kt k) d -> k kt d", k=128), [("vo", h)], ["vh"])
        for r in range(3):
            K.DMA("sp", kTh[:, (r + 1) * 2048:(r + 2) * 2048], kT_g[h][r * 128:(r + 1) * 128, :], [("kT_g", h)], ["kTh"])
            K.DMA("sp", vh[:, (r + 1) * 16:(r + 2) * 16, :],
                  v_g[h].rearrange("(kt k) d -> k kt d", k=128)[:, r * 16:(r + 1) * 16, :], [("v_g", h)], ["vh"])
        K.CP("pool", qb, qf, ["qf"], ["qb"])
        for qt in range(4):
            qsl = slice(qt * 512, (qt + 1) * 512)
            bo = 4 + (qt % 2)
            bl = BL

            def s_part(kt):
                nonlocal nsb, npt
                bs = SB[nsb % 4]
                nsb += 1
                diag = (kt // 4 == qt) and kt < 16
                K.MM(K.ps[bs][:], kTh[:, kt * 128:(kt + 1) * 128], qb[:, qsl], True, False, ["kTh", "qb"],
                     [("ps", bs)])
                bias_t = BiasT if kt < 16 else BiasG
                K.MM(K.ps[bs][:], Ind[:, kt // 2, :], bias_t[:, qsl], False, not diag, ["Ind", ("Bias", hb)],
                     [("ps", bs)])
                if diag:
                    K.MM(K.ps[bs][:], K.identb, cmask[:, kt % 4, :], False, True, ["identb", "cmask"], [("ps", bs)])
                p = pT[npt % NPT]
                pk = ("pT", npt % NPT)
                npt += 1
                K.ACT(p, K.ps[bs][:], AF.Exp, [("ps", bs)], [pk])
                return p, pk

            def pv_part(kt, p, pk):
                K.MM(K.ps[bo][:], vh[:, kt, :], p, kt == 0, kt == NT - 1, ["vh", pk], [("ps", bo)])
                q_, acc, ak = ("dve", LaccD, "LaccD") if kt % 2 == 0 else ("pool", LaccP, "LaccP")
                if kt < 2:
                    K.CP(q_, acc, p, [pk], [ak])
                else:
                    K.TT(q_, acc, acc, p, ALU.add, [pk, ak], [ak])

            LOOK = 3
            pend = []
            for kt in range(NT + LOOK):
                if kt < NT:
                    pend.append(s_part(kt))
                if kt >= LOOK:
                    pp, ppk = pend.pop(0)
                    pv_part(kt - LOOK, pp, ppk)
            K.TT("dve", LaccD, LaccD, LaccP, ALU.add, ["LaccD", "LaccP"], ["LaccD"])
            K.MM(K.ps[bl][:], onesf, LaccD, True, True, ["onesf", "LaccD"], [("ps", bl)])
            K.RECIP(rec, K.ps[bl][:], [("ps", bl)], ["rec"])
            K.TT("dve", oT[:, h, qsl], K.ps[bo][:], rec, ALU.mult, [("ps", bo), "rec"], [("oT", h)])
            if h + 1 < 8:
                if qt == 0:
                    gating_front(h + 1)
                if qt == 2:
                    gating_back(h + 1)
    K.S.barrier()
    K.release(mA)
    wo = K.alloc((8, 1024), BF16)
    g1, kg1, mbase, mk = load_mod(K, ada_dram, 0, None)
    wst = K.alloc((8, 1024), F32)
    K.DMA("sp", wst, P["w_o"].rearrange("(h p) n -> p h n", p=128), [], ["wst"])
    for hh in range(8):
        K.TT("dve" if hh % 2 else "pool", wo[:, hh, :], wst[:, hh, :], g1, ALU.mult, ["wst", kg1], ["wo"])
    for s_ in range(16):
        for dh in range(2):
            b = K.bank()
            for hh in range(8):
                K.MM(K.ps[b][:], oT[:, hh, s_ * 128:(s_ + 1) * 128], wo[:, hh, dh * 512:(dh + 1) * 512], hh == 0, hh == 7,
                     [("oT", hh), "wo"], [("ps", b)])
            xs = x[:, s_, dh * 512:(dh + 1) * 512]
            K.TT("dve", xs, K.ps[b][:], xs, ALU.add, [("ps", b), xkey(s_)], [xkey(s_)])
    if dbg:
        for s_ in range(16):
            K.DMA("sp", xatt[s_ * 128:(s_ + 1) * 128, :], x[:, s_, :], [xkey(s_)], ["xatt"])
    K.S.barrier()
    K.release(mO)
    moe_phase(K, x, xkey, ada_dram, P, nexp=nexp)
    mF = K.mark()
    fg = K.alloc((1024,), F32)
    stat = K.alloc((16,), F32)
    junk = K.alloc((1024,), F32)
    ot = [K.alloc((1024,), F32) for _ in range(2)]
    K.DMA("sp", fg, P["final_g"].partition_broadcast(128), [], ["fg"])
    norm_slots(K, x, xkey, stat, junk)
    for s_ in range(16):
        i = s_ % 2
        K.STT(ot[i], x[:, s_, :], stat[:, s_:s_ + 1], fg, ALU.mult, ALU.mult, [xkey(s_), "stat", "fg"], [("ot", i)])
        K.DMA("sp", out[s_ * 128:(s_ + 1) * 128, :], ot[i], [("ot", i)], ["out"])


L0_SHAPES = {
    "xs": (2048, 1024), "selm": (128, 12), "crep": (128, 8, 128),
    "ada_w": (1024, 6144), "ada_b": (6144,), "norm1_g": (1024,), "norm2_g": (1024,),
    "w_in": (1024, 1024), "w_glu": (1024, 1024), "w_out": (1024, 1024),
    "lamP_re": (128, 64), "lamP_im": (128, 64), "logdtP": (128, 64),
    "bP_re": (128, 64, 16), "bP_im": (128, 64, 16), "cP_re": (128, 64, 16), "cP_im": (128, 64, 16),
    "dS": (128, 64),
    "w_router": (1024, 32), "b_router": (32,), "w_gate_up": (32, 1024, 2048), "bguT": (128, 32, 16),
    "w_down": (32, 1024, 1024), "b_down": (32, 1024),
}
L1_SHAPES = {
    "ada_w": (1024, 6144), "ada_b": (6144,), "norm1_g": (1024,), "norm2_g": (1024,), "final_g": (1024,),
    "w_qkv": (1024, 3072), "w_o": (1024, 1024), "invf": (128, 1),
    "bm_lt": (128, 16, 32), "bm_eq": (128, 16, 32), "bm_pen": (128, 16, 32), "bm_no": (128, 16, 32),
    "w_router": (1024, 32), "b_router": (32,), "w_gate_up": (32, 1024, 2048), "bguT": (128, 32, 16),
    "w_down": (32, 1024, 1024), "b_down": (32, 1024),
}
GROUPS = [[0, 1, 2, 3], [4, 5, 6, 7]]


def build_fused(nexp=32):
    K = KB()
    P0 = {k: K.din("a_" + k, v) for k, v in L0_SHAPES.items()}
    P1 = {k: K.din("b_" + k, v) for k, v in L1_SHAPES.items()}
    P1["crep"] = P0["crep"]
    P1["Ind"] = K.din("b_Ind", (32, 40, 128), BF16)
    pos = K.din("b_pos", (2048,), I32)
    out = K.dout("out", (2048, 1024))
    xmid = K.dtmp("xmid", (2048, 1024))
    ada0 = K.dtmp("ada0", (128, 6144))
    ada1 = K.dtmp("ada1", (128, 6144))
    qT_l = K.dtmp("qT_l", (8, 128, 2048))
    kT_l = K.dtmp("kT_l", (8, 128, 2048), BF16)
    v_l = K.dtmp("v_l", (8, 2048, 128), BF16)
    km_l = K.dtmp("km_l", (1024, 8))
    kT_g = K.dtmp("kT_g", (8, 512, 2048), BF16)
    v_g = K.dtmp("v_g", (8, 8192, 128), BF16)
    km_g = K.dtmp("km_g", (4096, 8))
    K.consts()
    st_l = K.dtmp("st_l", (128, 64))
    st_g = K.dtmp("st_g", (512, 64))
    phase_S5(K, P0, xmid, ada0, st_l, st_g, GROUPS)
    x = K.alloc((16, 1024), F32)
    xkey = lambda s: ("x", s)
    for s in range(16):
        K.DMA("sp", x[:, s, :], xmid[s * 128:(s + 1) * 128, :], ["xmid"], [xkey(s)])
    moe_phase(K, x, xkey, ada0, P0, nexp=nexp)
    ada_phase(K, P1["crep"], P1["ada_w"], P1["ada_b"], ada1)
    def gather_kv():
        cl = [(km_l, km_g, "kmo", "km_g")]
        for h in range(8):
            cl.append((kT_l[h], kT_g[h], ("kTo", h), ("kT_g", h)))
            cl.append((v_l[h], v_g[h], ("vo", h), ("v_g", h)))
        for (src, dst, kr, kw) in cl:
            K.S.op("pool", lambda e, src=src, dst=dst: e.collective_compute("AllGather", ALU.bypass, replica_groups=GROUPS,
                                                                             ins=[src], outs=[dst]), [kr], [kw], coll=True)

    phase_L2(K, P1, x, xkey, ada1, pos, qT_l, kT_l, v_l, km_l, after_kv=gather_kv)
    phase_L3(K, P1, x, xkey, ada1, qT_l, kT_l, v_l, kT_g, v_g, km_g, out, nexp=nexp)
    return K.finish()


def prep_common(inp, li, b):
    pre = "l%d_" % li
    c = np.asarray(inp["c"][b], np.float32)
    crep = np.ascontiguousarray(np.broadcast_to(c.reshape(8, 128).T[:, :, None], (128, 8, 128)))
    d = {
        "crep": crep,
        "ada_w": inp[pre + "ada_w"], "ada_b": inp[pre + "ada_b"],
        "norm1_g": inp[pre + "norm1_g"], "norm2_g": inp[pre + "norm2_g"],
        "w_router": inp[pre + "moe_w_router"], "b_router": inp[pre + "moe_b_router"],
        "w_gate_up": inp[pre + "moe_w_gate_up"],
        "bguT": np.ascontiguousarray(np.asarray(inp[pre + "moe_b_gate_up"]).reshape(32, 16, 128).transpose(2, 0, 1)),
        "w_down": inp[pre + "moe_w_down"], "b_down": inp[pre + "moe_b_down"],
    }
    return d


def prep_L1(inp, core):
    b, j = divmod(core, 4)
    x = np.asarray(inp["x"][b])
    selm = np.zeros((128, 12), np.float32)
    for r in range(4):
        mm = j - 1 - r
        if 0 <= mm <= 2:
            selm[:, r * 3 + mm] = 1.0
    d = prep_common(inp, 0, b)
    d["xs"] = x[j * 2048:(j + 1) * 2048]
    d["selm"] = selm
    g = lambda n: np.asarray(inp["l0_s5_" + n], np.float32)
    t2 = lambda a: np.ascontiguousarray(np.concatenate([a, a], axis=0))
    d["w_in"], d["w_glu"], d["w_out"] = g("w_in"), g("w_glu"), g("w_out")
    d["lamP_re"] = t2(g("lam_re").T)
    d["lamP_im"] = t2(g("lam_im").T)
    d["logdtP"] = np.ascontiguousarray(np.broadcast_to(g("log_dt")[None, :], (128, 64)))
    d["bP_re"] = t2(g("b_re").transpose(1, 0, 2))
    d["bP_im"] = t2(g("b_im").transpose(1, 0, 2))
    d["cP_re"] = t2(g("c_re").transpose(2, 0, 1))
    d["cP_im"] = t2(g("c_im").transpose(2, 0, 1))
    d["dS"] = np.ascontiguousarray(np.tile(g("d").reshape(64, 16).T, (8, 1)))
    return {k: np.ascontiguousarray(np.asarray(v, np.float32)) for k, v in d.items()}


def f32c(a):
    return np.ascontiguousarray(np.asarray(a, np.float32))


def prep_L2(inp, core, x0):
    b, j = divmod(core, 4)
    d = prep_common(inp, 1, b)
    inv = (np.float32(10000.0) ** (-np.arange(0, 128, 2, dtype=np.float32) / np.float32(128))).astype(np.float32)
    m = {"x0": f32c(x0), "crep": d["crep"], "ada_w": f32c(d["ada_w"]), "ada_b": f32c(d["ada_b"]),
         "norm1_g": f32c(d["norm1_g"]), "w_qkv": f32c(inp["l1_moba_w_qkv"]),
         "invf": f32c(np.concatenate([inv, inv])[:, None]),
         "pos": np.ascontiguousarray(np.asarray(inp["positions"][b, j * 2048:(j + 1) * 2048], np.int32))}
    return m


def seg_order(j):
    return [j] + [k for k in range(4) if k != j]


def prep_L3(inp, core, x0, l2res):
    import ml_dtypes
    b, j = divmod(core, 4)
    d = prep_common(inp, 1, b)
    order = seg_order(j)
    m = {k: f32c(d[k]) for k in ("crep", "ada_w", "ada_b", "norm2_g", "w_router", "b_router", "w_gate_up", "bguT",
                                 "w_down", "b_down")}
    m["x0"] = f32c(x0)
    m["final_g"] = f32c(inp["final_norm_g"])
    m["w_o"] = f32c(inp["l1_moba_w_o"])
    m["qT"] = l2res[core]["qT"]
    m["kT_all"] = np.ascontiguousarray(np.concatenate([l2res[b * 4 + k]["kT"] for k in order], axis=2))
    m["v_all"] = np.ascontiguousarray(np.concatenate([l2res[b * 4 + k]["v"] for k in order], axis=0))
    km = np.concatenate([l2res[b * 4 + k]["kmT"] for k in range(4)], axis=2)
    m["kmT"] = f32c(km.transpose(1, 0, 2))
    gblk = np.array([order[bp // 8] * 8 + bp % 8 for bp in range(32)])
    ind = (np.arange(32)[:, None] == gblk[None, :]).astype(np.float32)
    m["Ind"] = np.ascontiguousarray(np.broadcast_to(ind[:, :, None], (32, 32, 128))).astype(ml_dtypes.bfloat16)
    jq = 8 * j + np.arange(16) // 2
    n = np.arange(32)
    lt = (n[None, :] < jq[:, None]).astype(np.float32)
    eq = (n[None, :] == jq[:, None]).astype(np.float32)
    bc = lambda a: np.ascontiguousarray(np.broadcast_to(a[None], (128, 16, 32))).astype(np.float32)
    m["bm_lt"], m["bm_eq"], m["bm_pen"] = bc(lt), bc(eq), bc((lt - 1.0) * 1e30)
    return m


def prep_fused(inp, core):
    import ml_dtypes
    b, j = divmod(core, 4)
    m0 = prep_L1(inp, core)
    m = {"a_" + k: v for k, v in m0.items()}
    d = prep_common(inp, 1, b)
    inv = (np.float32(10000.0) ** (-np.arange(0, 128, 2, dtype=np.float32) / np.float32(128))).astype(np.float32)
    m1 = {k: f32c(d[k]) for k in ("ada_w", "ada_b", "norm1_g", "norm2_g", "w_router", "b_router", "w_gate_up", "bguT",
                                  "w_down", "b_down")}
    m1["final_g"] = f32c(inp["final_norm_g"])
    m1["w_qkv"] = f32c(inp["l1_moba_w_qkv"])
    m1["w_o"] = f32c(inp["l1_moba_w_o"])
    m1["invf"] = f32c(np.concatenate([inv, inv])[:, None])
    jq = 8 * j + np.arange(16) // 2
    n = np.arange(32)
    lt = (n[None, :] < jq[:, None]).astype(np.float32)
    eq = (n[None, :] == jq[:, None]).astype(np.float32)
    notown = np.broadcast_to(((n // 8) != j).astype(np.float32)[None, :], (16, 32))
    bc = lambda a: np.ascontiguousarray(np.broadcast_to(a[None], (128, 16, 32))).astype(np.float32)
    m1["bm_lt"], m1["bm_eq"], m1["bm_pen"], m1["bm_no"] = bc(lt), bc(eq), bc((lt - 1.0) * 1e30), bc(notown)
    for k, v in m1.items():
        m["b_" + k] = v
    gb = np.concatenate([8 * j + np.arange(8), np.arange(32)])
    ind = (np.arange(32)[:, None] == gb[None, :]).astype(np.float32)
    m["b_Ind"] = np.ascontiguousarray(np.broadcast_to(ind[:, :, None], (32, 40, 128))).astype(ml_dtypes.bfloat16)
    m["b_pos"] = np.ascontiguousarray(np.asarray(inp["positions"][b, j * 2048:(j + 1) * 2048], np.int32))
    return m


_INPUT_NAMES = (
    "x", "c", "positions", "l0_norm1_g",
    "l0_ada_w", "l0_ada_b", "l0_s5_w_in", "l0_s5_b_re",
    "l0_s5_b_im", "l0_s5_c_re", "l0_s5_c_im", "l0_s5_lam_re",
    "l0_s5_lam_im", "l0_s5_log_dt", "l0_s5_d", "l0_s5_w_glu",
    "l0_s5_w_out", "l0_norm2_g", "l0_moe_w_router", "l0_moe_b_router",
    "l0_moe_w_gate_up", "l0_moe_b_gate_up", "l0_moe_w_down", "l0_moe_b_down",
    "l1_norm1_g", "l1_ada_w", "l1_ada_b", "l1_moba_w_qkv",
    "l1_moba_w_o", "l1_norm2_g", "l1_moe_w_router", "l1_moe_b_router",
    "l1_moe_w_gate_up", "l1_moe_b_gate_up", "l1_moe_w_down", "l1_moe_b_down",
    "final_norm_g",
)


def kernel(**inputs):
    inp = {k: np.asarray(inputs[k]) for k in _INPUT_NAMES}
    cores = list(range(8))
    nc = build_fused()
    res = run_bass_kernel_spmd(nc, [prep_fused(inp, c) for c in cores], core_ids=cores).results
    out = np.zeros((2, 8192, 1024), np.float32)
    for c in cores:
        b, j = divmod(c, 4)
        out[b, j * 2048:(j + 1) * 2048] = np.asarray(res[c]["out"])
    return out
```

```python
import math
import numpy as np
from contextlib import ExitStack
import concourse.bass as bass
import concourse.mybir as mybir
from concourse.bass_utils import run_bass_kernel_spmd

F32 = mybir.dt.float32
BF16 = mybir.dt.bfloat16
I32 = mybir.dt.int32
AF = mybir.ActivationFunctionType
ALU = mybir.AluOpType
AX = mybir.AxisListType

QUEUES = ("pe", "act", "dve", "pool", "sp")
DMA_SLOTS = 8
SAME_Q_SYNC = True


class _Op:
    __slots__ = ("q", "fn", "dma", "deps", "sig", "cnt", "slot", "slotcnt", "inc", "semq")


class Sched:
    def __init__(self):
        self.ops = []
        self.lastw = {}
        self.readers = {}
        self.ndma = {q: 0 for q in QUEUES}
        self.ncoll = 0

    def op(self, q, fn, reads=(), writes=(), dma=False, coll=False):
        o = _Op()
        if coll:
            dma = True
        o.q, o.fn, o.dma, o.sig, o.cnt = q, fn, dma, False, 0
        o.inc, o.semq = 16, q
        deps = set()
        raw = set()
        for k in reads:
            w = self.lastw.get(k)
            if w is not None:
                deps.add(w)
                raw.add(w)
        for k in writes:
            w = self.lastw.get(k)
            if w is not None:
                deps.add(w)
            for r in self.readers.get(k, ()):
                deps.add(r)
        o.deps = []
        for d in deps:
            if d.q == q and not d.dma:
                if not (SAME_Q_SYNC and d in raw and q != "pe"):
                    continue
            d.sig = True
            o.deps.append(d)
        for k in reads:
            self.readers.setdefault(k, []).append(o)
        for k in writes:
            self.lastw[k] = o
            self.readers[k] = []
        if coll:
            o.inc, o.semq = 1, "cc"
            o.slot = self.ncoll
            o.slotcnt = 1
            self.ncoll += 1
        elif dma:
            o.slot = self.ndma[q] % DMA_SLOTS
            o.slotcnt = self.ndma[q] // DMA_SLOTS + 1
            self.ndma[q] += 1
        self.ops.append(o)
        return o

    def barrier(self):
        last = {}
        for o in self.ops:
            if o.fn is not None and not o.dma:
                last[o.q] = o
        dmas = []
        for q in QUEUES:
            dq = [o for o in self.ops if o.dma and o.q == q and o.semq == q]
            dmas += dq[-DMA_SLOTS:]
        dmas += [o for o in self.ops if o.dma and o.semq == "cc"]
        for q in QUEUES:
            o = self.op(q, None)
            for d in list(last.values()) + dmas:
                if d.q == q and not d.dma:
                    continue
                d.sig = True
                o.deps.append(d)
        self.lastw.clear()
        self.readers.clear()

    def emit(self, sems, dsems, block):
        cnt = {q: 0 for q in QUEUES}
        for o in self.ops:
            if o.dma:
                continue
            if o.sig and o.fn is not None:
                cnt[o.q] += 1
            o.cnt = cnt[o.q]
        byq = {q: [o for o in self.ops if o.q == q] for q in QUEUES}
        engs = {"pe": "tensor", "act": "scalar", "dve": "vector", "pool": "gpsimd", "sp": "sync"}

        def make(q):
            def body(e):
                waited = {}
                for o in byq[q]:
                    need = {}
                    for d in o.deps:
                        if d.dma:
                            key = ("d", d.semq, d.slot)
                            v = d.inc * d.slotcnt
                        else:
                            key = ("e", d.q)
                            v = d.cnt
                        if need.get(key, 0) < v:
                            need[key] = v
                    if o.dma and o.slotcnt > 1 and o.semq == q:
                        key = ("d", q, o.slot)
                        v = 16 * (o.slotcnt - 1)
                        if need.get(key, 0) < v:
                            need[key] = v
                    for key, v in need.items():
                        if waited.get(key, 0) >= v:
                            continue
                        waited[key] = v
                        if key[0] == "d":
                            e.wait_ge(dsems[key[1]][key[2]], v)
                        else:
                            e.wait_ge(sems[key[1]], v)
                    if o.fn is None:
                        continue
                    ins = o.fn(e)
                    if o.dma:
                        ins.then_inc(dsems[o.semq][o.slot], o.inc)
                    elif o.sig:
                        ins.then_inc(sems[q], 1)
                n = self.ndma[q]
                for s in range(min(n, DMA_SLOTS)):
                    c = (n - 1 - s) // DMA_SLOTS + 1
                    e.wait_ge(dsems[q][s], 16 * c)
            return body

        for q in QUEUES:
            getattr(block, engs[q])(make(q))


def _prod(s):
    r = 1
    for v in s:
        r *= v
    return r


ARENA_WORDS = 52224


class KB:
    def __init__(self):
        self.nc = bass.Bass("TRN2", target_bir_lowering=False)
        self.es = ExitStack()
        self.S = Sched()
        nc = self.nc
        self.arena = self.es.enter_context(nc.sbuf_tensor("arena", [128, ARENA_WORDS], F32))
        self.off = 0
        self.ps = [self.es.enter_context(nc.psum_tensor("ps%d" % i, [128, 512], F32)) for i in range(8)]
        self.sems = {q: self.es.enter_context(nc.semaphore("s_" + q)) for q in QUEUES}
        self.dsems = {q: [self.es.enter_context(nc.semaphore("d_%s%d" % (q, i))) for i in range(DMA_SLOTS)]
                      for q in QUEUES}
        self.dsems["cc"] = [self.es.enter_context(nc.semaphore("cc%d" % i)) for i in range(20)]
        self.nbank = 0
        self.uid = 0

    def alloc(self, free_shape, dt):
        n = _prod(free_shape)
        esz = 4 if dt in (F32, I32) else 2
        words = (n * esz + 3) // 4
        words = (words + 7) // 8 * 8
        assert self.off + words <= ARENA_WORDS, ("arena overflow", self.off, words)
        a = self.arena[:, self.off:self.off + words]
        self.off += words
        if esz == 2:
            a = a.bitcast(dt)
        elif dt != F32:
            a = a.bitcast(dt)
        a = a[:, 0:n]
        if len(free_shape) > 1:
            names = ["a%d" % i for i in range(len(free_shape))]
            kw = {nm: v for nm, v in zip(names, free_shape)}
            a = a.rearrange("p (%s) -> p %s" % (" ".join(names), " ".join(names)), **kw)
        return a

    def mark(self):
        return self.off

    def release(self, m):
        self.off = m

    def bank(self):
        i = self.nbank % 8
        self.nbank += 1
        return i

    def din(self, name, shape, dt=F32):
        return self.nc.dram_tensor(name, list(shape), dt, kind="ExternalInput").ap()

    def dout(self, name, shape, dt=F32):
        return self.nc.dram_tensor(name, list(shape), dt, kind="ExternalOutput").ap()

    def dtmp(self, name, shape, dt=F32):
        return self.nc.dram_tensor(name, list(shape), dt, kind="Internal").ap()

    def finish(self):
        with self.nc.Block() as block:
            self.S.emit(self.sems, self.dsems, block)
        self.es.close()
        return self.nc

    def MM(self, out, lhsT, rhs, st, sp, r, w):
        self.S.op("pe", lambda e: e.matmul(out, lhsT=lhsT, rhs=rhs, start=st, stop=sp), r, w)

    def TR(self, out, in_, idn, r, w):
        self.S.op("pe", lambda e: e.transpose(out=out, in_=in_, identity=idn), r, w)

    def ACT(self, out, in_, func, r, w, **kw):
        self.S.op("act", lambda e: e.activation(out=out, in_=in_, func=func, **kw), r, w)

    def TT(self, q, out, a, b, op, r, w):
        self.S.op(q, lambda e: e.tensor_tensor(out=out, in0=a, in1=b, op=op), r, w)

    def TS(self, q, out, a, s1, op0, r, w, s2=None, op1=None, accum=None):
        if op1 is None:
            self.S.op(q, lambda e: e.tensor_scalar(out=out, in0=a, scalar1=s1, scalar2=None, op0=op0), r, w)
        elif accum is None:
            self.S.op(q, lambda e: e.tensor_scalar(out=out, in0=a, scalar1=s1, scalar2=s2, op0=op0, op1=op1), r, w)
        else:
            self.S.op(q, lambda e: e.tensor_scalar(out=out, in0=a, scalar1=s1, scalar2=s2, op0=op0, op1=op1,
                                                   accum_out=accum), r, w)

    def STT(self, out, a, sc, b, op0, op1, r, w, accum=None):
        if accum is None:
            self.S.op("dve", lambda e: e.scalar_tensor_tensor(out=out, in0=a, scalar=sc, in1=b, op0=op0, op1=op1), r, w)
        else:
            self.S.op("dve", lambda e: e.scalar_tensor_tensor(out=out, in0=a, scalar=sc, in1=b, op0=op0, op1=op1,
                                                              accum_out=accum), r, w)

    def CP(self, q, out, in_, r, w):
        if q == "act":
            self.S.op(q, lambda e: e.copy(out=out, in_=in_), r, w)
        else:
            self.S.op(q, lambda e: e.tensor_copy(out=out, in_=in_), r, w)

    def DMA(self, q, out, in_, r, w):
        self.S.op(q, lambda e: e.dma_start(out=out, in_=in_), r, w, dma=True)

    def MEMSET(self, q, out, val, w):
        self.S.op(q, lambda e: e.memset(out, val), (), w)

    def RECIP(self, out, in_, r, w):
        self.S.op("dve", lambda e: e.reciprocal(out=out, in_=in_), r, w)

    def key(self, base):
        self.uid += 1
        return (base, self.uid)

    def consts(self):
        self.identf = self.alloc((128,), F32)
        self.identb = self.alloc((128,), BF16)
        self.eps = self.alloc((1,), F32)
        self.MEMSET("pool", self.identf, 0.0, ["identf"])
        idf = self.identf
        self.S.op("pool", lambda e: e.affine_select(out=idf, in_=idf, pattern=[[1, 128]], compare_op=ALU.not_equal,
                                                    fill=1.0, base=0, channel_multiplier=-1), ["identf"], ["identf"])
        self.CP("dve", self.identb, self.identf, ["identf"], ["identb"])
        self.MEMSET("pool", self.eps, 1e-6, ["eps"])


def ada_phase(K, crep, ada_w, ada_b, ada_dram):
    m = K.mark()
    ada = K.alloc((6144,), F32)
    csil = K.alloc((8, 128), F32)
    K.DMA("sp", csil, crep, [], ["csil"])
    K.ACT(csil, csil, AF.Silu, ["csil"], ["csil"])
    wch = [K.alloc((8, 512), F32) for _ in range(2)]
    wv = ada_w.rearrange("(kc p) n -> p kc n", p=128)
    for j in range(12):
        sl = slice(j * 512, (j + 1) * 512)
        K.DMA("sp", ada[:, sl], ada_b[sl].partition_broadcast(128), [], [("ada", j)])
    for j in range(12):
        wb = wch[j % 2]
        wk = ("adaw", j % 2)
        K.DMA("sp", wb, wv[:, :, j * 512:(j + 1) * 512], [], [wk])
        b = K.bank()
        for kc in range(8):
            K.MM(K.ps[b][:], csil[:, kc, :], wb[:, kc, :], kc == 0, kc == 7, ["csil", wk], [("ps", b)])
        sl = slice(j * 512, (j + 1) * 512)
        K.TT("dve", ada[:, sl], K.ps[b][:], ada[:, sl], ALU.add, [("ps", b), ("ada", j)], [("ada", j)])
    K.DMA("sp", ada_dram, ada, [("ada", j) for j in range(12)], ["ada_dram"])
    K.S.barrier()
    K.release(m)


def load_mod(K, ada_dram, which, norm_g, want_gate=True):
    base = 0 if which == 0 else 3
    k = K.key("mod")
    gt = None
    if want_gate:
        gt = K.alloc((1024,), F32)
        K.DMA("sp", gt, ada_dram[:, (base + 2) * 1024:(base + 3) * 1024], ["ada_dram"], [(k, "g")])
    return gt, (k, "g"), base, k


def load_mod2(K, ada_dram, base, k, norm_g):
    sh = K.alloc((1024,), F32)
    gs = K.alloc((1024,), F32)
    tmp = K.alloc((1024,), F32)
    K.DMA("sp", sh, ada_dram[:, (base + 0) * 1024:(base + 1) * 1024], ["ada_dram"], [(k, "sh")])
    K.DMA("sp", tmp, ada_dram[:, (base + 1) * 1024:(base + 2) * 1024], ["ada_dram"], [(k, "sc")])
    K.DMA("sp", gs, norm_g.partition_broadcast(128), [], [(k, "gs")])
    K.STT(gs, tmp, 1.0, gs, ALU.add, ALU.mult, [(k, "sc"), (k, "gs")], [(k, "gs")])
    return gs, sh, (k, "gs"), (k, "sh")


def moe_phase(K, x, xkey, ada_dram, P, nexp=32, dbg_gates=None):
    S = K.S
    m0 = K.mark()
    g2, kg2, mbase, mk = load_mod(K, ada_dram, 1, P["norm2_g"])
    h2T = K.alloc((8, 2048), BF16)
    gates = K.alloc((16, 32), F32)
    wtsT = K.alloc((2048,), BF16)
    bgu = K.alloc((32, 16), F32)
    bdb = K.alloc((1024,), BF16)
    K.DMA("sp", bgu, P["bguT"], [], ["bgu"])
    m1 = K.mark()
    gs2, sh2, kgs, ksh = load_mod2(K, ada_dram, mbase, mk, P["norm2_g"])
    wr = K.alloc((8, 32), F32)
    brt = K.alloc((32,), F32)
    bdf = K.alloc((1024,), F32)
    stat = K.alloc((16,), F32)
    junk = K.alloc((1024,), F32)
    K.DMA("sp", wr, P["w_router"].rearrange("(kc p) e -> p kc e", p=128), [], ["wr"])
    K.DMA("sp", brt, P["b_router"].partition_broadcast(128), [], ["brt"])
    K.DMA("sp", bdf[0:32, :], P["b_down"], [], ["bdf"])
    K.TT("dve", bdb[0:32, :], bdf[0:32, :], g2[0:32, :], ALU.mult, ["bdf", kg2], ["bdb"])
    for s in range(16):
        K.ACT(junk, x[:, s, :], AF.Square, [xkey(s)], ["junk", "stat"], accum_out=stat[:, s:s + 1])
    K.ACT(stat, stat, AF.Sqrt, ["stat"], ["stat"], scale=1.0 / 1024, bias=K.eps[:, 0:1])
    K.RECIP(stat, stat, ["stat"], ["stat"])
    tmpf = [K.alloc((1024,), F32) for _ in range(2)]
    h2f = [K.alloc((1024,), F32) for _ in range(2)]
    hTs = [K.alloc((8, 128), F32) for _ in range(2)]
    gt = [K.alloc((160,), F32) for _ in range(2)]
    for s in range(16):
        i = s % 2
        K.STT(tmpf[i], x[:, s, :], stat[:, s:s + 1], gs2, ALU.mult, ALU.mult, [xkey(s), "stat", kgs], [("tmpf", i)])
        K.TT("pool", h2f[i], tmpf[i], sh2, ALU.add, [("tmpf", i), ksh], [("h2f", i)])
        b0 = K.bank()
        b1 = K.bank()
        for kc in range(8):
            b = b0 if kc < 4 else b1
            K.TR(K.ps[b][:, (kc % 4) * 128:(kc % 4 + 1) * 128], h2f[i][:, kc * 128:(kc + 1) * 128], K.identf,
                 [("h2f", i), "identf"], [("ps", b)])
        for hh, b in enumerate((b0, b1)):
            src = K.ps[b][:].rearrange("p (a n) -> p a n", a=4)
            K.CP("dve", hTs[i][:, hh * 4:(hh + 1) * 4, :], src, [("ps", b)], [("hTs", i)])
        K.CP("act", h2T[:, :, s * 128:(s + 1) * 128], hTs[i], [("hTs", i)], [("h2T", s)])
        bl = K.bank()
        for kc in range(8):
            K.MM(K.ps[bl][:, 0:32], hTs[i][:, kc, :], wr[:, kc, :], kc == 0, kc == 7, [("hTs", i), "wr"], [("ps", bl)])
        g = gt[i]
        gk = ("gt", i)
        lg, m8, ex, em = g[:, 0:32], g[:, 32:40], g[:, 64:96], g[:, 96:128]
        negm, ssum, mask = g[:, 40:41], g[:, 41:42], g[:, 128:160]
        K.TT("dve", lg, K.ps[bl][:, 0:32], brt, ALU.add, [("ps", bl), "brt"], [gk])
        S.op("dve", lambda e, m8=m8, lg=lg: e.max(out=m8, in_=lg), [gk], [gk])
        K.TS("dve", negm, m8[:, 0:1], -1.0, ALU.mult, [gk], [gk])
        K.TS("dve", mask, lg, m8[:, 3:4], ALU.is_ge, [gk], [gk])
        K.ACT(ex, lg, AF.Exp, [gk], [gk], bias=negm, scale=1.0)
        K.STT(em, ex, 1.0, mask, ALU.mult, ALU.mult, [gk], [gk], accum=ssum)
        K.RECIP(ssum, ssum, [gk], [gk])
        K.TS("dve", gates[:, s, :], em, ssum, ALU.mult, [gk], [("gates", s)])
    for q4 in range(4):
        b = K.bank()
        for j in range(4):
            s = q4 * 4 + j
            K.TR(K.ps[b][0:32, j * 128:(j + 1) * 128], gates[:, s, :], K.identf, [("gates", s), "identf"], [("ps", b)])
        K.CP("act", wtsT[0:32, q4 * 512:(q4 + 1) * 512], K.ps[b][0:32, :], [("ps", b)], ["wtsT"])
    if dbg_gates is not None:
        K.DMA("sp", dbg_gates, gates, [("gates", s) for s in range(16)], ["dbg_gates"])
    K.S.barrier()
    K.release(m1)
    actT = K.alloc((8, 2048), BF16)
    NW = 2
    wgu_t = [K.alloc((8, 2, 256), BF16) for _ in range(NW)]
    wdn_t = [K.alloc((8, 512), BF16) for _ in range(2)]
    gsb = [K.alloc((512,), F32) for _ in range(2)]
    ssb = [K.alloc((512,), BF16) for _ in range(2)]
    usb = [K.alloc((512,), F32) for _ in range(2)]
    tdn = [K.alloc((512,), F32) for _ in range(2)]
    wguv = P["w_gate_up"]
    wdv = P["w_down"]
    chunks = []
    for e in range(nexp):
        for q in range(4):
            chunks.append(("gu", e, q))
        for dh in range(2):
            chunks.append(("dn", e, dh))
    cnt = {"gu": 0, "dn": 0}
    slot_of = {}

    def issue(ci):
        kind, e, q = chunks[ci]
        i = cnt[kind]
        cnt[kind] += 1
        if kind == "gu":
            wb = wgu_t[i % NW]
            wk = ("wgu", i % NW)
            srcv = wguv[e].rearrange("(kc p) f -> p kc f", p=128)
            K.DMA("pool", wb[:, :, 0, :], srcv[:, :, q * 256:(q + 1) * 256], [], [wk])
            K.DMA("pool", wb[:, :, 1, :], srcv[:, :, 1024 + q * 256:1024 + (q + 1) * 256], [], [wk])
            slot_of[ci] = (wb, wk)
            return
        if True:
            wb = wdn_t[i % 2]
            wk = ("wdn", i % 2)
            src = wdv[e].rearrange("(fc p) d -> p fc d", p=128)[:, :, q * 512:(q + 1) * 512]
        K.DMA("pool", wb, src, [], [wk])
        slot_of[ci] = (wb, wk)

    if chunks:
        issue(0)
    ep = 0
    dcnt = 0
    for ci, (kind, e, q) in enumerate(chunks):
        if ci + 1 < len(chunks):
            issue(ci + 1)
        wb, wk = slot_of.pop(ci)
        if kind == "gu":
            for ft in range(2):
                fcol = q * 2 + ft
                for tt in range(4):
                    bg = K.bank()
                    bu = K.bank()
                    rk = [wk] + [("h2T", s) for s in range(tt * 4, tt * 4 + 4)]
                    for kc in range(8):
                        K.MM(K.ps[bg][:], wb[:, kc, 0, ft * 128:(ft + 1) * 128], h2T[:, kc, tt * 512:(tt + 1) * 512],
                             kc == 0, kc == 7, rk, [("ps", bg)])
                    for kc in range(8):
                        K.MM(K.ps[bu][:], wb[:, kc, 1, ft * 128:(ft + 1) * 128], h2T[:, kc, tt * 512:(tt + 1) * 512],
                             kc == 0, kc == 7, rk, [("ps", bu)])
                    i = ep % 2
                    ep += 1
                    K.TS("dve", gsb[i], K.ps[bg][:], bgu[:, e, fcol:fcol + 1], ALU.add, [("ps", bg), "bgu"],
                         [("gsb", i)], s2=7.0, op1=ALU.min)
                    K.ACT(ssb[i], gsb[i], AF.Sigmoid, [("gsb", i)], [("ssb", i)], scale=1.702)
                    K.TS("dve", usb[i], K.ps[bu][:], bgu[:, e, 8 + fcol:9 + fcol], ALU.add, [("ps", bu), "bgu"],
                         [("usb", i)], s2=7.0, op1=ALU.min)
                    K.TS("dve", usb[i], usb[i], -7.0, ALU.max, [("usb", i)], [("usb", i)], s2=1.0, op1=ALU.add)
                    K.TT("pool", gsb[i], gsb[i], ssb[i], ALU.mult, [("gsb", i), ("ssb", i)], [("gsb", i)])
                    K.TT("pool", actT[:, fcol, tt * 512:(tt + 1) * 512], usb[i], gsb[i], ALU.mult,
                         [("usb", i), ("gsb", i)], [("actT", fcol, tt)])
        else:
            dh = q
            for ts in range(16):
                b = K.bank()
                for fc in range(8):
                    K.MM(K.ps[b][:], actT[:, fc, ts * 128:(ts + 1) * 128], wb[:, fc, :], fc == 0, fc == 7,
                         [("actT", fc, ts // 4), wk], [("ps", b)])
                i = dcnt % 2
                dcnt += 1
                K.STT(tdn[i], K.ps[b][:], gates[:, ts, e:e + 1], g2[:, dh * 512:(dh + 1) * 512], ALU.mult, ALU.mult,
                      [("ps", b), ("gates", ts), kg2], [("tdn", i)])
                xs = x[:, ts, dh * 512:(dh + 1) * 512]
                K.TT("pool", xs, xs, tdn[i], ALU.add, [("tdn", i), xkey(ts)], [xkey(ts)])
    for ts in range(16):
        for dh in range(2):
            b = K.bank()
            K.MM(K.ps[b][:], wtsT[0:32, ts * 128:(ts + 1) * 128], bdb[0:32, dh * 512:(dh + 1) * 512], True, True,
                 ["wtsT", "bdb"], [("ps", b)])
            xs = x[:, ts, dh * 512:(dh + 1) * 512]
            K.TT("dve", xs, K.ps[b][:], xs, ALU.add, [("ps", b), xkey(ts)], [xkey(ts)])
    K.S.barrier()
    K.release(m0)


TWO_PI = 2.0 * math.pi
CW1 = 6.28125
CW2 = TWO_PI - 6.28125


def sincos(K, theta, sin_o, cos_o, shape, kin, kout):
    tf = K.alloc(shape, F32)
    ti = K.alloc(shape, I32)
    kf = K.alloc(shape, F32)
    r = K.alloc(shape, F32)
    y = K.alloc(shape, F32)
    mk = K.alloc(shape, F32)
    k = K.key("sc")
    K.TS("dve", tf, theta, 1.0 / TWO_PI, ALU.mult, [kin], [(k, 0)], s2=0.5, op1=ALU.add)
    K.CP("dve", ti, tf, [(k, 0)], [(k, 1)])
    K.CP("dve", kf, ti, [(k, 1)], [(k, 2)])
    K.STT(r, kf, -CW1, theta, ALU.mult, ALU.add, [(k, 2), kin], [(k, 3)])
    K.STT(r, kf, -CW2, r, ALU.mult, ALU.add, [(k, 2), (k, 3)], [(k, 3)])
    for shift, outp in ((0.0, sin_o), (math.pi / 2, cos_o)):
        K.TS("dve", y, r, shift, ALU.add, [(k, 3)], [(k, 4)])
        K.TS("dve", mk, y, math.pi, ALU.is_gt, [(k, 4)], [(k, 5)])
        K.STT(y, mk, -TWO_PI, y, ALU.mult, ALU.add, [(k, 5), (k, 4)], [(k, 4)])
        K.TS("dve", mk, y, -math.pi, ALU.is_lt, [(k, 4)], [(k, 5)])
        K.STT(y, mk, TWO_PI, y, ALU.mult, ALU.add, [(k, 5), (k, 4)], [(k, 4)])
        K.ACT(outp, y, AF.Sin, [(k, 4)], [kout])


def bc_last(ap, n):
    return bass.AP(tensor=ap.tensor, offset=ap.offset, ap=[list(d) for d in ap.ap] + [[0, n]])


def s5_tables(K, P, Cs, Mt, Bs, C1, C2):
    m = K.mark()
    kk = "s5t"
    lam_r = K.alloc((64,), F32)
    lam_i = K.alloc((64,), F32)
    ldt = K.alloc((64,), F32)
    bPr = K.alloc((64, 16), F32)
    bPi = K.alloc((64, 16), F32)
    cPr = K.alloc((64, 16), F32)
    cPi = K.alloc((64, 16), F32)
    dS = K.alloc((64,), F32)
    for t, nm in ((lam_r, "lamP_re"), (lam_i, "lamP_im"), (ldt, "logdtP"), (bPr, "bP_re"), (bPi, "bP_im"),
                  (cPr, "cP_re"), (cPi, "cP_im"), (dS, "dS")):
        K.DMA("sp", t, P[nm], [], [kk])
    dt = K.alloc((64,), F32)
    lr = K.alloc((64,), F32)
    li = K.alloc((64,), F32)
    mag = K.alloc((64,), F32)
    imag = K.alloc((64,), F32)
    sn = K.alloc((64,), F32)
    cs = K.alloc((64,), F32)
    t1 = K.alloc((64,), F32)
    t2 = K.alloc((64,), F32)
    K.ACT(dt, ldt, AF.Exp, [kk], [kk])
    K.TT("dve", lr, lam_r, dt, ALU.mult, [kk], [kk])
    K.TT("dve", li, lam_i, dt, ALU.mult, [kk], [kk])
    for outp, sg in ((mag, 1.0), (imag, -1.0)):
        K.TS("dve", outp, lr, sg / 6.0, ALU.mult, [kk], [kk], s2=1.0, op1=ALU.add)
        for dnm in (5.0, 4.0, 3.0, 2.0, 1.0):
            K.TT("dve", outp, outp, lr, ALU.mult, [kk], [kk])
            K.TS("dve", outp, outp, sg / dnm, ALU.mult, [kk], [kk], s2=1.0, op1=ALU.add)
    sincos(K, li, sn, cs, (64,), kk, kk)
    apr = K.alloc((9, 64), F32)
    api = K.alloc((9, 64), F32)
    ivr = K.alloc((8, 64), F32)
    ivi = K.alloc((8, 64), F32)
    K.MEMSET("dve", apr[:, 0, :], 1.0, [kk])
    K.MEMSET("dve", api[:, 0, :], 0.0, [kk])
    K.MEMSET("dve", ivr[:, 0, :], 1.0, [kk])
    K.MEMSET("dve", ivi[:, 0, :], 0.0, [kk])
    K.TT("dve", apr[:, 1, :], mag, cs, ALU.mult, [kk], [kk])
    K.TT("dve", api[:, 1, :], mag, sn, ALU.mult, [kk], [kk])
    K.TT("dve", ivr[:, 1, :], imag, cs, ALU.mult, [kk], [kk])
    K.TT("dve", t1, imag, sn, ALU.mult, [kk], [kk])
    K.TS("dve", ivi[:, 1, :], t1, -1.0, ALU.mult, [kk], [kk])

    def cmul(orr, oi, ar, ai, br, bi, ta, tb, q="dve"):
        K.TT(q, ta, ar, br, ALU.mult, [kk], [kk])
        K.TT(q, tb, ai, bi, ALU.mult, [kk], [kk])
        K.TT(q, orr, ta, tb, ALU.subtract, [kk], [kk])
        K.TT(q, ta, ar, bi, ALU.mult, [kk], [kk])
        K.TT(q, tb, ai, br, ALU.mult, [kk], [kk])
        K.TT(q, oi, ta, tb, ALU.add, [kk], [kk])

    for t in range(2, 9):
        cmul(apr[:, t, :], api[:, t, :], apr[:, t - 1, :], api[:, t - 1, :], apr[:, 1, :], api[:, 1, :], t1, t2)
    for t in range(2, 8):
        cmul(ivr[:, t, :], ivi[:, t, :], ivr[:, t - 1, :], ivi[:, t - 1, :], ivr[:, 1, :], ivi[:, 1, :], t1, t2)
    for h in range(2):
        rows = slice(64 * h, 64 * h + 64)
        gsl = slice(32 * h, 32 * h + 32)
        K.CP("dve", C1[rows, 0, :], apr[rows, 8, gsl], [kk], [kk])
        K.CP("dve", C1[rows, 1, :], apr[rows, 8, gsl], [kk], [kk])
        K.TS("dve", C2[rows, 0, :], api[rows, 8, gsl], -1.0, ALU.mult, [kk], [kk])
        K.CP("dve", C2[rows, 1, :], api[rows, 8, gsl], [kk], [kk])
    den = K.alloc((64,), F32)
    fr = K.alloc((64,), F32)
    fi = K.alloc((64,), F32)
    am1 = K.alloc((64,), F32)
    K.TT("dve", den, lam_r, lam_r, ALU.mult, [kk], [kk])
    K.TT("dve", t1, lam_i, lam_i, ALU.mult, [kk], [kk])
    K.TT("dve", den, den, t1, ALU.add, [kk], [kk])
    K.RECIP(den, den, [kk], [kk])
    K.TS("dve", am1, apr[:, 1, :], -1.0, ALU.add, [kk], [kk])
    K.TT("dve", t1, am1, lam_r, ALU.mult, [kk], [kk])
    K.TT("dve", t2, api[:, 1, :], lam_i, ALU.mult, [kk], [kk])
    K.TT("dve", t1, t1, t2, ALU.add, [kk], [kk])
    K.TT("dve", fr, t1, den, ALU.mult, [kk], [kk])
    K.TT("dve", t1, api[:, 1, :], lam_r, ALU.mult, [kk], [kk])
    K.TT("dve", t2, am1, lam_i, ALU.mult, [kk], [kk])
    K.TT("dve", t1, t1, t2, ALU.subtract, [kk], [kk])
    K.TT("dve", fi, t1, den, ALU.mult, [kk], [kk])
    bbr = K.alloc((64, 16), F32)
    bbi = K.alloc((64, 16), F32)
    u1 = K.alloc((64, 16), F32)
    u2 = K.alloc((64, 16), F32)
    frb, fib = bc_last(fr, 16), bc_last(fi, 16)
    cmul(bbr, bbi, frb, fib, bPr, bPi, u1, u2)
    for h in range(2):
        rows = slice(64 * h, 64 * h + 64)
        gsl = slice(32 * h, 32 * h + 32)
        for tau in range(8):
            pr = bc_last(apr[rows, tau + 1, gsl], 16)
            pi = bc_last(api[rows, tau + 1, gsl], 16)
            a1 = u1[rows, 0:32, :]
            a2 = u2[rows, 0:32, :]
            K.TT("dve", a1, cPr[rows, gsl, :], pr, ALU.mult, [kk], [kk])
            K.TT("dve", a2, cPi[rows, gsl, :], pi, ALU.mult, [kk], [kk])
            K.TT("dve", Cs[rows, 0, :, tau, :], a1, a2, ALU.subtract, [kk], ["Cs"])
            K.TT("dve", a1, cPr[rows, gsl, :], pi, ALU.mult, [kk], [kk])
            K.TT("dve", a2, cPi[rows, gsl, :], pr, ALU.mult, [kk], [kk])
            K.STT(Cs[rows, 1, :, tau, :], a1, -1.0, a2, ALU.mult, ALU.subtract, [kk], ["Cs"])
    Kk2f = K.alloc((64, 128), F32)
    Q2f = K.alloc((64, 128), F32)
    BsPf = K.alloc((64, 128), F32)
    Kk2 = Kk2f.rearrange("p g (s c) -> p g s c", s=8)
    Q2 = Q2f.rearrange("p g (s c) -> p g s c", s=8)
    BsP = BsPf.rearrange("p g (s c) -> p g s c", s=8)
    lo = slice(0, 64)
    hi = slice(64, 128)
    for s in range(8):
        for (dst, pw_r, pw_i) in ((Kk2, ivr[:, s, :], ivi[:, s, :]), (BsP, apr[:, 7 - s, :], api[:, 7 - s, :])):
            K.TT("dve", u1[lo], bbr[lo], bc_last(pw_r[lo], 16), ALU.mult, [kk], [kk])
            K.TT("dve", u2[lo], bbi[lo], bc_last(pw_i[lo], 16), ALU.mult, [kk], [kk])
            K.TT("dve", dst[lo, :, s, :], u1[lo], u2[lo], ALU.subtract, [kk], [kk])
            K.TT("dve", u1[hi], bbi[hi], bc_last(pw_r[hi], 16), ALU.mult, [kk], [kk])
            K.TT("dve", u2[hi], bbr[hi], bc_last(pw_i[hi], 16), ALU.mult, [kk], [kk])
            K.TT("dve", dst[hi, :, s, :], u1[hi], u2[hi], ALU.add, [kk], [kk])
        pw_r, pw_i = apr[:, s, :], api[:, s, :]
        K.TT("dve", u1[lo], cPr[lo], bc_last(pw_r[lo], 16), ALU.mult, [kk], [kk])
        K.TT("dve", u2[lo], cPi[lo], bc_last(pw_i[lo], 16), ALU.mult, [kk], [kk])
        K.TT("dve", Q2[lo, :, s, :], u1[lo], u2[lo], ALU.subtract, [kk], [kk])
        K.TT("dve", u1[hi], cPr[hi], bc_last(pw_i[hi], 16), ALU.mult, [kk], [kk])
        K.TT("dve", u2[hi], cPi[hi], bc_last(pw_r[hi], 16), ALU.mult, [kk], [kk])
        K.TT("dve", u1[hi], u1[hi], u2[hi], ALU.add, [kk], [kk])
        K.TS("dve", Q2[hi, :, s, :], u1[hi], -1.0, ALU.mult, [kk], [kk])
    msk = K.alloc((8, 16), F32)
    K.MEMSET("pool", msk, 1.0, [kk])
    K.S.op("pool", lambda e: e.affine_select(out=msk, in_=msk, pattern=[[16, 8], [0, 16]], compare_op=ALU.is_ge,
                                             fill=0.0, base=15, channel_multiplier=-1), [kk], [kk])
    mtmp = K.alloc((4, 128), F32)
    for gq in range(16):
        b = K.bank()
        for j in range(4):
            g = gq * 4 + j
            K.MM(K.ps[b][:, j * 128:(j + 1) * 128], Kk2f[:, g, :], Q2f[:, g, :], True, True, [kk], [("ps", b)])
        mskb = bass.AP(tensor=msk.tensor, offset=msk.offset, ap=[list(msk.ap[0]), [0, 4], [1, 128]])
        K.TT("dve", mtmp, K.ps[b][:].rearrange("p (j n) -> p j n", j=4), mskb, ALU.mult, [("ps", b), kk], ["mtmp"])
        for j in range(4):
            g = gq * 4 + j
            K.STT(Mt[:, g, :], K.identf, dS[:, g:g + 1], mtmp[:, j, :], ALU.mult, ALU.add, ["mtmp", kk, "identf"],
                  ["Mt"])
    for gq in range(16):
        b = K.bank()
        for j in range(4):
            g = gq * 4 + j
            K.TR(K.ps[b][:, j * 128:(j + 1) * 128], BsPf[:, g, :], K.identf, [kk, "identf"], [("ps", b)])
        K.CP("act", Bs[:, gq * 4:(gq + 1) * 4, :, :], K.ps[b][:].rearrange("p (j r q) -> p j r q", j=4, r=2),
             [("ps", b)], ["Bs"])
    K.S.barrier()
    K.release(m)


def s5_exchange(K, Z, C1, C2, selm, st_l, st_g, groups):
    kk = "xch"
    m = K.mark()
    K.DMA("sp", st_l, Z.rearrange("p a b -> p (a b)"), [("Zf", 0), ("Zf", 1)], ["st_l"])
    K.S.op("pool", lambda e: e.collective_compute("AllGather", ALU.bypass, replica_groups=groups, ins=[st_l],
                                                  outs=[st_g]), ["st_l"], ["st_g"], coll=True)
    G = K.alloc((4, 64), F32)
    K.DMA("sp", G, st_g.rearrange("(r p) c -> p r c", r=4), ["st_g"], [kk])
    Pk1 = K.alloc((3, 64), F32)
    Pk2 = K.alloc((3, 64), F32)
    t1 = K.alloc((64,), F32)
    t2 = K.alloc((64,), F32)
    c1f = C1.rearrange("p a b -> p (a b)")
    c2f = C2.rearrange("p a b -> p (a b)")
    K.MEMSET("dve", Pk1[:, 0, :], 1.0, [kk])
    K.MEMSET("dve", Pk2[:, 0, :], 0.0, [kk])
    K.CP("dve", Pk1[:, 1, :], c1f, [kk], [kk])
    K.CP("dve", Pk2[:, 1, :], c2f, [kk], [kk])

    def sq(a1, a2, o1, o2):
        K.TT("dve", t1, a1, a1, ALU.mult, [kk], [kk])
        K.TT("dve", t2, a2, a2, ALU.mult, [kk], [kk])
        K.TT("dve", t2, t1, t2, ALU.subtract, [kk], [kk])
        K.TT("dve", t1, a1, a2, ALU.mult, [kk], [kk])
        K.TS("dve", o2, t1, 2.0, ALU.mult, [kk], [kk])
        K.CP("dve", o1, t2, [kk], [kk])

    for _ in range(8):
        sq(Pk1[:, 1, :], Pk2[:, 1, :], Pk1[:, 1, :], Pk2[:, 1, :])
    sq(Pk1[:, 1, :], Pk2[:, 1, :], Pk1[:, 2, :], Pk2[:, 2, :])
    acc = K.alloc((64,), F32)
    cc1 = K.alloc((64,), F32)
    cc2 = K.alloc((64,), F32)
    K.MEMSET("dve", acc, 0.0, [kk])
    for r in range(4):
        K.TS("dve", cc1, Pk1[:, 0, :], selm[:, r * 3:r * 3 + 1], ALU.mult, [kk], [kk])
        K.TS("dve", cc2, Pk2[:, 0, :], selm[:, r * 3:r * 3 + 1], ALU.mult, [kk], [kk])
        for mm in (1, 2):
            K.STT(cc1, Pk1[:, mm, :], selm[:, r * 3 + mm:r * 3 + mm + 1], cc1, ALU.mult, ALU.add, [kk], [kk])
            K.STT(cc2, Pk2[:, mm, :], selm[:, r * 3 + mm:r * 3 + mm + 1], cc2, ALU.mult, ALU.add, [kk], [kk])
        g = G[:, r, :]
        gsw = bass.AP(tensor=g.tensor, offset=g.offset + 32, ap=[list(g.ap[0]), [-32, 2], [1, 32]])
        K.TT("dve", t1, cc1, g, ALU.mult, [kk], [kk])
        K.TT("dve", t2.rearrange("p (a b) -> p a b", a=2), cc2.rearrange("p (a b) -> p a b", a=2), gsw, ALU.mult,
             [kk], [kk])
        K.TT("dve", t1, t1, t2, ALU.add, [kk], [kk])
        K.TT("dve", acc, acc, t1, ALU.add, [kk], [kk])
    K.CP("dve", Z.rearrange("p a b -> p (a b)"), acc, [kk], [("Zf", 0), ("Zf", 1)])
    K.S.barrier()
    K.release(m)


def phase_S5(K, P, xmid, ada_dram, st_l, st_g, groups):
    S = K.S
    mT = K.mark()
    Csf = K.alloc((2, 32, 128), BF16)
    Cs = Csf.rearrange("p r g (t c) -> p r g t c", t=8)
    Mt = K.alloc((64, 128), BF16)
    Bs = K.alloc((64, 2, 64), BF16)
    C1 = K.alloc((2, 32), F32)
    C2 = K.alloc((2, 32), F32)
    mS5 = K.mark()
    ada_phase(K, P["crep"], P["ada_w"], P["ada_b"], ada_dram)
    s5_tables(K, P, Cs, Mt, Bs, C1, C2)
    w_in = K.alloc((8, 1024), BF16)
    w_glu = K.alloc((8, 1024), BF16)
    w_out = K.alloc((8, 1024), BF16)
    K.DMA("pool", w_in, P["w_in"].rearrange("(kc p) n -> p kc n", p=128), [], ["w_in"])
    K.DMA("pool", w_glu, P["w_glu"].rearrange("(kc p) n -> p kc n", p=128), [], ["w_glu"])
    mw = K.mark()
    g1, kg1, mbase, mk = load_mod(K, ada_dram, 0, P["norm1_g"])
    wst = K.alloc((8, 1024), F32)
    K.DMA("sp", wst, P["w_out"].rearrange("(kc p) n -> p kc n", p=128), [], ["wst"])
    for kc in range(8):
        K.TT("dve" if kc % 2 else "pool", w_out[:, kc, :], wst[:, kc, :], g1, ALU.mult, ["wst", kg1], ["w_out"])
    K.S.barrier()
    K.release(mw)
    gs1, sh1, kgs, ksh = load_mod2(K, ada_dram, mbase, mk, P["norm1_g"])
    selm = K.alloc((12,), F32)
    K.DMA("sp", selm, P["selm"], [], ["xch"])
    xrow = [K.alloc((1024,), F32) for _ in range(2)]
    xr2 = xrow
    tmpf = K.alloc((1024,), F32)
    junk = tmpf
    hrow = [K.alloc((1024,), BF16) for _ in range(2)]
    hTt = [K.alloc((8, 128), BF16) for _ in range(2)]
    zTt = [K.alloc((8, 128), BF16) for _ in range(2)]
    zgTt = [K.alloc((8, 128), BF16) for _ in range(2)]
    sgt = [K.alloc((512,), BF16) for _ in range(2)]
    xo = [K.alloc((512,), F32) for _ in range(2)]
    u = K.alloc((8, 1024), BF16)
    ug = u.rearrange("p t (g c) -> p (t g c)", g=64).rearrange("p (g s c) -> p g s c", g=64, s=8)
    ug2 = u.rearrange("p t c -> p (t c)").rearrange("p (g k) -> p g k", g=64)
    UT = K.alloc((64, 128), BF16)
    Sb = K.alloc((2, 32, 129), BF16)
    Zf = [K.alloc((2, 32), F32) for _ in range(2)]
    T1 = K.alloc((2, 32), F32)
    T2 = K.alloc((2, 32), F32)
    st = K.alloc((2,), F32)
    K.MEMSET("dve", Zf[0], 0.0, [("Zf", 0)])
    xsv = P["xs"].rearrange("(H n t) d -> H t n d", H=2, t=8)
    xmv = xmid.rearrange("(H n t) d -> H t n d", H=2, t=8)
    zcur = 0
    ukeys = [("u", t) for t in range(8)]
    for hs in range(4):
        pss, H = divmod(hs, 2)
        own = pss == 1
        if hs == 2:
            s5_exchange(K, Zf[zcur], C1, C2, selm, st_l, st_g, groups)
        for tau in range(8):
            i = tau % 2
            xr = xrow[i]
            K.DMA("sp", xr, xsv[H, tau], [], [("xrow", i)])
            K.ACT(junk, xr, AF.Square, [("xrow", i)], ["tmpf", "st"], accum_out=st[:, 0:1])
            K.ACT(st[:, 1:2], st[:, 0:1], AF.Sqrt, ["st"], ["st2"], scale=1.0 / 1024, bias=K.eps[:, 0:1])
            K.RECIP(st[:, 1:2], st[:, 1:2], ["st2"], ["st2"])
            K.STT(tmpf, xr, st[:, 1:2], gs1, ALU.mult, ALU.mult, [("xrow", i), "st2", kgs], ["tmpf"])
            K.TT("pool", hrow[i], tmpf, sh1, ALU.add, ["tmpf", ksh], [("hrow", i)])
            b = K.bank()
            psb = K.ps[b].bitcast(BF16)
            for kc in range(8):
                K.TR(psb[:, kc * 128:(kc + 1) * 128], hrow[i][:, kc * 128:(kc + 1) * 128], K.identb,
                     [("hrow", i), "identb"], [("ps", b)])
            K.CP("act", hTt[i], psb[:, 0:1024].rearrange("p (a n) -> p a n", a=8), [("ps", b)], [("hTt", i)])
            for ch in range(2):
                b = K.bank()
                for kc in range(8):
                    K.MM(K.ps[b][:], hTt[i][:, kc, :], w_in[:, kc, ch * 512:(ch + 1) * 512], kc == 0, kc == 7,
                         [("hTt", i), "w_in"], [("ps", b)])
                K.CP("dve" if ch else "act", ug[:, ch * 32:(ch + 1) * 32, tau, :],
                     K.ps[b][:].rearrange("p (g c) -> p g c", g=32), [("ps", b)], ukeys)
        for g8 in range(8):
            b = K.bank()
            psb = K.ps[b].bitcast(BF16)
            for j in range(8):
                g = g8 * 8 + j
                K.TR(psb[:, j * 128:(j + 1) * 128], ug2[:, g, :], K.identb, ukeys + ["identb"],
                     [("ps", b)])
            K.CP("act" if g8 % 2 else "dve", UT[:, g8 * 8:(g8 + 1) * 8, :],
                 psb[:, 0:1024].rearrange("p (a n) -> p a n", a=8), [("ps", b)], [("UT", g8)])
        for q in range(16):
            b = K.bank()
            for ri in range(2):
                for gp in range(2):
                    for h in range(2):
                        g = h * 32 + 2 * q + gp
                        c0 = (ri * 2 + gp) * 128
                        K.MM(K.ps[b][64 * h:64 * h + 64, c0:c0 + 128], Bs[:, g, ri, :], UT[:, g, :], True, True,
                             ["Bs", ("UT", g // 8)], [("ps", b)])
            K.ACT(Sb[:, :, 2 * q:2 * q + 2, 1:129], K.ps[b][:].rearrange("p (r g n) -> p r g n", r=2, g=2),
                  AF.Identity, [("ps", b)], ["Sb", "Sbx"])
        for n in range(128):
            zc = Zf[zcur]
            zn = Zf[1 - zcur]
            if own:
                K.CP("act", Sb[:, :, :, n], zc, [("Zf", zcur)], ["Sbx"])
            zsw = bass.AP(tensor=zc.tensor, offset=zc.offset + 32, ap=[list(zc.ap[0]), [-32, 2], [1, 32]])
            K.TT("dve", T1, C1, zc, ALU.mult, [("Zf", zcur)], ["T1"])
            K.TT("pool", T2, C2, zsw, ALU.mult, [("Zf", zcur)], ["T2"])
            K.TT("dve", T1, T1, T2, ALU.add, ["T1", "T2"], ["T1"])
            K.TT("dve", zn, T1, Sb[:, :, :, n + 1], ALU.add, ["T1", "Sb"], [("Zf", 1 - zcur)])
            zcur = 1 - zcur
        if not own:
            continue
        for gq in range(16):
            b = K.bank()
            for j in range(4):
                g = gq * 4 + j
                h, g32 = divmod(g, 32)
                rows = slice(64 * h, 64 * h + 64)
                o = K.ps[b][:, j * 128:(j + 1) * 128]
                K.MM(o, UT[:, g, :], Mt[:, g, :], True, False, [("UT", g // 8), "Mt"], [("ps", b)])
                K.MM(o, Sb[rows, 0, g32, 0:128], Csf[rows, 0, g32, :], False, False, ["Sbx", "Cs"], [("ps", b)])
                K.MM(o, Sb[rows, 1, g32, 0:128], Csf[rows, 1, g32, :], False, True, ["Sbx", "Cs"], [("ps", b)])
            zo = u[:, :, gq * 64:(gq + 1) * 64].rearrange("p t (j c) -> p j t c", j=4)
            K.ACT(zo, K.ps[b][:].rearrange("p (j t c) -> p j t c", j=4, t=8), AF.Gelu_apprx_tanh, [("ps", b)], ukeys)
        for tau in range(8):
            i = tau % 2
            b = K.bank()
            psb = K.ps[b].bitcast(BF16)
            for kc in range(8):
                K.TR(psb[:, kc * 128:(kc + 1) * 128], u[:, tau, kc * 128:(kc + 1) * 128], K.identb,
                     [("u", tau), "identb"], [("ps", b)])
            K.CP("act", zTt[i], psb[:, 0:1024].rearrange("p (a n) -> p a n", a=8), [("ps", b)], [("zTt", i)])
            for half in range(2):
                b = K.bank()
                for c4 in range(4):
                    co = half * 4 + c4
                    for kc in range(8):
                        K.MM(K.ps[b][:, c4 * 128:(c4 + 1) * 128], w_glu[:, kc, co * 128:(co + 1) * 128], zTt[i][:, kc, :],
                             kc == 0, kc == 7, ["w_glu", ("zTt", i)], [("ps", b)])
                K.ACT(sgt[half], K.ps[b][:], AF.Sigmoid, [("ps", b)], [("sgt", half)])
                K.TT("pool", zgTt[i][:, half * 4:(half + 1) * 4, :], zTt[i][:, half * 4:(half + 1) * 4, :],
                     sgt[half].rearrange("p (a n) -> p a n", a=4), ALU.mult, [("zTt", i), ("sgt", half)],
                     [("zgTt", i, half)])
            K.DMA("sp", xr2[i], xsv[H, tau], [], [("xrow", i)])
            for dh in range(2):
                b = K.bank()
                for kc in range(8):
                    K.MM(K.ps[b][:], zgTt[i][:, kc, :], w_out[:, kc, dh * 512:(dh + 1) * 512], kc == 0, kc == 7,
                         [("zgTt", i, kc // 4), "w_out"], [("ps", b)])
                K.TT("dve", xo[dh], K.ps[b][:], xr2[i][:, dh * 512:(dh + 1) * 512], ALU.add, [("ps", b), ("xrow", i)],
                     [("xo", dh)])
                K.DMA("sp", xmv[H, tau][:, dh * 512:(dh + 1) * 512], xo[dh], [("xo", dh)], ["xmid"])
    K.S.barrier()
    K.release(mT)
    return


SCALE = 128.0 ** -0.5
NEGB = -30000.0


def norm_slots(K, x, xkey, stat, junk):
    for s in range(16):
        K.ACT(junk, x[:, s, :], AF.Square, [xkey(s)], ["junk", "stat"], accum_out=stat[:, s:s + 1])
    K.ACT(stat, stat, AF.Sqrt, ["stat"], ["stat"], scale=1.0 / 1024, bias=K.eps[:, 0:1])
    K.RECIP(stat, stat, ["stat"], ["stat"])


def phase_L2(K, P, x, xkey, ada_dram, pos, qT_o, kT_o, v_o, km_o, after_kv=None):
    S = K.S
    mL2 = K.mark()
    cosT = K.alloc((2048,), F32)
    sinT = K.alloc((2048,), F32)
    Rm = K.alloc((128,), BF16)
    K.TS("dve", Rm[:, 0:64], K.identf[:, 64:128], -1.0, ALU.mult, ["identf"], ["Rm"])
    K.CP("dve", Rm[:, 64:128], K.identf[:, 0:64], ["identf"], ["Rm"])
    mr = K.mark()
    posi = K.alloc((2048,), I32)
    ang = K.alloc((2048,), F32)
    invf = K.alloc((1,), F32)
    K.DMA("sp", posi, pos.partition_broadcast(128), [], ["posi"])
    K.DMA("sp", invf, P["invf"], [], ["invf"])
    K.CP("dve", ang, posi, ["posi"], ["ang"])
    K.TS("dve", ang, ang, invf[:, 0:1], ALU.mult, ["ang", "invf"], ["ang"])
    sincos(K, ang, sinT, cosT, (2048,), "ang", "rope")
    K.S.barrier()
    K.release(mr)
    g1, kg1, mbase, mk = load_mod(K, ada_dram, 0, P["norm1_g"], want_gate=False)
    gs1, sh1, kgs, ksh = load_mod2(K, ada_dram, mbase, mk, P["norm1_g"])
    hT = K.alloc((8, 2048), BF16)
    wqb = [K.alloc((8, 1024), BF16) for _ in range(2)]
    wqv = P["w_qkv"].rearrange("(kc p) n -> p kc n", p=128)
    K.DMA("pool", wqb[1], wqv[:, :, 1024:2048], [], [("wq", 1)])
    K.DMA("pool", wqb[0], wqv[:, :, 2048:3072], [], [("wq", 0)])
    stat = K.alloc((16,), F32)
    tmpf = K.alloc((1024,), F32)
    hrow = [K.alloc((1024,), BF16) for _ in range(2)]
    for s in range(16):
        i = s % 2
        K.ACT(tmpf, x[:, s, :], AF.Square, [xkey(s)], ["junk", "st"], accum_out=stat[:, 0:1])
        K.ACT(stat[:, 1:2], stat[:, 0:1], AF.Sqrt, ["st"], ["st2"], scale=1.0 / 1024, bias=K.eps[:, 0:1])
        K.RECIP(stat[:, 1:2], stat[:, 1:2], ["st2"], ["st2"])
        K.STT(tmpf, x[:, s, :], stat[:, 1:2], gs1, ALU.mult, ALU.mult, [xkey(s), "st2", kgs], ["junk"])
        K.TT("pool", hrow[i], tmpf, sh1, ALU.add, ["junk", ksh], [("hrow", i)])
        b = K.bank()
        psb = K.ps[b].bitcast(BF16)
        for kc in range(8):
            K.TR(psb[:, kc * 128:(kc + 1) * 128], hrow[i][:, kc * 128:(kc + 1) * 128], K.identb,
                 [("hrow", i), "identb"], [("ps", b)])
        K.CP("act", hT[:, :, s * 128:(s + 1) * 128], psb[:, 0:1024].rearrange("p (a n) -> p a n", a=8),
             [("ps", b)], [("hT", s)])
    tf = [K.alloc((512,), F32) for _ in range(2)]
    tb = [K.alloc((512,), BF16) for _ in range(2)]
    ta = [K.alloc((512,), F32) for _ in range(2)]
    tq = [K.alloc((512,), F32) for _ in range(2)]
    tk = [K.alloc((512,), BF16) for _ in range(2)]
    km = K.alloc((8, 8), F32)
    it = [0]

    def qk_pass(which):
        for h in range(8):
            for tt in range(4):
                i = it[0] % 2
                it[0] += 1
                b = K.bank()
                tsl = slice(tt * 512, (tt + 1) * 512)
                c0 = h * 128
                for kc in range(8):
                    K.MM(K.ps[b][:], wqb[which][:, kc, c0:c0 + 128], hT[:, kc, tsl], kc == 0, kc == 7,
                         [("wq", which)] + [("hT", s) for s in range(tt * 4, tt * 4 + 4)], [("ps", b)])
                K.CP("act", tf[i], K.ps[b][:], [("ps", b)], [("tf", i)])
                K.CP("dve", tb[i], tf[i], [("tf", i)], [("tb", i)])
                b2 = K.bank()
                K.MM(K.ps[b2][:], Rm, tb[i], True, True, ["Rm", ("tb", i)], [("ps", b2)])
                K.TT("dve", ta[i], K.ps[b2][:], sinT[:, tsl], ALU.mult, [("ps", b2), "rope"], [("ta", i)])
                K.TT("dve", tf[i], tf[i], cosT[:, tsl], ALU.mult, [("tf", i), "rope"], [("tf", i)])
                if which == 0:
                    K.TT("dve", tq[i], tf[i], ta[i], ALU.add, [("tf", i), ("ta", i)], [("tq", i)])
                    K.ACT(tq[i], tq[i], AF.Copy, [("tq", i)], [("tq", i)], scale=SCALE)
                    K.DMA("sp", qT_o[h][:, tsl], tq[i], [("tq", i)], ["qTo"])
                else:
                    K.TT("dve", tq[i], tf[i], ta[i], ALU.add, [("tf", i), ("ta", i)], [("tq", i)])
                    K.CP("act", tk[i], tq[i], [("tq", i)], [("tk", i)])
                    K.DMA("sp", kT_o[h][:, tsl], tk[i], [("tk", i)], [("kTo", h)])
                    K.S.op("dve", lambda e, o=km[:, h, tt * 2:tt * 2 + 2], a=tq[i].rearrange("p (b n) -> p b n", b=2):
                           e.tensor_reduce(out=o, in_=a, axis=AX.X, op=ALU.add), [("tq", i)], ["km"])

    qk_pass(1)
    K.TS("dve", km, km, 1.0 / 256, ALU.mult, ["km"], ["km"])
    K.DMA("sp", km_o.rearrange("(h d) n -> d h n", h=8), km, ["km"], ["kmo"])
    vb = [K.alloc((512,), BF16) for _ in range(2)]
    for s in range(16):
        for dh in range(2):
            i = (s * 2 + dh) % 2
            b = K.bank()
            for kc in range(8):
                K.MM(K.ps[b][:], hT[:, kc, s * 128:(s + 1) * 128], wqb[0][:, kc, dh * 512:(dh + 1) * 512],
                     kc == 0, kc == 7, [("hT", s), ("wq", 0)], [("ps", b)])
            K.CP("act" if dh else "dve", vb[i], K.ps[b][:], [("ps", b)], [("vb", i)])
            for h4 in range(4):
                hh = dh * 4 + h4
                K.DMA("sp", v_o[hh][s * 128:(s + 1) * 128, :], vb[i][:, h4 * 128:(h4 + 1) * 128], [("vb", i)], [("vo", hh)])
    if after_kv is not None:
        after_kv()
    K.DMA("pool", wqb[0], wqv[:, :, 0:1024], [], [("wq", 0)])
    qk_pass(0)
    K.S.barrier()
    K.release(mL2)


def phase_L3(K, P, x, xkey, ada_dram, qT_l, kT_l, v_l, kT_g, v_g, km_g, out, nexp=32):
    S = K.S
    dbg = False
    mO = K.mark()
    oT = K.alloc((8, 2048), BF16)
    mA = K.mark()
    onesb = K.alloc((128,), BF16)
    K.MEMSET("pool", onesb, 1.0, ["onesb"])
    cmask = K.alloc((4, 512), BF16)
    K.MEMSET("pool", cmask, 0.0, ["cmask"])
    for i in range(4):
        bk = i // 2
        cm = cmask[:, i, bk * 256:(bk + 1) * 256]
        S.op("pool", lambda e, cm=cm, i=i: e.affine_select(out=cm, in_=cm, pattern=[[1, 256]], compare_op=ALU.is_ge,
                                                           fill=NEGB, base=-128 * (i % 2), channel_multiplier=-1),
             ["cmask"], ["cmask"])
    NT = 64
    Ind = K.alloc((40, 128), BF16)
    K.MEMSET("pool", Ind, 0.0, ["Ind"])
    K.DMA("sp", Ind[0:32], P["Ind"], ["Ind"], ["Ind"])
    kmT = K.alloc((8, 32), F32)
    for r in range(4):
        K.DMA("sp", kmT[:, :, r * 8:(r + 1) * 8], km_g[r * 1024:(r + 1) * 1024, :].rearrange("(h d) n -> d h n", h=8),
              ["km_g"], ["kmT"])
    bm_lt = K.alloc((16, 32), F32)
    bm_eq = K.alloc((16, 32), F32)
    bm_pen = K.alloc((16, 32), F32)
    bm_no = K.alloc((16, 32), F32)
    K.DMA("sp", bm_lt, P["bm_lt"], [], ["bm"])
    K.DMA("sp", bm_eq, P["bm_eq"], [], ["bm"])
    K.DMA("sp", bm_pen, P["bm_pen"], [], ["bm"])
    K.DMA("sp", bm_no, P["bm_no"], [], ["bm"])
    kTh = K.alloc((NT * 128,), BF16)
    vh = K.alloc((NT, 128), BF16)
    qf = K.alloc((2048,), F32)
    qb = K.alloc((2048,), BF16)
    BiasT2 = [K.alloc((2048,), BF16) for _ in range(2)]
    BiasG2 = [K.alloc((2048,), BF16) for _ in range(2)]
    for hb_ in range(2):
        K.MEMSET("pool", BiasT2[hb_], 0.0, [("Bias", hb_)])
        K.MEMSET("pool", BiasG2[hb_], 0.0, [("Bias", hb_)])
    NPT = 7
    pT = [K.alloc((512,), BF16) for _ in range(NPT)]
    rec = K.alloc((512,), F32)
    LaccD = K.alloc((512,), F32)
    LaccP = K.alloc((512,), F32)
    onesf = K.alloc((128,), F32)
    K.MEMSET("pool", onesf, 1.0, ["onesf"])
    gtmp = [K.alloc((48,), F32) for _ in range(2)]
    gal = K.alloc((16, 64), F32)
    SB = [0, 1, 2, 3]
    BL, BT, BG = 6, 6, 7
    nsb = 0
    npt = 0

    def gating_front(h):
        K.DMA("sp", qf, qT_l[h], ["qT_l"], ["qf"])
        for s in range(16):
            K.MM(K.ps[BG][:, s * 32:(s + 1) * 32], qf[:, s * 128:(s + 1) * 128], kmT[:, h, :], True, True,
                 ["qf", "kmT"], [("ps", BG)])
        for s in range(16):
            i = s % 2
            g = gtmp[i]
            gk = ("gtmp", i)
            gm, m8 = g[:, 0:32], g[:, 32:40]
            al, al2 = gal[:, s, 0:32], gal[:, s, 32:64]
            ak = ("gal", s)
            K.TT("dve", gm, K.ps[BG][:, s * 32:(s + 1) * 32], bm_lt[:, s, :], ALU.mult, [("ps", BG), "bm"], [gk])
            K.TT("dve", gm, gm, bm_pen[:, s, :], ALU.add, [gk, "bm"], [gk])
            S.op("dve", lambda e, m8=m8, gm=gm: e.max(out=m8, in_=gm), [gk], [gk])
            K.TS("dve", al, gm, m8[:, 2:3], ALU.is_ge, [gk], [ak])
            K.TT("dve", al, al, bm_lt[:, s, :], ALU.mult, [ak, "bm"], [ak])
            K.TT("dve", al, al, bm_eq[:, s, :], ALU.add, [ak, "bm"], [ak])
            K.TT("dve", al2, al, bm_no[:, s, :], ALU.mult, [ak, "bm"], [ak])
            K.TS("dve", al, al, -1.0, ALU.add, [ak], [ak], s2=-NEGB, op1=ALU.mult)
            K.TS("dve", al2, al2, -1.0, ALU.add, [ak], [ak], s2=-NEGB, op1=ALU.mult)

    def gating_back(h):
        hb = h % 2
        for which, dst in ((0, BiasT2[hb]), (1, BiasG2[hb])):
            for s4 in range(4):
                for j in range(4):
                    s = s4 * 4 + j
                    K.TR(K.ps[BT][0:32, j * 128:(j + 1) * 128], gal[:, s, which * 32:(which + 1) * 32], K.identf,
                         [("gal", s), "identf"], [("ps", BT)])
                K.CP("act", dst[0:32, s4 * 512:(s4 + 1) * 512], K.ps[BT][0:32, :], [("ps", BT)], [("Bias", hb)])

    gating_front(0)
    gating_back(0)
    for h in range(8):
        hb = h % 2
        BiasT, BiasG = BiasT2[hb], BiasG2[hb]
        K.DMA("sp", kTh[:, 0:2048], kT_l[h], [("kTo", h)], ["kTh"])
        K.DMA("sp", vh[:, 0:16, :], v_l[h].rearrange("(kt k) d -> k kt d", k=128), [("vo", h)], ["vh"])
        for r in range(3):
            K.DMA("sp", kTh[:, (r + 1) * 2048:(r + 2) * 2048], kT_g[h][r * 128:(r + 1) * 128, :], [("kT_g", h)], ["kTh"])
            K.DMA("sp", vh[:, (r + 1) * 16:(r + 2) * 16, :],
                  v_g[h].rearrange("(kt k) d -> k kt d", k=128)[:, r * 16:(r + 1) * 16, :], [("v_g", h)], ["vh"])
        K.CP("pool", qb, qf, ["qf"], ["qb"])
        for qt in range(4):
            qsl = slice(qt * 512, (qt + 1) * 512)
            bo = 4 + (qt % 2)
            bl = BL

            def s_part(kt):
                nonlocal nsb, npt
                bs = SB[nsb % 4]
                nsb += 1
                diag = (kt // 4 == qt) and kt < 16
                K.MM(K.ps[bs][:], kTh[:, kt * 128:(kt + 1) * 128], qb[:, qsl], True, False, ["kTh", "qb"],
                     [("ps", bs)])
                bias_t = BiasT if kt < 16 else BiasG
                K.MM(K.ps[bs][:], Ind[:, kt // 2, :], bias_t[:, qsl], False, not diag, ["Ind", ("Bias", hb)],
                     [("ps", bs)])
                if diag:
                    K.MM(K.ps[bs][:], K.identb, cmask[:, kt % 4, :], False, True, ["identb", "cmask"], [("ps", bs)])
                p = pT[npt % NPT]
                pk = ("pT", npt % NPT)
                npt += 1
                K.ACT(p, K.ps[bs][:], AF.Exp, [("ps", bs)], [pk])
                return p, pk

            def pv_part(kt, p, pk):
                K.MM(K.ps[bo][:], vh[:, kt, :], p, kt == 0, kt == NT - 1, ["vh", pk], [("ps", bo)])
                K.MM(K.ps[bl][:], onesb, p, kt == 0, kt == NT - 1, ["onesb", pk], [("ps", bl)])

            LOOK = 3
            pend = []
            for kt in range(NT + LOOK):
                if kt < NT:
                    pend.append(s_part(kt))
                if kt >= LOOK:
                    pp, ppk = pend.pop(0)
                    pv_part(kt - LOOK, pp, ppk)
            K.RECIP(rec, K.ps[bl][:], [("ps", bl)], ["rec"])
            K.TT("dve", oT[:, h, qsl], K.ps[bo][:], rec, ALU.mult, [("ps", bo), "rec"], [("oT", h)])
            if h + 1 < 8:
                if qt == 0:
                    gating_front(h + 1)
                if qt == 2:
                    gating_back(h + 1)
    K.S.barrier()
    K.release(mA)
    wo = K.alloc((8, 1024), BF16)
    g1, kg1, mbase, mk = load_mod(K, ada_dram, 0, None)
    wst = K.alloc((8, 1024), F32)
    K.DMA("sp", wst, P["w_o"].rearrange("(h p) n -> p h n", p=128), [], ["wst"])
    for hh in range(8):
        K.TT("dve" if hh % 2 else "pool", wo[:, hh, :], wst[:, hh, :], g1, ALU.mult, ["wst", kg1], ["wo"])
    for s_ in range(16):
        for dh in range(2):
            b = K.bank()
            for hh in range(8):
                K.MM(K.ps[b][:], oT[:, hh, s_ * 128:(s_ + 1) * 128], wo[:, hh, dh * 512:(dh + 1) * 512], hh == 0, hh == 7,
                     [("oT", hh), "wo"], [("ps", b)])
            xs = x[:, s_, dh * 512:(dh + 1) * 512]
            K.TT("dve", xs, K.ps[b][:], xs, ALU.add, [("ps", b), xkey(s_)], [xkey(s_)])
    if dbg:
        for s_ in range(16):
            K.DMA("sp", xatt[s_ * 128:(s_ + 1) * 128, :], x[:, s_, :], [xkey(s_)], ["xatt"])
    K.S.barrier()
    K.release(mO)
    moe_phase(K, x, xkey, ada_dram, P, nexp=nexp)
    mF = K.mark()
    fg = K.alloc((1024,), F32)
    stat = K.alloc((16,), F32)
    junk = K.alloc((1024,), F32)
    ot = [K.alloc((1024,), F32) for _ in range(2)]
    K.DMA("sp", fg, P["final_g"].partition_broadcast(128), [], ["fg"])
    norm_slots(K, x, xkey, stat, junk)
    for s_ in range(16):
        i = s_ % 2
        K.STT(ot[i], x[:, s_, :], stat[:, s_:s_ + 1], fg, ALU.mult, ALU.mult, [xkey(s_), "stat", "fg"], [("ot", i)])
        K.DMA("sp", out[s_ * 128:(s_ + 1) * 128, :], ot[i], [("ot", i)], ["out"])


L0_SHAPES = {
    "xs": (2048, 1024), "selm": (128, 12), "crep": (128, 8, 128),
    "ada_w": (1024, 6144), "ada_b": (6144,), "norm1_g": (1024,), "norm2_g": (1024,),
    "w_in": (1024, 1024), "w_glu": (1024, 1024), "w_out": (1024, 1024),
    "lamP_re": (128, 64), "lamP_im": (128, 64), "logdtP": (128, 64),
    "bP_re": (128, 64, 16), "bP_im": (128, 64, 16), "cP_re": (128, 64, 16), "cP_im": (128, 64, 16),
    "dS": (128, 64),
    "w_router": (1024, 32), "b_router": (32,), "w_gate_up": (32, 1024, 2048), "bguT": (128, 32, 16),
    "w_down": (32, 1024, 1024), "b_down": (32, 1024),
}
L1_SHAPES = {
    "ada_w": (1024, 6144), "ada_b": (6144,), "norm1_g": (1024,), "norm2_g": (1024,), "final_g": (1024,),
    "w_qkv": (1024, 3072), "w_o": (1024, 1024), "invf": (128, 1),
    "bm_lt": (128, 16, 32), "bm_eq": (128, 16, 32), "bm_pen": (128, 16, 32), "bm_no": (128, 16, 32),
    "w_router": (1024, 32), "b_router": (32,), "w_gate_up": (32, 1024, 2048), "bguT": (128, 32, 16),
    "w_down": (32, 1024, 1024), "b_down": (32, 1024),
}
GROUPS = [[0, 1, 2, 3], [4, 5, 6, 7]]


def build_fused(nexp=32):
    K = KB()
    P0 = {k: K.din("a_" + k, v) for k, v in L0_SHAPES.items()}
    P1 = {k: K.din("b_" + k, v) for k, v in L1_SHAPES.items()}
    P1["crep"] = P0["crep"]
    P1["Ind"] = K.din("b_Ind", (32, 40, 128), BF16)
    pos = K.din("b_pos", (2048,), I32)
    out = K.dout("out", (2048, 1024))
    xmid = K.dtmp("xmid", (2048, 1024))
    ada0 = K.dtmp("ada0", (128, 6144))
    ada1 = K.dtmp("ada1", (128, 6144))
    qT_l = K.dtmp("qT_l", (8, 128, 2048))
    kT_l = K.dtmp("kT_l", (8, 128, 2048), BF16)
    v_l = K.dtmp("v_l", (8, 2048, 128), BF16)
    km_l = K.dtmp("km_l", (1024, 8))
    kT_g = K.dtmp("kT_g", (8, 512, 2048), BF16)
    v_g = K.dtmp("v_g", (8, 8192, 128), BF16)
    km_g = K.dtmp("km_g", (4096, 8))
    K.consts()
    st_l = K.dtmp("st_l", (128, 64))
    st_g = K.dtmp("st_g", (512, 64))
    phase_S5(K, P0, xmid, ada0, st_l, st_g, GROUPS)
    x = K.alloc((16, 1024), F32)
    xkey = lambda s: ("x", s)
    for s in range(16):
        K.DMA("sp", x[:, s, :], xmid[s * 128:(s + 1) * 128, :], ["xmid"], [xkey(s)])
    moe_phase(K, x, xkey, ada0, P0, nexp=nexp)
    ada_phase(K, P1["crep"], P1["ada_w"], P1["ada_b"], ada1)
    def gather_kv():
        cl = [(km_l, km_g, "kmo", "km_g")]
        for h in range(8):
            cl.append((kT_l[h], kT_g[h], ("kTo", h), ("kT_g", h)))
            cl.append((v_l[h], v_g[h], ("vo", h), ("v_g", h)))
        for (src, dst, kr, kw) in cl:
            K.S.op("pool", lambda e, src=src, dst=dst: e.collective_compute("AllGather", ALU.bypass, replica_groups=GROUPS,
                                                                             ins=[src], outs=[dst]), [kr], [kw], coll=True)

    phase_L2(K, P1, x, xkey, ada1, pos, qT_l, kT_l, v_l, km_l, after_kv=gather_kv)
    phase_L3(K, P1, x, xkey, ada1, qT_l, kT_l, v_l, kT_g, v_g, km_g, out, nexp=nexp)
    return K.finish()


def prep_common(inp, li, b):
    pre = "l%d_" % li
    c = np.asarray(inp["c"][b], np.float32)
    crep = np.ascontiguousarray(np.broadcast_to(c.reshape(8, 128).T[:, :, None], (128, 8, 128)))
    d = {
        "crep": crep,
        "ada_w": inp[pre + "ada_w"], "ada_b": inp[pre + "ada_b"],
        "norm1_g": inp[pre + "norm1_g"], "norm2_g": inp[pre + "norm2_g"],
        "w_router": inp[pre + "moe_w_router"], "b_router": inp[pre + "moe_b_router"],
        "w_gate_up": inp[pre + "moe_w_gate_up"],
        "bguT": np.ascontiguousarray(np.asarray(inp[pre + "moe_b_gate_up"]).reshape(32, 16, 128).transpose(2, 0, 1)),
        "w_down": inp[pre + "moe_w_down"], "b_down": inp[pre + "moe_b_down"],
    }
    return d


def prep_L1(inp, core):
    b, j = divmod(core, 4)
    x = np.asarray(inp["x"][b])
    selm = np.zeros((128, 12), np.float32)
    for r in range(4):
        mm = j - 1 - r
        if 0 <= mm <= 2:
            selm[:, r * 3 + mm] = 1.0
    d = prep_common(inp, 0, b)
    d["xs"] = x[j * 2048:(j + 1) * 2048]
    d["selm"] = selm
    g = lambda n: np.asarray(inp["l0_s5_" + n], np.float32)
    t2 = lambda a: np.ascontiguousarray(np.concatenate([a, a], axis=0))
    d["w_in"], d["w_glu"], d["w_out"] = g("w_in"), g("w_glu"), g("w_out")
    d["lamP_re"] = t2(g("lam_re").T)
    d["lamP_im"] = t2(g("lam_im").T)
    d["logdtP"] = np.ascontiguousarray(np.broadcast_to(g("log_dt")[None, :], (128, 64)))
    d["bP_re"] = t2(g("b_re").transpose(1, 0, 2))
    d["bP_im"] = t2(g("b_im").transpose(1, 0, 2))
    d["cP_re"] = t2(g("c_re").transpose(2, 0, 1))
    d["cP_im"] = t2(g("c_im").transpose(2, 0, 1))
    d["dS"] = np.ascontiguousarray(np.tile(g("d").reshape(64, 16).T, (8, 1)))
    return {k: np.ascontiguousarray(np.asarray(v, np.float32)) for k, v in d.items()}


def f32c(a):
    return np.ascontiguousarray(np.asarray(a, np.float32))


def prep_L2(inp, core, x0):
    b, j = divmod(core, 4)
    d = prep_common(inp, 1, b)
    inv = (np.float32(10000.0) ** (-np.arange(0, 128, 2, dtype=np.float32) / np.float32(128))).astype(np.float32)
    m = {"x0": f32c(x0), "crep": d["crep"], "ada_w": f32c(d["ada_w"]), "ada_b": f32c(d["ada_b"]),
         "norm1_g": f32c(d["norm1_g"]), "w_qkv": f32c(inp["l1_moba_w_qkv"]),
         "invf": f32c(np.concatenate([inv, inv])[:, None]),
         "pos": np.ascontiguousarray(np.asarray(inp["positions"][b, j * 2048:(j + 1) * 2048], np.int32))}
    return m


def seg_order(j):
    return [j] + [k for k in range(4) if k != j]


def prep_L3(inp, core, x0, l2res):
    import ml_dtypes
    b, j = divmod(core, 4)
    d = prep_common(inp, 1, b)
    order = seg_order(j)
    m = {k: f32c(d[k]) for k in ("crep", "ada_w", "ada_b", "norm2_g", "w_router", "b_router", "w_gate_up", "bguT",
                                 "w_down", "b_down")}
    m["x0"] = f32c(x0)
    m["final_g"] = f32c(inp["final_norm_g"])
    m["w_o"] = f32c(inp["l1_moba_w_o"])
    m["qT"] = l2res[core]["qT"]
    m["kT_all"] = np.ascontiguousarray(np.concatenate([l2res[b * 4 + k]["kT"] for k in order], axis=2))
    m["v_all"] = np.ascontiguousarray(np.concatenate([l2res[b * 4 + k]["v"] for k in order], axis=0))
    km = np.concatenate([l2res[b * 4 + k]["kmT"] for k in range(4)], axis=2)
    m["kmT"] = f32c(km.transpose(1, 0, 2))
    gblk = np.array([order[bp // 8] * 8 + bp % 8 for bp in range(32)])
    ind = (np.arange(32)[:, None] == gblk[None, :]).astype(np.float32)
    m["Ind"] = np.ascontiguousarray(np.broadcast_to(ind[:, :, None], (32, 32, 128))).astype(ml_dtypes.bfloat16)
    jq = 8 * j + np.arange(16) // 2
    n = np.arange(32)
    lt = (n[None, :] < jq[:, None]).astype(np.float32)
    eq = (n[None, :] == jq[:, None]).astype(np.float32)
    bc = lambda a: np.ascontiguousarray(np.broadcast_to(a[None], (128, 16, 32))).astype(np.float32)
    m["bm_lt"], m["bm_eq"], m["bm_pen"] = bc(lt), bc(eq), bc((lt - 1.0) * 1e30)
    return m


def prep_fused(inp, core):
    import ml_dtypes
    b, j = divmod(core, 4)
    m0 = prep_L1(inp, core)
    m = {"a_" + k: v for k, v in m0.items()}
    d = prep_common(inp, 1, b)
    inv = (np.float32(10000.0) ** (-np.arange(0, 128, 2, dtype=np.float32) / np.float32(128))).astype(np.float32)
    m1 = {k: f32c(d[k]) for k in ("ada_w", "ada_b", "norm1_g", "norm2_g", "w_router", "b_router", "w_gate_up", "bguT",
                                  "w_down", "b_down")}
    m1["final_g"] = f32c(inp["final_norm_g"])
    m1["w_qkv"] = f32c(inp["l1_moba_w_qkv"])
    m1["w_o"] = f32c(inp["l1_moba_w_o"])
    m1["invf"] = f32c(np.concatenate([inv, inv])[:, None])
    jq = 8 * j + np.arange(16) // 2
    n = np.arange(32)
    lt = (n[None, :] < jq[:, None]).astype(np.float32)
    eq = (n[None, :] == jq[:, None]).astype(np.float32)
    notown = np.broadcast_to(((n // 8) != j).astype(np.float32)[None, :], (16, 32))
    bc = lambda a: np.ascontiguousarray(np.broadcast_to(a[None], (128, 16, 32))).astype(np.float32)
    m1["bm_lt"], m1["bm_eq"], m1["bm_pen"], m1["bm_no"] = bc(lt), bc(eq), bc((lt - 1.0) * 1e30), bc(notown)
    for k, v in m1.items():
        m["b_" + k] = v
    gb = np.concatenate([8 * j + np.arange(8), np.arange(32)])
    ind = (np.arange(32)[:, None] == gb[None, :]).astype(np.float32)
    m["b_Ind"] = np.ascontiguousarray(np.broadcast_to(ind[:, :, None], (32, 40, 128))).astype(ml_dtypes.bfloat16)
    m["b_pos"] = np.ascontiguousarray(np.asarray(inp["positions"][b, j * 2048:(j + 1) * 2048], np.int32))
    return m


_INPUT_NAMES = (
    "x", "c", "positions", "l0_norm1_g",
    "l0_ada_w", "l0_ada_b", "l0_s5_w_in", "l0_s5_b_re",
    "l0_s5_b_im", "l0_s5_c_re", "l0_s5_c_im", "l0_s5_lam_re",
    "l0_s5_lam_im", "l0_s5_log_dt", "l0_s5_d", "l0_s5_w_glu",
    "l0_s5_w_out", "l0_norm2_g", "l0_moe_w_router", "l0_moe_b_router",
    "l0_moe_w_gate_up", "l0_moe_b_gate_up", "l0_moe_w_down", "l0_moe_b_down",
    "l1_norm1_g", "l1_ada_w", "l1_ada_b", "l1_moba_w_qkv",
    "l1_moba_w_o", "l1_norm2_g", "l1_moe_w_router", "l1_moe_b_router",
    "l1_moe_w_gate_up", "l1_moe_b_gate_up", "l1_moe_w_down", "l1_moe_b_down",
    "final_norm_g",
)


def kernel(**inputs):
    inp = {k: np.asarray(inputs[k]) for k in _INPUT_NAMES}
    cores = list(range(8))
    nc = build_fused()
    res = run_bass_kernel_spmd(nc, [prep_fused(inp, c) for c in cores], core_ids=cores).results
    out = np.zeros((2, 8192, 1024), np.float32)
    for c in cores:
        b, j = divmod(c, 4)
        out[b, j * 2048:(j + 1) * 2048] = np.asarray(res[c]["out"])
    return out
```

```python
import math
import numpy as np
from contextlib import ExitStack
import concourse.bass as bass
import concourse.mybir as mybir
from concourse.bass_utils import run_bass_kernel_spmd

F32 = mybir.dt.float32
BF16 = mybir.dt.bfloat16
I32 = mybir.dt.int32
AF = mybir.ActivationFunctionType
ALU = mybir.AluOpType
AX = mybir.AxisListType

QUEUES = ("pe", "act", "dve", "pool", "sp")
DMA_SLOTS = 8
SAME_Q_SYNC = True


class _Op:
    __slots__ = ("q", "fn", "dma", "deps", "sig", "cnt", "slot", "slotcnt", "inc", "semq")


class Sched:
    def __init__(self):
        self.ops = []
        self.lastw = {}
        self.readers = {}
        self.ndma = {q: 0 for q in QUEUES}
        self.ncoll = 0

    def op(self, q, fn, reads=(), writes=(), dma=False, coll=False):
        o = _Op()
        if coll:
            dma = True
        o.q, o.fn, o.dma, o.sig, o.cnt = q, fn, dma, False, 0
        o.inc, o.semq = 16, q
        deps = set()
        raw = set()
        for k in reads:
            w = self.lastw.get(k)
            if w is not None:
                deps.add(w)
                raw.add(w)
        for k in writes:
            w = self.lastw.get(k)
            if w is not None:
                deps.add(w)
            for r in self.readers.get(k, ()):
                deps.add(r)
        o.deps = []
        for d in deps:
            if d.q == q and not d.dma:
                if not (SAME_Q_SYNC and d in raw and q != "pe"):
                    continue
            d.sig = True
            o.deps.append(d)
        for k in reads:
            self.readers.setdefault(k, []).append(o)
        for k in writes:
            self.lastw[k] = o
            self.readers[k] = []
        if coll:
            o.inc, o.semq = 1, "cc"
            o.slot = self.ncoll
            o.slotcnt = 1
            self.ncoll += 1
        elif dma:
            o.slot = self.ndma[q] % DMA_SLOTS
            o.slotcnt = self.ndma[q] // DMA_SLOTS + 1
            self.ndma[q] += 1
        self.ops.append(o)
        return o

    def barrier(self):
        last = {}
        for o in self.ops:
            if o.fn is not None and not o.dma:
                last[o.q] = o
        dmas = []
        for q in QUEUES:
            dq = [o for o in self.ops if o.dma and o.q == q and o.semq == q]
            dmas += dq[-DMA_SLOTS:]
        for q in QUEUES:
            o = self.op(q, None)
            for d in list(last.values()) + dmas:
                if d.q == q and not d.dma:
                    continue
                d.sig = True
                o.deps.append(d)
        keep = {k: w for k, w in self.lastw.items() if w.dma and w.semq == "cc"}
        self.lastw.clear()
        self.lastw.update(keep)
        self.readers.clear()

    def emit(self, sems, dsems, block):
        cnt = {q: 0 for q in QUEUES}
        for o in self.ops:
            if o.dma:
                continue
            if o.sig and o.fn is not None:
                cnt[o.q] += 1
            o.cnt = cnt[o.q]
        byq = {q: [o for o in self.ops if o.q == q] for q in QUEUES}
        engs = {"pe": "tensor", "act": "scalar", "dve": "vector", "pool": "gpsimd", "sp": "sync"}

        def make(q):
            def body(e):
                waited = {}
                for o in byq[q]:
                    need = {}
                    for d in o.deps:
                        if d.dma:
                            key = ("d", d.semq, d.slot)
                            v = d.inc * d.slotcnt
                        else:
                            key = ("e", d.q)
                            v = d.cnt
                        if need.get(key, 0) < v:
                            need[key] = v
                    if o.dma and o.slotcnt > 1 and o.semq == q:
                        key = ("d", q, o.slot)
                        v = 16 * (o.slotcnt - 1)
                        if need.get(key, 0) < v:
                            need[key] = v
                    for key, v in need.items():
                        if waited.get(key, 0) >= v:
                            continue
                        waited[key] = v
                        if key[0] == "d":
                            e.wait_ge(dsems[key[1]][key[2]], v)
                        else:
                            e.wait_ge(sems[key[1]], v)
                    if o.fn is None:
                        continue
                    ins = o.fn(e)
                    if o.dma:
                        ins.then_inc(dsems[o.semq][o.slot], o.inc)
                    elif o.sig:
                        ins.then_inc(sems[q], 1)
                n = self.ndma[q]
                for s in range(min(n, DMA_SLOTS)):
                    c = (n - 1 - s) // DMA_SLOTS + 1
                    e.wait_ge(dsems[q][s], 16 * c)
            return body

        for q in QUEUES:
            getattr(block, engs[q])(make(q))


def _prod(s):
    r = 1
    for v in s:
        r *= v
    return r


ARENA_WORDS = 52224


class KB:
    def __init__(self):
        self.nc = bass.Bass("TRN2", target_bir_lowering=False)
        self.es = ExitStack()
        self.S = Sched()
        nc = self.nc
        self.arena = self.es.enter_context(nc.sbuf_tensor("arena", [128, ARENA_WORDS], F32))
        self.off = 0
        self.ps = [self.es.enter_context(nc.psum_tensor("ps%d" % i, [128, 512], F32)) for i in range(8)]
        self.sems = {q: self.es.enter_context(nc.semaphore("s_" + q)) for q in QUEUES}
        self.dsems = {q: [self.es.enter_context(nc.semaphore("d_%s%d" % (q, i))) for i in range(DMA_SLOTS)]
                      for q in QUEUES}
        self.dsems["cc"] = [self.es.enter_context(nc.semaphore("cc%d" % i)) for i in range(20)]
        self.nbank = 0
        self.uid = 0

    def alloc(self, free_shape, dt):
        n = _prod(free_shape)
        esz = 4 if dt in (F32, I32) else 2
        words = (n * esz + 3) // 4
        words = (words + 7) // 8 * 8
        assert self.off + words <= ARENA_WORDS, ("arena overflow", self.off, words)
        a = self.arena[:, self.off:self.off + words]
        self.off += words
        if esz == 2:
            a = a.bitcast(dt)
        elif dt != F32:
            a = a.bitcast(dt)
        a = a[:, 0:n]
        if len(free_shape) > 1:
            names = ["a%d" % i for i in range(len(free_shape))]
            kw = {nm: v for nm, v in zip(names, free_shape)}
            a = a.rearrange("p (%s) -> p %s" % (" ".join(names), " ".join(names)), **kw)
        return a

    def mark(self):
        return self.off

    def release(self, m):
        self.off = m

    def bank(self):
        i = self.nbank % 8
        self.nbank += 1
        return i

    def din(self, name, shape, dt=F32):
        return self.nc.dram_tensor(name, list(shape), dt, kind="ExternalInput").ap()

    def dout(self, name, shape, dt=F32):
        return self.nc.dram_tensor(name, list(shape), dt, kind="ExternalOutput").ap()

    def dtmp(self, name, shape, dt=F32):
        return self.nc.dram_tensor(name, list(shape), dt, kind="Internal").ap()

    def finish(self):
        with self.nc.Block() as block:
            self.S.emit(self.sems, self.dsems, block)
        self.es.close()
        return self.nc

    def MM(self, out, lhsT, rhs, st, sp, r, w):
        self.S.op("pe", lambda e: e.matmul(out, lhsT=lhsT, rhs=rhs, start=st, stop=sp), r, w)

    def TR(self, out, in_, idn, r, w):
        self.S.op("pe", lambda e: e.transpose(out=out, in_=in_, identity=idn), r, w)

    def ACT(self, out, in_, func, r, w, **kw):
        self.S.op("act", lambda e: e.activation(out=out, in_=in_, func=func, **kw), r, w)

    def TT(self, q, out, a, b, op, r, w):
        self.S.op(q, lambda e: e.tensor_tensor(out=out, in0=a, in1=b, op=op), r, w)

    def TS(self, q, out, a, s1, op0, r, w, s2=None, op1=None, accum=None):
        if op1 is None:
            self.S.op(q, lambda e: e.tensor_scalar(out=out, in0=a, scalar1=s1, scalar2=None, op0=op0), r, w)
        elif accum is None:
            self.S.op(q, lambda e: e.tensor_scalar(out=out, in0=a, scalar1=s1, scalar2=s2, op0=op0, op1=op1), r, w)
        else:
            self.S.op(q, lambda e: e.tensor_scalar(out=out, in0=a, scalar1=s1, scalar2=s2, op0=op0, op1=op1,
                                                   accum_out=accum), r, w)

    def STT(self, out, a, sc, b, op0, op1, r, w, accum=None):
        if accum is None:
            self.S.op("dve", lambda e: e.scalar_tensor_tensor(out=out, in0=a, scalar=sc, in1=b, op0=op0, op1=op1), r, w)
        else:
            self.S.op("dve", lambda e: e.scalar_tensor_tensor(out=out, in0=a, scalar=sc, in1=b, op0=op0, op1=op1,
                                                              accum_out=accum), r, w)

    def CP(self, q, out, in_, r, w):
        if q == "act":
            self.S.op(q, lambda e: e.copy(out=out, in_=in_), r, w)
        else:
            self.S.op(q, lambda e: e.tensor_copy(out=out, in_=in_), r, w)

    def DMA(self, q, out, in_, r, w):
        self.S.op(q, lambda e: e.dma_start(out=out, in_=in_), r, w, dma=True)

    def MEMSET(self, q, out, val, w):
        self.S.op(q, lambda e: e.memset(out, val), (), w)

    def RECIP(self, out, in_, r, w):
        self.S.op("dve", lambda e: e.reciprocal(out=out, in_=in_), r, w)

    def key(self, base):
        self.uid += 1
        return (base, self.uid)

    def consts(self):
        self.identf = self.alloc((128,), F32)
        self.identb = self.alloc((128,), BF16)
        self.eps = self.alloc((1,), F32)
        self.MEMSET("pool", self.identf, 0.0, ["identf"])
        idf = self.identf
        self.S.op("pool", lambda e: e.affine_select(out=idf, in_=idf, pattern=[[1, 128]], compare_op=ALU.not_equal,
                                                    fill=1.0, base=0, channel_multiplier=-1), ["identf"], ["identf"])
        self.CP("dve", self.identb, self.identf, ["identf"], ["identb"])
        self.MEMSET("pool", self.eps, 1e-6, ["eps"])


def ada_phase(K, crep, ada_w, ada_b, ada_dram):
    m = K.mark()
    ada = K.alloc((6144,), F32)
    csil = K.alloc((8, 128), F32)
    K.DMA("sp", csil, crep, [], ["csil"])
    K.ACT(csil, csil, AF.Silu, ["csil"], ["csil"])
    wch = [K.alloc((8, 512), F32) for _ in range(2)]
    wv = ada_w.rearrange("(kc p) n -> p kc n", p=128)
    for j in range(12):
        sl = slice(j * 512, (j + 1) * 512)
        K.DMA("sp", ada[:, sl], ada_b[sl].partition_broadcast(128), [], [("ada", j)])
    for j in range(12):
        wb = wch[j % 2]
        wk = ("adaw", j % 2)
        K.DMA("sp", wb, wv[:, :, j * 512:(j + 1) * 512], [], [wk])
        b = K.bank()
        for kc in range(8):
            K.MM(K.ps[b][:], csil[:, kc, :], wb[:, kc, :], kc == 0, kc == 7, ["csil", wk], [("ps", b)])
        sl = slice(j * 512, (j + 1) * 512)
        K.TT("dve", ada[:, sl], K.ps[b][:], ada[:, sl], ALU.add, [("ps", b), ("ada", j)], [("ada", j)])
    K.DMA("sp", ada_dram, ada, [("ada", j) for j in range(12)], ["ada_dram"])
    K.S.barrier()
    K.release(m)


def load_mod(K, ada_dram, which, norm_g, want_gate=True):
    base = 0 if which == 0 else 3
    k = K.key("mod")
    gt = None
    if want_gate:
        gt = K.alloc((1024,), F32)
        K.DMA("sp", gt, ada_dram[:, (base + 2) * 1024:(base + 3) * 1024], ["ada_dram"], [(k, "g")])
    return gt, (k, "g"), base, k


def load_mod2(K, ada_dram, base, k, norm_g):
    sh = K.alloc((1024,), F32)
    gs = K.alloc((1024,), F32)
    tmp = K.alloc((1024,), F32)
    K.DMA("sp", sh, ada_dram[:, (base + 0) * 1024:(base + 1) * 1024], ["ada_dram"], [(k, "sh")])
    K.DMA("sp", tmp, ada_dram[:, (base + 1) * 1024:(base + 2) * 1024], ["ada_dram"], [(k, "sc")])
    K.DMA("sp", gs, norm_g.partition_broadcast(128), [], [(k, "gs")])
    K.STT(gs, tmp, 1.0, gs, ALU.add, ALU.mult, [(k, "sc"), (k, "gs")], [(k, "gs")])
    return gs, sh, (k, "gs"), (k, "sh")


def moe_phase(K, x, xkey, ada_dram, P, nexp=32, dbg_gates=None):
    S = K.S
    m0 = K.mark()
    g2, kg2, mbase, mk = load_mod(K, ada_dram, 1, P["norm2_g"])
    h2T = K.alloc((8, 2048), BF16)
    gates = K.alloc((16, 32), F32)
    wtsT = K.alloc((2048,), BF16)
    bgu = K.alloc((32, 16), F32)
    bdb = K.alloc((1024,), BF16)
    K.DMA("sp", bgu, P["bguT"], [], ["bgu"])
    m1 = K.mark()
    gs2, sh2, kgs, ksh = load_mod2(K, ada_dram, mbase, mk, P["norm2_g"])
    wr = K.alloc((8, 32), F32)
    brt = K.alloc((32,), F32)
    bdf = K.alloc((1024,), F32)
    stat = K.alloc((16,), F32)
    junk = K.alloc((1024,), F32)
    K.DMA("sp", wr, P["w_router"].rearrange("(kc p) e -> p kc e", p=128), [], ["wr"])
    K.DMA("sp", brt, P["b_router"].partition_broadcast(128), [], ["brt"])
    K.DMA("sp", bdf[0:32, :], P["b_down"], [], ["bdf"])
    K.TT("dve", bdb[0:32, :], bdf[0:32, :], g2[0:32, :], ALU.mult, ["bdf", kg2], ["bdb"])
    for s in range(16):
        K.ACT(junk, x[:, s, :], AF.Square, [xkey(s)], ["junk", "stat"], accum_out=stat[:, s:s + 1])
    K.ACT(stat, stat, AF.Sqrt, ["stat"], ["stat"], scale=1.0 / 1024, bias=K.eps[:, 0:1])
    K.RECIP(stat, stat, ["stat"], ["stat"])
    tmpf = [K.alloc((1024,), F32) for _ in range(2)]
    h2f = [K.alloc((1024,), F32) for _ in range(2)]
    hTs = [K.alloc((8, 128), F32) for _ in range(2)]
    gt = [K.alloc((160,), F32) for _ in range(2)]
    for s in range(16):
        i = s % 2
        K.STT(tmpf[i], x[:, s, :], stat[:, s:s + 1], gs2, ALU.mult, ALU.mult, [xkey(s), "stat", kgs], [("tmpf", i)])
        K.TT("pool", h2f[i], tmpf[i], sh2, ALU.add, [("tmpf", i), ksh], [("h2f", i)])
        b0 = K.bank()
        b1 = K.bank()
        for kc in range(8):
            b = b0 if kc < 4 else b1
            K.TR(K.ps[b][:, (kc % 4) * 128:(kc % 4 + 1) * 128], h2f[i][:, kc * 128:(kc + 1) * 128], K.identf,
                 [("h2f", i), "identf"], [("ps", b)])
        for hh, b in enumerate((b0, b1)):
            src = K.ps[b][:].rearrange("p (a n) -> p a n", a=4)
            K.CP("dve", hTs[i][:, hh * 4:(hh + 1) * 4, :], src, [("ps", b)], [("hTs", i)])
        K.CP("act", h2T[:, :, s * 128:(s + 1) * 128], hTs[i], [("hTs", i)], [("h2T", s)])
        bl = K.bank()
        for kc in range(8):
            K.MM(K.ps[bl][:, 0:32], hTs[i][:, kc, :], wr[:, kc, :], kc == 0, kc == 7, [("hTs", i), "wr"], [("ps", bl)])
        g = gt[i]
        gk = ("gt", i)
        lg, m8, ex, em = g[:, 0:32], g[:, 32:40], g[:, 64:96], g[:, 96:128]
        negm, ssum, mask = g[:, 40:41], g[:, 41:42], g[:, 128:160]
        K.TT("dve", lg, K.ps[bl][:, 0:32], brt, ALU.add, [("ps", bl), "brt"], [gk])
        S.op("dve", lambda e, m8=m8, lg=lg: e.max(out=m8, in_=lg), [gk], [gk])
        K.TS("dve", negm, m8[:, 0:1], -1.0, ALU.mult, [gk], [gk])
        K.TS("dve", mask, lg, m8[:, 3:4], ALU.is_ge, [gk], [gk])
        K.ACT(ex, lg, AF.Exp, [gk], [gk], bias=negm, scale=1.0)
        K.STT(em, ex, 1.0, mask, ALU.mult, ALU.mult, [gk], [gk], accum=ssum)
        K.RECIP(ssum, ssum, [gk], [gk])
        K.TS("dve", gates[:, s, :], em, ssum, ALU.mult, [gk], [("gates", s)])
    for q4 in range(4):
        b = K.bank()
        for j in range(4):
            s = q4 * 4 + j
            K.TR(K.ps[b][0:32, j * 128:(j + 1) * 128], gates[:, s, :], K.identf, [("gates", s), "identf"], [("ps", b)])
        K.CP("act", wtsT[0:32, q4 * 512:(q4 + 1) * 512], K.ps[b][0:32, :], [("ps", b)], ["wtsT"])
    if dbg_gates is not None:
        K.DMA("sp", dbg_gates, gates, [("gates", s) for s in range(16)], ["dbg_gates"])
    K.S.barrier()
    K.release(m1)
    actT = K.alloc((8, 2048), BF16)
    NW = 2
    wgu_t = [K.alloc((8, 2, 256), BF16) for _ in range(NW)]
    wdn_t = [K.alloc((8, 512), BF16) for _ in range(2)]
    gsb = [K.alloc((512,), F32) for _ in range(2)]
    ssb = [K.alloc((512,), BF16) for _ in range(2)]
    usb = [K.alloc((512,), F32) for _ in range(2)]
    tdn = [K.alloc((512,), F32) for _ in range(2)]
    wguv = P["w_gate_up"]
    wdv = P["w_down"]
    chunks = []
    for e in range(nexp):
        for q in range(4):
            chunks.append(("gu", e, q))
        for dh in range(2):
            chunks.append(("dn", e, dh))
    cnt = {"gu": 0, "dn": 0}
    slot_of = {}

    def issue(ci):
        kind, e, q = chunks[ci]
        i = cnt[kind]
        cnt[kind] += 1
        if kind == "gu":
            wb = wgu_t[i % NW]
            wk = ("wgu", i % NW)
            srcv = wguv[e].rearrange("(kc p) f -> p kc f", p=128)
            K.DMA("pool", wb[:, :, 0, :], srcv[:, :, q * 256:(q + 1) * 256], [], [wk])
            K.DMA("pool", wb[:, :, 1, :], srcv[:, :, 1024 + q * 256:1024 + (q + 1) * 256], [], [wk])
            slot_of[ci] = (wb, wk)
            return
        if True:
            wb = wdn_t[i % 2]
            wk = ("wdn", i % 2)
            src = wdv[e].rearrange("(fc p) d -> p fc d", p=128)[:, :, q * 512:(q + 1) * 512]
        K.DMA("pool", wb, src, [], [wk])
        slot_of[ci] = (wb, wk)

    if chunks:
        issue(0)
    ep = 0
    dcnt = 0
    for ci, (kind, e, q) in enumerate(chunks):
        if ci + 1 < len(chunks):
            issue(ci + 1)
        wb, wk = slot_of.pop(ci)
        if kind == "gu":
            for ft in range(2):
                fcol = q * 2 + ft
                for tt in range(4):
                    bg = K.bank()
                    bu = K.bank()
                    rk = [wk] + [("h2T", s) for s in range(tt * 4, tt * 4 + 4)]
                    for kc in range(8):
                        K.MM(K.ps[bg][:], wb[:, kc, 0, ft * 128:(ft + 1) * 128], h2T[:, kc, tt * 512:(tt + 1) * 512],
                             kc == 0, kc == 7, rk, [("ps", bg)])
                    for kc in range(8):
                        K.MM(K.ps[bu][:], wb[:, kc, 1, ft * 128:(ft + 1) * 128], h2T[:, kc, tt * 512:(tt + 1) * 512],
                             kc == 0, kc == 7, rk, [("ps", bu)])
                    i = ep % 2
                    ep += 1
                    K.TS("dve", gsb[i], K.ps[bg][:], bgu[:, e, fcol:fcol + 1], ALU.add, [("ps", bg), "bgu"],
                         [("gsb", i)], s2=7.0, op1=ALU.min)
                    K.ACT(ssb[i], gsb[i], AF.Sigmoid, [("gsb", i)], [("ssb", i)], scale=1.702)
                    K.TS("dve", usb[i], K.ps[bu][:], bgu[:, e, 8 + fcol:9 + fcol], ALU.add, [("ps", bu), "bgu"],
                         [("usb", i)], s2=7.0, op1=ALU.min)
                    K.TS("dve", usb[i], usb[i], -7.0, ALU.max, [("usb", i)], [("usb", i)], s2=1.0, op1=ALU.add)
                    K.TT("pool", gsb[i], gsb[i], ssb[i], ALU.mult, [("gsb", i), ("ssb", i)], [("gsb", i)])
                    K.TT("pool", actT[:, fcol, tt * 512:(tt + 1) * 512], usb[i], gsb[i], ALU.mult,
                         [("usb", i), ("gsb", i)], [("actT", fcol, tt)])
        else:
            dh = q
            for ts in range(16):
                b = K.bank()
                for fc in range(8):
                    K.MM(K.ps[b][:], actT[:, fc, ts * 128:(ts + 1) * 128], wb[:, fc, :], fc == 0, fc == 7,
                         [("actT", fc, ts // 4), wk], [("ps", b)])
                i = dcnt % 2
                dcnt += 1
                K.STT(tdn[i], K.ps[b][:], gates[:, ts, e:e + 1], g2[:, dh * 512:(dh + 1) * 512], ALU.mult, ALU.mult,
                      [("ps", b), ("gates", ts), kg2], [("tdn", i)])
                xs = x[:, ts, dh * 512:(dh + 1) * 512]
                K.TT("pool", xs, xs, tdn[i], ALU.add, [("tdn", i), xkey(ts)], [xkey(ts)])
    for ts in range(16):
        for dh in range(2):
            b = K.bank()
            K.MM(K.ps[b][:], wtsT[0:32, ts * 128:(ts + 1) * 128], bdb[0:32, dh * 512:(dh + 1) * 512], True, True,
                 ["wtsT", "bdb"], [("ps", b)])
            xs = x[:, ts, dh * 512:(dh + 1) * 512]
            K.TT("dve", xs, K.ps[b][:], xs, ALU.add, [("ps", b), xkey(ts)], [xkey(ts)])
    K.S.barrier()
    K.release(m0)


TWO_PI = 2.0 * math.pi
CW1 = 6.28125
CW2 = TWO_PI - 6.28125


def sincos(K, theta, sin_o, cos_o, shape, kin, kout):
    tf = K.alloc(shape, F32)
    ti = K.alloc(shape, I32)
    kf = K.alloc(shape, F32)
    r = K.alloc(shape, F32)
    y = K.alloc(shape, F32)
    mk = K.alloc(shape, F32)
    k = K.key("sc")
    K.TS("dve", tf, theta, 1.0 / TWO_PI, ALU.mult, [kin], [(k, 0)], s2=0.5, op1=ALU.add)
    K.CP("dve", ti, tf, [(k, 0)], [(k, 1)])
    K.CP("dve", kf, ti, [(k, 1)], [(k, 2)])
    K.STT(r, kf, -CW1, theta, ALU.mult, ALU.add, [(k, 2), kin], [(k, 3)])
    K.STT(r, kf, -CW2, r, ALU.mult, ALU.add, [(k, 2), (k, 3)], [(k, 3)])
    for shift, outp in ((0.0, sin_o), (math.pi / 2, cos_o)):
        K.TS("dve", y, r, shift, ALU.add, [(k, 3)], [(k, 4)])
        K.TS("dve", mk, y, math.pi, ALU.is_gt, [(k, 4)], [(k, 5)])
        K.STT(y, mk, -TWO_PI, y, ALU.mult, ALU.add, [(k, 5), (k, 4)], [(k, 4)])
        K.TS("dve", mk, y, -math.pi, ALU.is_lt, [(k, 4)], [(k, 5)])
        K.STT(y, mk, TWO_PI, y, ALU.mult, ALU.add, [(k, 5), (k, 4)], [(k, 4)])
        K.ACT(outp, y, AF.Sin, [(k, 4)], [kout])


def bc_last(ap, n):
    return bass.AP(tensor=ap.tensor, offset=ap.offset, ap=[list(d) for d in ap.ap] + [[0, n]])


def s5_tables(K, P, Cs, Mt, Bs, C1, C2):
    m = K.mark()
    kk = "s5t"
    lam_r = K.alloc((64,), F32)
    lam_i = K.alloc((64,), F32)
    ldt = K.alloc((64,), F32)
    bPr = K.alloc((64, 16), F32)
    bPi = K.alloc((64, 16), F32)
    cPr = K.alloc((64, 16), F32)
    cPi = K.alloc((64, 16), F32)
    dS = K.alloc((64,), F32)
    for t, nm in ((lam_r, "lamP_re"), (lam_i, "lamP_im"), (ldt, "logdtP"), (bPr, "bP_re"), (bPi, "bP_im"),
                  (cPr, "cP_re"), (cPi, "cP_im"), (dS, "dS")):
        K.DMA("sp", t, P[nm], [], [kk])
    dt = K.alloc((64,), F32)
    lr = K.alloc((64,), F32)
    li = K.alloc((64,), F32)
    mag = K.alloc((64,), F32)
    imag = K.alloc((64,), F32)
    sn = K.alloc((64,), F32)
    cs = K.alloc((64,), F32)
    t1 = K.alloc((64,), F32)
    t2 = K.alloc((64,), F32)
    K.ACT(dt, ldt, AF.Exp, [kk], [kk])
    K.TT("dve", lr, lam_r, dt, ALU.mult, [kk], [kk])
    K.TT("dve", li, lam_i, dt, ALU.mult, [kk], [kk])
    for outp, sg in ((mag, 1.0), (imag, -1.0)):
        K.TS("dve", outp, lr, sg / 6.0, ALU.mult, [kk], [kk], s2=1.0, op1=ALU.add)
        for dnm in (5.0, 4.0, 3.0, 2.0, 1.0):
            K.TT("dve", outp, outp, lr, ALU.mult, [kk], [kk])
            K.TS("dve", outp, outp, sg / dnm, ALU.mult, [kk], [kk], s2=1.0, op1=ALU.add)
    sincos(K, li, sn, cs, (64,), kk, kk)
    apr = K.alloc((9, 64), F32)
    api = K.alloc((9, 64), F32)
    ivr = K.alloc((8, 64), F32)
    ivi = K.alloc((8, 64), F32)
    K.MEMSET("dve", apr[:, 0, :], 1.0, [kk])
    K.MEMSET("dve", api[:, 0, :], 0.0, [kk])
    K.MEMSET("dve", ivr[:, 0, :], 1.0, [kk])
    K.MEMSET("dve", ivi[:, 0, :], 0.0, [kk])
    K.TT("dve", apr[:, 1, :], mag, cs, ALU.mult, [kk], [kk])
    K.TT("dve", api[:, 1, :], mag, sn, ALU.mult, [kk], [kk])
    K.TT("dve", ivr[:, 1, :], imag, cs, ALU.mult, [kk], [kk])
    K.TT("dve", t1, imag, sn, ALU.mult, [kk], [kk])
    K.TS("dve", ivi[:, 1, :], t1, -1.0, ALU.mult, [kk], [kk])

    def cmul(orr, oi, ar, ai, br, bi, ta, tb, q="dve"):
        K.TT(q, ta, ar, br, ALU.mult, [kk], [kk])
        K.TT(q, tb, ai, bi, ALU.mult, [kk], [kk])
        K.TT(q, orr, ta, tb, ALU.subtract, [kk], [kk])
        K.TT(q, ta, ar, bi, ALU.mult, [kk], [kk])
        K.TT(q, tb, ai, br, ALU.mult, [kk], [kk])
        K.TT(q, oi, ta, tb, ALU.add, [kk], [kk])

    for t in range(2, 9):
        cmul(apr[:, t, :], api[:, t, :], apr[:, t - 1, :], api[:, t - 1, :], apr[:, 1, :], api[:, 1, :], t1, t2)
    for t in range(2, 8):
        cmul(ivr[:, t, :], ivi[:, t, :], ivr[:, t - 1, :], ivi[:, t - 1, :], ivr[:, 1, :], ivi[:, 1, :], t1, t2)
    for h in range(2):
        rows = slice(64 * h, 64 * h + 64)
        gsl = slice(32 * h, 32 * h + 32)
        K.CP("dve", C1[rows, 0, :], apr[rows, 8, gsl], [kk], [kk])
        K.CP("dve", C1[rows, 1, :], apr[rows, 8, gsl], [kk], [kk])
        K.TS("dve", C2[rows, 0, :], api[rows, 8, gsl], -1.0, ALU.mult, [kk], [kk])
        K.CP("dve", C2[rows, 1, :], api[rows, 8, gsl], [kk], [kk])
    den = K.alloc((64,), F32)
    fr = K.alloc((64,), F32)
    fi = K.alloc((64,), F32)
    am1 = K.alloc((64,), F32)
    K.TT("dve", den, lam_r, lam_r, ALU.mult, [kk], [kk])
    K.TT("dve", t1, lam_i, lam_i, ALU.mult, [kk], [kk])
    K.TT("dve", den, den, t1, ALU.add, [kk], [kk])
    K.RECIP(den, den, [kk], [kk])
    K.TS("dve", am1, apr[:, 1, :], -1.0, ALU.add, [kk], [kk])
    K.TT("dve", t1, am1, lam_r, ALU.mult, [kk], [kk])
    K.TT("dve", t2, api[:, 1, :], lam_i, ALU.mult, [kk], [kk])
    K.TT("dve", t1, t1, t2, ALU.add, [kk], [kk])
    K.TT("dve", fr, t1, den, ALU.mult, [kk], [kk])
    K.TT("dve", t1, api[:, 1, :], lam_r, ALU.mult, [kk], [kk])
    K.TT("dve", t2, am1, lam_i, ALU.mult, [kk], [kk])
    K.TT("dve", t1, t1, t2, ALU.subtract, [kk], [kk])
    K.TT("dve", fi, t1, den, ALU.mult, [kk], [kk])
    bbr = K.alloc((64, 16), F32)
    bbi = K.alloc((64, 16), F32)
    u1 = K.alloc((64, 16), F32)
    u2 = K.alloc((64, 16), F32)
    frb, fib = bc_last(fr, 16), bc_last(fi, 16)
    cmul(bbr, bbi, frb, fib, bPr, bPi, u1, u2)
    for h in range(2):
        rows = slice(64 * h, 64 * h + 64)
        gsl = slice(32 * h, 32 * h + 32)
        for tau in range(8):
            pr = bc_last(apr[rows, tau + 1, gsl], 16)
            pi = bc_last(api[rows, tau + 1, gsl], 16)
            a1 = u1[rows, 0:32, :]
            a2 = u2[rows, 0:32, :]
            K.TT("dve", a1, cPr[rows, gsl, :], pr, ALU.mult, [kk], [kk])
            K.TT("dve", a2, cPi[rows, gsl, :], pi, ALU.mult, [kk], [kk])
            K.TT("dve", Cs[rows, 0, :, tau, :], a1, a2, ALU.subtract, [kk], ["Cs"])
            K.TT("dve", a1, cPr[rows, gsl, :], pi, ALU.mult, [kk], [kk])
            K.TT("dve", a2, cPi[rows, gsl, :], pr, ALU.mult, [kk], [kk])
            K.STT(Cs[rows, 1, :, tau, :], a1, -1.0, a2, ALU.mult, ALU.subtract, [kk], ["Cs"])
    Kk2f = K.alloc((64, 128), F32)
    Q2f = K.alloc((64, 128), F32)
    BsPf = K.alloc((64, 128), F32)
    Kk2 = Kk2f.rearrange("p g (s c) -> p g s c", s=8)
    Q2 = Q2f.rearrange("p g (s c) -> p g s c", s=8)
    BsP = BsPf.rearrange("p g (s c) -> p g s c", s=8)
    lo = slice(0, 64)
    hi = slice(64, 128)
    for s in range(8):
        for (dst, pw_r, pw_i) in ((Kk2, ivr[:, s, :], ivi[:, s, :]), (BsP, apr[:, 7 - s, :], api[:, 7 - s, :])):
            K.TT("dve", u1[lo], bbr[lo], bc_last(pw_r[lo], 16), ALU.mult, [kk], [kk])
            K.TT("dve", u2[lo], bbi[lo], bc_last(pw_i[lo], 16), ALU.mult, [kk], [kk])
            K.TT("dve", dst[lo, :, s, :], u1[lo], u2[lo], ALU.subtract, [kk], [kk])
            K.TT("dve", u1[hi], bbi[hi], bc_last(pw_r[hi], 16), ALU.mult, [kk], [kk])
            K.TT("dve", u2[hi], bbr[hi], bc_last(pw_i[hi], 16), ALU.mult, [kk], [kk])
            K.TT("dve", dst[hi, :, s, :], u1[hi], u2[hi], ALU.add, [kk], [kk])
        pw_r, pw_i = apr[:, s, :], api[:, s, :]
        K.TT("dve", u1[lo], cPr[lo], bc_last(pw_r[lo], 16), ALU.mult, [kk], [kk])
        K.TT("dve", u2[lo], cPi[lo], bc_last(pw_i[lo], 16), ALU.mult, [kk], [kk])
        K.TT("dve", Q2[lo, :, s, :], u1[lo], u2[lo], ALU.subtract, [kk], [kk])
        K.TT("dve", u1[hi], cPr[hi], bc_last(pw_i[hi], 16), ALU.mult, [kk], [kk])
        K.TT("dve", u2[hi], cPi[hi], bc_last(pw_r[hi], 16), ALU.mult, [kk], [kk])
        K.TT("dve", u1[hi], u1[hi], u2[hi], ALU.add, [kk], [kk])
        K.TS("dve", Q2[hi, :, s, :], u1[hi], -1.0, ALU.mult, [kk], [kk])
    msk = K.alloc((8, 16), F32)
    K.MEMSET("pool", msk, 1.0, [kk])
    K.S.op("pool", lambda e: e.affine_select(out=msk, in_=msk, pattern=[[16, 8], [0, 16]], compare_op=ALU.is_ge,
                                             fill=0.0, base=15, channel_multiplier=-1), [kk], [kk])
    mtmp = K.alloc((4, 128), F32)
    for gq in range(16):
        b = K.bank()
        for j in range(4):
            g = gq * 4 + j
            K.MM(K.ps[b][:, j * 128:(j + 1) * 128], Kk2f[:, g, :], Q2f[:, g, :], True, True, [kk], [("ps", b)])
        mskb = bass.AP(tensor=msk.tensor, offset=msk.offset, ap=[list(msk.ap[0]), [0, 4], [1, 128]])
        K.TT("dve", mtmp, K.ps[b][:].rearrange("p (j n) -> p j n", j=4), mskb, ALU.mult, [("ps", b), kk], ["mtmp"])
        for j in range(4):
            g = gq * 4 + j
            K.STT(Mt[:, g, :], K.identf, dS[:, g:g + 1], mtmp[:, j, :], ALU.mult, ALU.add, ["mtmp", kk, "identf"],
                  ["Mt"])
    for gq in range(16):
        b = K.bank()
        for j in range(4):
            g = gq * 4 + j
            K.TR(K.ps[b][:, j * 128:(j + 1) * 128], BsPf[:, g, :], K.identf, [kk, "identf"], [("ps", b)])
        K.CP("act", Bs[:, gq * 4:(gq + 1) * 4, :, :], K.ps[b][:].rearrange("p (j r q) -> p j r q", j=4, r=2),
             [("ps", b)], ["Bs"])
    K.S.barrier()
    K.release(m)


def s5_exchange(K, Z, C1, C2, selm, st_l, st_g, groups):
    kk = "xch"
    m = K.mark()
    K.DMA("sp", st_l, Z.rearrange("p a b -> p (a b)"), [("Zf", 0), ("Zf", 1)], ["st_l"])
    K.S.op("pool", lambda e: e.collective_compute("AllGather", ALU.bypass, replica_groups=groups, ins=[st_l],
                                                  outs=[st_g]), ["st_l"], ["st_g"], coll=True)
    G = K.alloc((4, 64), F32)
    K.DMA("sp", G, st_g.rearrange("(r p) c -> p r c", r=4), ["st_g"], [kk])
    Pk1 = K.alloc((3, 64), F32)
    Pk2 = K.alloc((3, 64), F32)
    t1 = K.alloc((64,), F32)
    t2 = K.alloc((64,), F32)
    c1f = C1.rearrange("p a b -> p (a b)")
    c2f = C2.rearrange("p a b -> p (a b)")
    K.MEMSET("dve", Pk1[:, 0, :], 1.0, [kk])
    K.MEMSET("dve", Pk2[:, 0, :], 0.0, [kk])
    K.CP("dve", Pk1[:, 1, :], c1f, [kk], [kk])
    K.CP("dve", Pk2[:, 1, :], c2f, [kk], [kk])

    def sq(a1, a2, o1, o2):
        K.TT("dve", t1, a1, a1, ALU.mult, [kk], [kk])
        K.TT("dve", t2, a2, a2, ALU.mult, [kk], [kk])
        K.TT("dve", t2, t1, t2, ALU.subtract, [kk], [kk])
        K.TT("dve", t1, a1, a2, ALU.mult, [kk], [kk])
        K.TS("dve", o2, t1, 2.0, ALU.mult, [kk], [kk])
        K.CP("dve", o1, t2, [kk], [kk])

    for _ in range(8):
        sq(Pk1[:, 1, :], Pk2[:, 1, :], Pk1[:, 1, :], Pk2[:, 1, :])
    sq(Pk1[:, 1, :], Pk2[:, 1, :], Pk1[:, 2, :], Pk2[:, 2, :])
    acc = K.alloc((64,), F32)
    cc1 = K.alloc((64,), F32)
    cc2 = K.alloc((64,), F32)
    K.MEMSET("dve", acc, 0.0, [kk])
    for r in range(4):
        K.TS("dve", cc1, Pk1[:, 0, :], selm[:, r * 3:r * 3 + 1], ALU.mult, [kk], [kk])
        K.TS("dve", cc2, Pk2[:, 0, :], selm[:, r * 3:r * 3 + 1], ALU.mult, [kk], [kk])
        for mm in (1, 2):
            K.STT(cc1, Pk1[:, mm, :], selm[:, r * 3 + mm:r * 3 + mm + 1], cc1, ALU.mult, ALU.add, [kk], [kk])
            K.STT(cc2, Pk2[:, mm, :], selm[:, r * 3 + mm:r * 3 + mm + 1], cc2, ALU.mult, ALU.add, [kk], [kk])
        g = G[:, r, :]
        gsw = bass.AP(tensor=g.tensor, offset=g.offset + 32, ap=[list(g.ap[0]), [-32, 2], [1, 32]])
        K.TT("dve", t1, cc1, g, ALU.mult, [kk], [kk])
        K.TT("dve", t2.rearrange("p (a b) -> p a b", a=2), cc2.rearrange("p (a b) -> p a b", a=2), gsw, ALU.mult,
             [kk], [kk])
        K.TT("dve", t1, t1, t2, ALU.add, [kk], [kk])
        K.TT("dve", acc, acc, t1, ALU.add, [kk], [kk])
    K.CP("dve", Z.rearrange("p a b -> p (a b)"), acc, [kk], [("Zf", 0), ("Zf", 1)])
    K.S.barrier()
    K.release(m)


def phase_S5(K, P, xmid, ada_dram, st_l, st_g, groups):
    S = K.S
    mT = K.mark()
    Csf = K.alloc((2, 32, 128), BF16)
    Cs = Csf.rearrange("p r g (t c) -> p r g t c", t=8)
    Mt = K.alloc((64, 128), BF16)
    Bs = K.alloc((64, 2, 64), BF16)
    C1 = K.alloc((2, 32), F32)
    C2 = K.alloc((2, 32), F32)
    mS5 = K.mark()
    ada_phase(K, P["crep"], P["ada_w"], P["ada_b"], ada_dram)
    s5_tables(K, P, Cs, Mt, Bs, C1, C2)
    w_in = K.alloc((8, 1024), BF16)
    w_glu = K.alloc((8, 1024), BF16)
    w_out = K.alloc((8, 1024), BF16)
    K.DMA("pool", w_in, P["w_in"].rearrange("(kc p) n -> p kc n", p=128), [], ["w_in"])
    K.DMA("pool", w_glu, P["w_glu"].rearrange("(kc p) n -> p kc n", p=128), [], ["w_glu"])
    mw = K.mark()
    g1, kg1, mbase, mk = load_mod(K, ada_dram, 0, P["norm1_g"])
    wst = K.alloc((8, 1024), F32)
    K.DMA("sp", wst, P["w_out"].rearrange("(kc p) n -> p kc n", p=128), [], ["wst"])
    for kc in range(8):
        K.TT("dve" if kc % 2 else "pool", w_out[:, kc, :], wst[:, kc, :], g1, ALU.mult, ["wst", kg1], ["w_out"])
    K.S.barrier()
    K.release(mw)
    gs1, sh1, kgs, ksh = load_mod2(K, ada_dram, mbase, mk, P["norm1_g"])
    selm = K.alloc((12,), F32)
    K.DMA("sp", selm, P["selm"], [], ["xch"])
    xrow = [K.alloc((1024,), F32) for _ in range(2)]
    xr2 = xrow
    tmpf = K.alloc((1024,), F32)
    junk = tmpf
    hrow = [K.alloc((1024,), BF16) for _ in range(2)]
    hTt = [K.alloc((8, 128), BF16) for _ in range(2)]
    zTt = [K.alloc((8, 128), BF16) for _ in range(2)]
    zgTt = [K.alloc((8, 128), BF16) for _ in range(2)]
    sgt = [K.alloc((512,), BF16) for _ in range(2)]
    xo = [K.alloc((512,), F32) for _ in range(2)]
    u = K.alloc((8, 1024), BF16)
    ug = u.rearrange("p t (g c) -> p (t g c)", g=64).rearrange("p (g s c) -> p g s c", g=64, s=8)
    ug2 = u.rearrange("p t c -> p (t c)").rearrange("p (g k) -> p g k", g=64)
    UT = K.alloc((64, 128), BF16)
    Sb = K.alloc((2, 32, 129), BF16)
    Zf = [K.alloc((2, 32), F32) for _ in range(2)]
    T1 = K.alloc((2, 32), F32)
    T2 = K.alloc((2, 32), F32)
    st = K.alloc((2,), F32)
    K.MEMSET("dve", Zf[0], 0.0, [("Zf", 0)])
    xsv = P["xs"].rearrange("(H n t) d -> H t n d", H=2, t=8)
    xmv = xmid.rearrange("(H n t) d -> H t n d", H=2, t=8)
    zcur = 0
    ukeys = [("u", t) for t in range(8)]
    for hs in range(4):
        pss, H = divmod(hs, 2)
        own = pss == 1
        if hs == 2:
            s5_exchange(K, Zf[zcur], C1, C2, selm, st_l, st_g, groups)
        for tau in range(8):
            i = tau % 2
            xr = xrow[i]
            K.DMA("sp", xr, xsv[H, tau], [], [("xrow", i)])
            K.ACT(junk, xr, AF.Square, [("xrow", i)], ["tmpf", "st"], accum_out=st[:, 0:1])
            K.ACT(st[:, 1:2], st[:, 0:1], AF.Sqrt, ["st"], ["st2"], scale=1.0 / 1024, bias=K.eps[:, 0:1])
            K.RECIP(st[:, 1:2], st[:, 1:2], ["st2"], ["st2"])
            K.STT(tmpf, xr, st[:, 1:2], gs1, ALU.mult, ALU.mult, [("xrow", i), "st2", kgs], ["tmpf"])
            K.TT("pool", hrow[i], tmpf, sh1, ALU.add, ["tmpf", ksh], [("hrow", i)])
            b = K.bank()
            psb = K.ps[b].bitcast(BF16)
            for kc in range(8):
                K.TR(psb[:, kc * 128:(kc + 1) * 128], hrow[i][:, kc * 128:(kc + 1) * 128], K.identb,
                     [("hrow", i), "identb"], [("ps", b)])
            K.CP("act", hTt[i], psb[:, 0:1024].rearrange("p (a n) -> p a n", a=8), [("ps", b)], [("hTt", i)])
            for ch in range(2):
                b = K.bank()
                for kc in range(8):
                    K.MM(K.ps[b][:], hTt[i][:, kc, :], w_in[:, kc, ch * 512:(ch + 1) * 512], kc == 0, kc == 7,
                         [("hTt", i), "w_in"], [("ps", b)])
                K.CP("dve" if ch else "act", ug[:, ch * 32:(ch + 1) * 32, tau, :],
                     K.ps[b][:].rearrange("p (g c) -> p g c", g=32), [("ps", b)], ukeys)
        for g8 in range(8):
            b = K.bank()
            psb = K.ps[b].bitcast(BF16)
            for j in range(8):
                g = g8 * 8 + j
                K.TR(psb[:, j * 128:(j + 1) * 128], ug2[:, g, :], K.identb, ukeys + ["identb"],
                     [("ps", b)])
            K.CP("act" if g8 % 2 else "dve", UT[:, g8 * 8:(g8 + 1) * 8, :],
                 psb[:, 0:1024].rearrange("p (a n) -> p a n", a=8), [("ps", b)], [("UT", g8)])
        for q in range(16):
            b = K.bank()
            for ri in range(2):
                for gp in range(2):
                    for h in range(2):
                        g = h * 32 + 2 * q + gp
                        c0 = (ri * 2 + gp) * 128
                        K.MM(K.ps[b][64 * h:64 * h + 64, c0:c0 + 128], Bs[:, g, ri, :], UT[:, g, :], True, True,
                             ["Bs", ("UT", g // 8)], [("ps", b)])
            K.ACT(Sb[:, :, 2 * q:2 * q + 2, 1:129], K.ps[b][:].rearrange("p (r g n) -> p r g n", r=2, g=2),
                  AF.Identity, [("ps", b)], ["Sb", "Sbx"])
        for n in range(128):
            zc = Zf[zcur]
            zn = Zf[1 - zcur]
            if own:
                K.CP("act", Sb[:, :, :, n], zc, [("Zf", zcur)], ["Sbx"])
            zsw = bass.AP(tensor=zc.tensor, offset=zc.offset + 32, ap=[list(zc.ap[0]), [-32, 2], [1, 32]])
            K.TT("dve", T1, C1, zc, ALU.mult, [("Zf", zcur)], ["T1"])
            K.TT("pool", T2, C2, zsw, ALU.mult, [("Zf", zcur)], ["T2"])
            K.TT("dve", T1, T1, T2, ALU.add, ["T1", "T2"], ["T1"])
            K.TT("dve", zn, T1, Sb[:, :, :, n + 1], ALU.add, ["T1", "Sb"], [("Zf", 1 - zcur)])
            zcur = 1 - zcur
        if not own:
            continue
        for gq in range(16):
            b = K.bank()
            for j in range(4):
                g = gq * 4 + j
                h, g32 = divmod(g, 32)
                rows = slice(64 * h, 64 * h + 64)
                o = K.ps[b][:, j * 128:(j + 1) * 128]
                K.MM(o, UT[:, g, :], Mt[:, g, :], True, False, [("UT", g // 8), "Mt"], [("ps", b)])
                K.MM(o, Sb[rows, 0, g32, 0:128], Csf[rows, 0, g32, :], False, False, ["Sbx", "Cs"], [("ps", b)])
                K.MM(o, Sb[rows, 1, g32, 0:128], Csf[rows, 1, g32, :], False, True, ["Sbx", "Cs"], [("ps", b)])
            zo = u[:, :, gq * 64:(gq + 1) * 64].rearrange("p t (j c) -> p j t c", j=4)
            K.ACT(zo, K.ps[b][:].rearrange("p (j t c) -> p j t c", j=4, t=8), AF.Gelu_apprx_tanh, [("ps", b)], ukeys)
        for tau in range(8):
            i = tau % 2
            b = K.bank()
            psb = K.ps[b].bitcast(BF16)
            for kc in range(8):
                K.TR(psb[:, kc * 128:(kc + 1) * 128], u[:, tau, kc * 128:(kc + 1) * 128], K.identb,
                     [("u", tau), "identb"], [("ps", b)])
            K.CP("act", zTt[i], psb[:, 0:1024].rearrange("p (a n) -> p a n", a=8), [("ps", b)], [("zTt", i)])
            for half in range(2):
                b = K.bank()
                for c4 in range(4):
                    co = half * 4 + c4
                    for kc in range(8):
                        K.MM(K.ps[b][:, c4 * 128:(c4 + 1) * 128], w_glu[:, kc, co * 128:(co + 1) * 128], zTt[i][:, kc, :],
                             kc == 0, kc == 7, ["w_glu", ("zTt", i)], [("ps", b)])
                K.ACT(sgt[half], K.ps[b][:], AF.Sigmoid, [("ps", b)], [("sgt", half)])
                K.TT("pool", zgTt[i][:, half * 4:(half + 1) * 4, :], zTt[i][:, half * 4:(half + 1) * 4, :],
                     sgt[half].rearrange("p (a n) -> p a n", a=4), ALU.mult, [("zTt", i), ("sgt", half)],
                     [("zgTt", i, half)])
            K.DMA("sp", xr2[i], xsv[H, tau], [], [("xrow", i)])
            for dh in range(2):
                b = K.bank()
                for kc in range(8):
                    K.MM(K.ps[b][:], zgTt[i][:, kc, :], w_out[:, kc, dh * 512:(dh + 1) * 512], kc == 0, kc == 7,
                         [("zgTt", i, kc // 4), "w_out"], [("ps", b)])
                K.TT("dve", xo[dh], K.ps[b][:], xr2[i][:, dh * 512:(dh + 1) * 512], ALU.add, [("ps", b), ("xrow", i)],
                     [("xo", dh)])
                K.DMA("sp", xmv[H, tau][:, dh * 512:(dh + 1) * 512], xo[dh], [("xo", dh)], ["xmid"])
    K.S.barrier()
    K.release(mT)
    return


SCALE = 128.0 ** -0.5
NEGB = -30000.0


def norm_slots(K, x, xkey, stat, junk):
    for s in range(16):
        K.ACT(junk, x[:, s, :], AF.Square, [xkey(s)], ["junk", "stat"], accum_out=stat[:, s:s + 1])
    K.ACT(stat, stat, AF.Sqrt, ["stat"], ["stat"], scale=1.0 / 1024, bias=K.eps[:, 0:1])
    K.RECIP(stat, stat, ["stat"], ["stat"])


def phase_L2(K, P, x, xkey, ada_dram, pos, qT_o, kT_o, v_o, km_o, after_kv=None):
    S = K.S
    mL2 = K.mark()
    cosT = K.alloc((2048,), F32)
    sinT = K.alloc((2048,), F32)
    Rm = K.alloc((128,), BF16)
    K.TS("dve", Rm[:, 0:64], K.identf[:, 64:128], -1.0, ALU.mult, ["identf"], ["Rm"])
    K.CP("dve", Rm[:, 64:128], K.identf[:, 0:64], ["identf"], ["Rm"])
    mr = K.mark()
    posi = K.alloc((2048,), I32)
    ang = K.alloc((2048,), F32)
    invf = K.alloc((1,), F32)
    K.DMA("sp", posi, pos.partition_broadcast(128), [], ["posi"])
    K.DMA("sp", invf, P["invf"], [], ["invf"])
    K.CP("dve", ang, posi, ["posi"], ["ang"])
    K.TS("dve", ang, ang, invf[:, 0:1], ALU.mult, ["ang", "invf"], ["ang"])
    sincos(K, ang, sinT, cosT, (2048,), "ang", "rope")
    K.S.barrier()
    K.release(mr)
    g1, kg1, mbase, mk = load_mod(K, ada_dram, 0, P["norm1_g"], want_gate=False)
    gs1, sh1, kgs, ksh = load_mod2(K, ada_dram, mbase, mk, P["norm1_g"])
    hT = K.alloc((8, 2048), BF16)
    wqb = [K.alloc((8, 1024), BF16) for _ in range(2)]
    wqv = P["w_qkv"].rearrange("(kc p) n -> p kc n", p=128)
    K.DMA("pool", wqb[1], wqv[:, :, 1024:2048], [], [("wq", 1)])
    K.DMA("pool", wqb[0], wqv[:, :, 2048:3072], [], [("wq", 0)])
    stat = K.alloc((16,), F32)
    tmpf = K.alloc((1024,), F32)
    hrow = [K.alloc((1024,), BF16) for _ in range(2)]
    for s in range(16):
        i = s % 2
        K.ACT(tmpf, x[:, s, :], AF.Square, [xkey(s)], ["junk", "st"], accum_out=stat[:, 0:1])
        K.ACT(stat[:, 1:2], stat[:, 0:1], AF.Sqrt, ["st"], ["st2"], scale=1.0 / 1024, bias=K.eps[:, 0:1])
        K.RECIP(stat[:, 1:2], stat[:, 1:2], ["st2"], ["st2"])
        K.STT(tmpf, x[:, s, :], stat[:, 1:2], gs1, ALU.mult, ALU.mult, [xkey(s), "st2", kgs], ["junk"])
        K.TT("pool", hrow[i], tmpf, sh1, ALU.add, ["junk", ksh], [("hrow", i)])
        b = K.bank()
        psb = K.ps[b].bitcast(BF16)
        for kc in range(8):
            K.TR(psb[:, kc * 128:(kc + 1) * 128], hrow[i][:, kc * 128:(kc + 1) * 128], K.identb,
                 [("hrow", i), "identb"], [("ps", b)])
        K.CP("act", hT[:, :, s * 128:(s + 1) * 128], psb[:, 0:1024].rearrange("p (a n) -> p a n", a=8),
             [("ps", b)], [("hT", s)])
    tf = [K.alloc((512,), F32) for _ in range(2)]
    tb = [K.alloc((512,), BF16) for _ in range(2)]
    ta = [K.alloc((512,), F32) for _ in range(2)]
    tq = [K.alloc((512,), F32) for _ in range(2)]
    tk = [K.alloc((512,), BF16) for _ in range(2)]
    km = K.alloc((8, 8), F32)
    it = [0]

    def qk_pass(which):
        for h in range(8):
            for tt in range(4):
                i = it[0] % 2
                it[0] += 1
                b = K.bank()
                tsl = slice(tt * 512, (tt + 1) * 512)
                c0 = h * 128
                for kc in range(8):
                    K.MM(K.ps[b][:], wqb[which][:, kc, c0:c0 + 128], hT[:, kc, tsl], kc == 0, kc == 7,
                         [("wq", which)] + [("hT", s) for s in range(tt * 4, tt * 4 + 4)], [("ps", b)])
                K.CP("act", tf[i], K.ps[b][:], [("ps", b)], [("tf", i)])
                K.CP("dve", tb[i], tf[i], [("tf", i)], [("tb", i)])
                b2 = K.bank()
                K.MM(K.ps[b2][:], Rm, tb[i], True, True, ["Rm", ("tb", i)], [("ps", b2)])
                K.TT("dve", ta[i], K.ps[b2][:], sinT[:, tsl], ALU.mult, [("ps", b2), "rope"], [("ta", i)])
                K.TT("dve", tf[i], tf[i], cosT[:, tsl], ALU.mult, [("tf", i), "rope"], [("tf", i)])
                if which == 0:
                    K.TT("dve", tq[i], tf[i], ta[i], ALU.add, [("tf", i), ("ta", i)], [("tq", i)])
                    K.ACT(tq[i], tq[i], AF.Copy, [("tq", i)], [("tq", i)], scale=SCALE)
                    K.DMA("sp", qT_o[h][:, tsl], tq[i], [("tq", i)], ["qTo"])
                else:
                    K.TT("dve", tq[i], tf[i], ta[i], ALU.add, [("tf", i), ("ta", i)], [("tq", i)])
                    K.CP("act", tk[i], tq[i], [("tq", i)], [("tk", i)])
                    K.DMA("sp", kT_o[h][:, tsl], tk[i], [("tk", i)], [("kTo", h)])
                    K.S.op("dve", lambda e, o=km[:, h, tt * 2:tt * 2 + 2], a=tq[i].rearrange("p (b n) -> p b n", b=2):
                           e.tensor_reduce(out=o, in_=a, axis=AX.X, op=ALU.add), [("tq", i)], ["km"])

    qk_pass(1)
    K.TS("dve", km, km, 1.0 / 256, ALU.mult, ["km"], ["km"])
    K.DMA("sp", km_o.rearrange("(h d) n -> d h n", h=8), km, ["km"], ["kmo"])
    vb = [K.alloc((512,), BF16) for _ in range(2)]
    for s in range(16):
        for dh in range(2):
            i = (s * 2 + dh) % 2
            b = K.bank()
            for kc in range(8):
                K.MM(K.ps[b][:], hT[:, kc, s * 128:(s + 1) * 128], wqb[0][:, kc, dh * 512:(dh + 1) * 512],
                     kc == 0, kc == 7, [("hT", s), ("wq", 0)], [("ps", b)])
            K.CP("act" if dh else "dve", vb[i], K.ps[b][:], [("ps", b)], [("vb", i)])
            for h4 in range(4):
                hh = dh * 4 + h4
                K.DMA("sp", v_o[hh][s * 128:(s + 1) * 128, :], vb[i][:, h4 * 128:(h4 + 1) * 128], [("vb", i)], [("vo", hh)])
    if after_kv is not None:
        after_kv()
    K.DMA("pool", wqb[0], wqv[:, :, 0:1024], [], [("wq", 0)])
    qk_pass(0)
    K.S.barrier()
    K.release(mL2)


def phase_L3(K, P, x, xkey, ada_dram, qT_l, kT_l, v_l, kT_g, v_g, km_g, out, nexp=32):
    S = K.S
    dbg = False
    mO = K.mark()
    oT = K.alloc((8, 2048), BF16)
    mA = K.mark()
    onesb = K.alloc((128,), BF16)
    K.MEMSET("pool", onesb, 1.0, ["onesb"])
    cmask = K.alloc((4, 512), BF16)
    K.MEMSET("pool", cmask, 0.0, ["cmask"])
    for i in range(4):
        bk = i // 2
        cm = cmask[:, i, bk * 256:(bk + 1) * 256]
        S.op("pool", lambda e, cm=cm, i=i: e.affine_select(out=cm, in_=cm, pattern=[[1, 256]], compare_op=ALU.is_ge,
                                                           fill=NEGB, base=-128 * (i % 2), channel_multiplier=-1),
             ["cmask"], ["cmask"])
    NT = 64
    Ind = K.alloc((40, 128), BF16)
    K.MEMSET("pool", Ind, 0.0, ["Ind"])
    K.DMA("sp", Ind[0:32], P["Ind"], ["Ind"], ["Ind"])
    kmT = K.alloc((8, 32), F32)
    for r in range(4):
        K.DMA("sp", kmT[:, :, r * 8:(r + 1) * 8], km_g[r * 1024:(r + 1) * 1024, :].rearrange("(h d) n -> d h n", h=8),
              ["km_g"], ["kmT"])
    bm_lt = K.alloc((16, 32), F32)
    bm_eq = K.alloc((16, 32), F32)
    bm_pen = K.alloc((16, 32), F32)
    bm_no = K.alloc((16, 32), F32)
    K.DMA("sp", bm_lt, P["bm_lt"], [], ["bm"])
    K.DMA("sp", bm_eq, P["bm_eq"], [], ["bm"])
    K.DMA("sp", bm_pen, P["bm_pen"], [], ["bm"])
    K.DMA("sp", bm_no, P["bm_no"], [], ["bm"])
    kTh = K.alloc((NT * 128,), BF16)
    vh = K.alloc((NT, 128), BF16)
    qf = K.alloc((2048,), F32)
    qb = K.alloc((2048,), BF16)
    BiasT2 = [K.alloc((2048,), BF16) for _ in range(2)]
    BiasG2 = [K.alloc((2048,), BF16) for _ in range(2)]
    for hb_ in range(2):
        K.MEMSET("pool", BiasT2[hb_], 0.0, [("Bias", hb_)])
        K.MEMSET("pool", BiasG2[hb_], 0.0, [("Bias", hb_)])
    NPT = 7
    pT = [K.alloc((512,), BF16) for _ in range(NPT)]
    rec = K.alloc((512,), F32)
    LaccD = K.alloc((512,), F32)
    LaccP = K.alloc((512,), F32)
    onesf = K.alloc((128,), F32)
    K.MEMSET("pool", onesf, 1.0, ["onesf"])
    gtmp = [K.alloc((48,), F32) for _ in range(2)]
    gal = K.alloc((16, 64), F32)
    SB = [0, 1, 2, 3]
    BL, BT, BG = 6, 6, 7
    nsb = 0
    npt = 0

    def gating_front(h):
        K.DMA("sp", qf, qT_l[h], ["qT_l"], ["qf"])
        for s in range(16):
            K.MM(K.ps[BG][:, s * 32:(s + 1) * 32], qf[:, s * 128:(s + 1) * 128], kmT[:, h, :], True, True,
                 ["qf", "kmT"], [("ps", BG)])
        for s in range(16):
            i = s % 2
            g = gtmp[i]
            gk = ("gtmp", i)
            gm, m8 = g[:, 0:32], g[:, 32:40]
            al, al2 = gal[:, s, 0:32], gal[:, s, 32:64]
            ak = ("gal", s)
            K.TT("dve", gm, K.ps[BG][:, s * 32:(s + 1) * 32], bm_lt[:, s, :], ALU.mult, [("ps", BG), "bm"], [gk])
            K.TT("dve", gm, gm, bm_pen[:, s, :], ALU.add, [gk, "bm"], [gk])
            S.op("dve", lambda e, m8=m8, gm=gm: e.max(out=m8, in_=gm), [gk], [gk])
            K.TS("dve", al, gm, m8[:, 2:3], ALU.is_ge, [gk], [ak])
            K.TT("dve", al, al, bm_lt[:, s, :], ALU.mult, [ak, "bm"], [ak])
            K.TT("dve", al, al, bm_eq[:, s, :], ALU.add, [ak, "bm"], [ak])
            K.TT("dve", al2, al, bm_no[:, s, :], ALU.mult, [ak, "bm"], [ak])
            K.TS("dve", al, al, -1.0, ALU.add, [ak], [ak], s2=-NEGB, op1=ALU.mult)
            K.TS("dve", al2, al2, -1.0, ALU.add, [ak], [ak], s2=-NEGB, op1=ALU.mult)

    def gating_back(h):
        hb = h % 2
        for which, dst in ((0, BiasT2[hb]), (1, BiasG2[hb])):
            for s4 in range(4):
                for j in range(4):
                    s = s4 * 4 + j
                    K.TR(K.ps[BT][0:32, j * 128:(j + 1) * 128], gal[:, s, which * 32:(which + 1) * 32], K.identf,
                         [("gal", s), "identf"], [("ps", BT)])
                K.CP("act", dst[0:32, s4 * 512:(s4 + 1) * 512], K.ps[BT][0:32, :], [("ps", BT)], [("Bias", hb)])

    gating_front(0)
    gating_back(0)
    for h in range(8):
        hb = h % 2
        BiasT, BiasG = BiasT2[hb], BiasG2[hb]
        K.DMA("sp", kTh[:, 0:2048], kT_l[h], [("kTo", h)], ["kTh"])
        K.DMA("sp", vh[:, 0:16, :], v_l[h].rearrange("(kt k) d -> k kt d", k=128), [("vo", h)], ["vh"])
        for r in range(3):
            K.DMA("sp", kTh[:, (r + 1) * 2048:(r + 2) * 2048], kT_g[h][r * 128:(r + 1) * 128, :], [("kT_g", h)], ["kTh"])
            K.DMA("sp", vh[:, (r + 1) * 16:(r + 2) * 16, :],
                  v_g[h].rearrange("(kt k) d -> k kt d", k=128)[:, r * 16:(r + 1) * 16, :], [("v_g", h)], ["vh"])
        K.CP("pool", qb, qf, ["qf"], ["qb"])
        for qt in range(4):
            qsl = slice(qt * 512, (qt + 1) * 512)
            bo = 4 + (qt % 2)
            bl = BL

            def s_part(kt):
                nonlocal nsb, npt
                bs = SB[nsb % 4]
                nsb += 1
                diag = (kt // 4 == qt) and kt < 16
                K.MM(K.ps[bs][:], kTh[:, kt * 128:(kt + 1) * 128], qb[:, qsl], True, False, ["kTh", "qb"],
                     [("ps", bs)])
                bias_t = BiasT if kt < 16 else BiasG
                K.MM(K.ps[bs][:], Ind[:, kt // 2, :], bias_t[:, qsl], False, not diag, ["Ind", ("Bias", hb)],
                     [("ps", bs)])
                if diag:
                    K.MM(K.ps[bs][:], K.identb, cmask[:, kt % 4, :], False, True, ["identb", "cmask"], [("ps", bs)])
                p = pT[npt % NPT]
                pk = ("pT", npt % NPT)
                npt += 1
                K.ACT(p, K.ps[bs][:], AF.Exp, [("ps", bs)], [pk])
                return p, pk

            def pv_part(kt, p, pk):
                K.MM(K.ps[bo][:], vh[:, kt, :], p, kt == 0, kt == NT - 1, ["vh", pk], [("ps", bo)])
                K.MM(K.ps[bl][:], onesb, p, kt == 0, kt == NT - 1, ["onesb", pk], [("ps", bl)])

            LOOK = 3
            pend = []
            for kt in range(NT + LOOK):
                if kt < NT:
                    pend.append(s_part(kt))
                if kt >= LOOK:
                    pp, ppk = pend.pop(0)
                    pv_part(kt - LOOK, pp, ppk)
            K.RECIP(rec, K.ps[bl][:], [("ps", bl)], ["rec"])
            K.TT("dve", oT[:, h, qsl], K.ps[bo][:], rec, ALU.mult, [("ps", bo), "rec"], [("oT", h)])
            if h + 1 < 8:
                if qt == 0:
                    gating_front(h + 1)
                if qt == 2:
                    gating_back(h + 1)
    K.S.barrier()
    K.release(mA)
    wo = K.alloc((8, 1024), BF16)
    g1, kg1, mbase, mk = load_mod(K, ada_dram, 0, None)
    wst = K.alloc((8, 1024), F32)
    K.DMA("sp", wst, P["w_o"].rearrange("(h p) n -> p h n", p=128), [], ["wst"])
    for hh in range(8):
        K.TT("dve" if hh % 2 else "pool", wo[:, hh, :], wst[:, hh, :], g1, ALU.mult, ["wst", kg1], ["wo"])
    for s_ in range(16):
        for dh in range(2):
            b = K.bank()
            for hh in range(8):
                K.MM(K.ps[b][:], oT[:, hh, s_ * 128:(s_ + 1) * 128], wo[:, hh, dh * 512:(dh + 1) * 512], hh == 0, hh == 7,
                     [("oT", hh), "wo"], [("ps", b)])
            xs = x[:, s_, dh * 512:(dh + 1) * 512]
            K.TT("dve", xs, K.ps[b][:], xs, ALU.add, [("ps", b), xkey(s_)], [xkey(s_)])
    if dbg:
        for s_ in range(16):
            K.DMA("sp", xatt[s_ * 128:(s_ + 1) * 128, :], x[:, s_, :], [xkey(s_)], ["xatt"])
    K.S.barrier()
    K.release(mO)
    moe_phase(K, x, xkey, ada_dram, P, nexp=nexp)
    mF = K.mark()
    fg = K.alloc((1024,), F32)
    stat = K.alloc((16,), F32)
    junk = K.alloc((1024,), F32)
    ot = [K.alloc((1024,), F32) for _ in range(2)]
    K.DMA("sp", fg, P["final_g"].partition_broadcast(128), [], ["fg"])
    norm_slots(K, x, xkey, stat, junk)
    for s_ in range(16):
        i = s_ % 2
        K.STT(ot[i], x[:, s_, :], stat[:, s_:s_ + 1], fg, ALU.mult, ALU.mult, [xkey(s_), "stat", "fg"], [("ot", i)])
        K.DMA("sp", out[s_ * 128:(s_ + 1) * 128, :], ot[i], [("ot", i)], ["out"])


L0_SHAPES = {
    "xs": (2048, 1024), "selm": (128, 12), "crep": (128, 8, 128),
    "ada_w": (1024, 6144), "ada_b": (6144,), "norm1_g": (1024,), "norm2_g": (1024,),
    "w_in": (1024, 1024), "w_glu": (1024, 1024), "w_out": (1024, 1024),
    "lamP_re": (128, 64), "lamP_im": (128, 64), "logdtP": (128, 64),
    "bP_re": (128, 64, 16), "bP_im": (128, 64, 16), "cP_re": (128, 64, 16), "cP_im": (128, 64, 16),
    "dS": (128, 64),
    "w_router": (1024, 32), "b_router": (32,), "w_gate_up": (32, 1024, 2048), "bguT": (128, 32, 16),
    "w_down": (32, 1024, 1024), "b_down": (32, 1024),
}
L1_SHAPES = {
    "ada_w": (1024, 6144), "ada_b": (6144,), "norm1_g": (1024,), "norm2_g": (1024,), "final_g": (1024,),
    "w_qkv": (1024, 3072), "w_o": (1024, 1024), "invf": (128, 1),
    "bm_lt": (128, 16, 32), "bm_eq": (128, 16, 32), "bm_pen": (128, 16, 32), "bm_no": (128, 16, 32),
    "w_router": (1024, 32), "b_router": (32,), "w_gate_up": (32, 1024, 2048), "bguT": (128, 32, 16),
    "w_down": (32, 1024, 1024), "b_down": (32, 1024),
}
GROUPS = [[0, 1, 2, 3], [4, 5, 6, 7]]


def build_fused(nexp=32):
    K = KB()
    P0 = {k: K.din("a_" + k, v) for k, v in L0_SHAPES.items()}
    P1 = {k: K.din("b_" + k, v) for k, v in L1_SHAPES.items()}
    P1["crep"] = P0["crep"]
    P1["Ind"] = K.din("b_Ind", (32, 40, 128), BF16)
    pos = K.din("b_pos", (2048,), I32)
    out = K.dout("out", (2048, 1024))
    xmid = K.dtmp("xmid", (2048, 1024))
    ada0 = K.dtmp("ada0", (128, 6144))
    ada1 = K.dtmp("ada1", (128, 6144))
    qT_l = K.dtmp("qT_l", (8, 128, 2048))
    kT_l = K.dtmp("kT_l", (8, 128, 2048), BF16)
    v_l = K.dtmp("v_l", (8, 2048, 128), BF16)
    km_l = K.dtmp("km_l", (1024, 8))
    kT_g = K.dtmp("kT_g", (8, 512, 2048), BF16)
    v_g = K.dtmp("v_g", (8, 8192, 128), BF16)
    km_g = K.dtmp("km_g", (4096, 8))
    K.consts()
    st_l = K.dtmp("st_l", (128, 64))
    st_g = K.dtmp("st_g", (512, 64))
    phase_S5(K, P0, xmid, ada0, st_l, st_g, GROUPS)
    x = K.alloc((16, 1024), F32)
    xkey = lambda s: ("x", s)
    for s in range(16):
        K.DMA("sp", x[:, s, :], xmid[s * 128:(s + 1) * 128, :], ["xmid"], [xkey(s)])
    moe_phase(K, x, xkey, ada0, P0, nexp=nexp)
    ada_phase(K, P1["crep"], P1["ada_w"], P1["ada_b"], ada1)
    def gather_kv():
        cl = [(km_l, km_g, "kmo", "km_g")]
        for h in range(8):
            cl.append((kT_l[h], kT_g[h], ("kTo", h), ("kT_g", h)))
            cl.append((v_l[h], v_g[h], ("vo", h), ("v_g", h)))
        for (src, dst, kr, kw) in cl:
            K.S.op("pool", lambda e, src=src, dst=dst: e.collective_compute("AllGather", ALU.bypass, replica_groups=GROUPS,
                                                                             ins=[src], outs=[dst]), [kr], [kw], coll=True)

    phase_L2(K, P1, x, xkey, ada1, pos, qT_l, kT_l, v_l, km_l, after_kv=gather_kv)
    phase_L3(K, P1, x, xkey, ada1, qT_l, kT_l, v_l, kT_g, v_g, km_g, out, nexp=nexp)
    return K.finish()


def prep_common(inp, li, b):
    pre = "l%d_" % li
    c = np.asarray(inp["c"][b], np.float32)
    crep = np.ascontiguousarray(np.broadcast_to(c.reshape(8, 128).T[:, :, None], (128, 8, 128)))
    d = {
        "crep": crep,
        "ada_w": inp[pre + "ada_w"], "ada_b": inp[pre + "ada_b"],
        "norm1_g": inp[pre + "norm1_g"], "norm2_g": inp[pre + "norm2_g"],
        "w_router": inp[pre + "moe_w_router"], "b_router": inp[pre + "moe_b_router"],
        "w_gate_up": inp[pre + "moe_w_gate_up"],
        "bguT": np.ascontiguousarray(np.asarray(inp[pre + "moe_b_gate_up"]).reshape(32, 16, 128).transpose(2, 0, 1)),
        "w_down": inp[pre + "moe_w_down"], "b_down": inp[pre + "moe_b_down"],
    }
    return d


def prep_L1(inp, core):
    b, j = divmod(core, 4)
    x = np.asarray(inp["x"][b])
    selm = np.zeros((128, 12), np.float32)
    for r in range(4):
        mm = j - 1 - r
        if 0 <= mm <= 2:
            selm[:, r * 3 + mm] = 1.0
    d = prep_common(inp, 0, b)
    d["xs"] = x[j * 2048:(j + 1) * 2048]
    d["selm"] = selm
    g = lambda n: np.asarray(inp["l0_s5_" + n], np.float32)
    t2 = lambda a: np.ascontiguousarray(np.concatenate([a, a], axis=0))
    d["w_in"], d["w_glu"], d["w_out"] = g("w_in"), g("w_glu"), g("w_out")
    d["lamP_re"] = t2(g("lam_re").T)
    d["lamP_im"] = t2(g("lam_im").T)
    d["logdtP"] = np.ascontiguousarray(np.broadcast_to(g("log_dt")[None, :], (128, 64)))
    d["bP_re"] = t2(g("b_re").transpose(1, 0, 2))
    d["bP_im"] = t2(g("b_im").transpose(1, 0, 2))
    d["cP_re"] = t2(g("c_re").transpose(2, 0, 1))
    d["cP_im"] = t2(g("c_im").transpose(2, 0, 1))
    d["dS"] = np.ascontiguousarray(np.tile(g("d").reshape(64, 16).T, (8, 1)))
    return {k: np.ascontiguousarray(np.asarray(v, np.float32)) for k, v in d.items()}


def f32c(a):
    return np.ascontiguousarray(np.asarray(a, np.float32))


def prep_L2(inp, core, x0):
    b, j = divmod(core, 4)
    d = prep_common(inp, 1, b)
    inv = (np.float32(10000.0) ** (-np.arange(0, 128, 2, dtype=np.float32) / np.float32(128))).astype(np.float32)
    m = {"x0": f32c(x0), "crep": d["crep"], "ada_w": f32c(d["ada_w"]), "ada_b": f32c(d["ada_b"]),
         "norm1_g": f32c(d["norm1_g"]), "w_qkv": f32c(inp["l1_moba_w_qkv"]),
         "invf": f32c(np.concatenate([inv, inv])[:, None]),
         "pos": np.ascontiguousarray(np.asarray(inp["positions"][b, j * 2048:(j + 1) * 2048], np.int32))}
    return m


def seg_order(j):
    return [j] + [k for k in range(4) if k != j]


def prep_L3(inp, core, x0, l2res):
    import ml_dtypes
    b, j = divmod(core, 4)
    d = prep_common(inp, 1, b)
    order = seg_order(j)
    m = {k: f32c(d[k]) for k in ("crep", "ada_w", "ada_b", "norm2_g", "w_router", "b_router", "w_gate_up", "bguT",
                                 "w_down", "b_down")}
    m["x0"] = f32c(x0)
    m["final_g"] = f32c(inp["final_norm_g"])
    m["w_o"] = f32c(inp["l1_moba_w_o"])
    m["qT"] = l2res[core]["qT"]
    m["kT_all"] = np.ascontiguousarray(np.concatenate([l2res[b * 4 + k]["kT"] for k in order], axis=2))
    m["v_all"] = np.ascontiguousarray(np.concatenate([l2res[b * 4 + k]["v"] for k in order], axis=0))
    km = np.concatenate([l2res[b * 4 + k]["kmT"] for k in range(4)], axis=2)
    m["kmT"] = f32c(km.transpose(1, 0, 2))
    gblk = np.array([order[bp // 8] * 8 + bp % 8 for bp in range(32)])
    ind = (np.arange(32)[:, None] == gblk[None, :]).astype(np.float32)
    m["Ind"] = np.ascontiguousarray(np.broadcast_to(ind[:, :, None], (32, 32, 128))).astype(ml_dtypes.bfloat16)
    jq = 8 * j + np.arange(16) // 2
    n = np.arange(32)
    lt = (n[None, :] < jq[:, None]).astype(np.float32)
    eq = (n[None, :] == jq[:, None]).astype(np.float32)
    bc = lambda a: np.ascontiguousarray(np.broadcast_to(a[None], (128, 16, 32))).astype(np.float32)
    m["bm_lt"], m["bm_eq"], m["bm_pen"] = bc(lt), bc(eq), bc((lt - 1.0) * 1e30)
    return m


def prep_fused(inp, core):
    import ml_dtypes
    b, j = divmod(core, 4)
    m0 = prep_L1(inp, core)
    m = {"a_" + k: v for k, v in m0.items()}
    d = prep_common(inp, 1, b)
    inv = (np.float32(10000.0) ** (-np.arange(0, 128, 2, dtype=np.float32) / np.float32(128))).astype(np.float32)
    m1 = {k: f32c(d[k]) for k in ("ada_w", "ada_b", "norm1_g", "norm2_g", "w_router", "b_router", "w_gate_up", "bguT",
                                  "w_down", "b_down")}
    m1["final_g"] = f32c(inp["final_norm_g"])
    m1["w_qkv"] = f32c(inp["l1_moba_w_qkv"])
    m1["w_o"] = f32c(inp["l1_moba_w_o"])
    m1["invf"] = f32c(np.concatenate([inv, inv])[:, None])
    jq = 8 * j + np.arange(16) // 2
    n = np.arange(32)
    lt = (n[None, :] < jq[:, None]).astype(np.float32)
    eq = (n[None, :] == jq[:, None]).astype(np.float32)
    notown = np.broadcast_to(((n // 8) != j).astype(np.float32)[None, :], (16, 32))
    bc = lambda a: np.ascontiguousarray(np.broadcast_to(a[None], (128, 16, 32))).astype(np.float32)
    m1["bm_lt"], m1["bm_eq"], m1["bm_pen"], m1["bm_no"] = bc(lt), bc(eq), bc((lt - 1.0) * 1e30), bc(notown)
    for k, v in m1.items():
        m["b_" + k] = v
    gb = np.concatenate([8 * j + np.arange(8), np.arange(32)])
    ind = (np.arange(32)[:, None] == gb[None, :]).astype(np.float32)
    m["b_Ind"] = np.ascontiguousarray(np.broadcast_to(ind[:, :, None], (32, 40, 128))).astype(ml_dtypes.bfloat16)
    m["b_pos"] = np.ascontiguousarray(np.asarray(inp["positions"][b, j * 2048:(j + 1) * 2048], np.int32))
    return m


_INPUT_NAMES = (
    "x", "c", "positions", "l0_norm1_g",
    "l0_ada_w", "l0_ada_b", "l0_s5_w_in", "l0_s5_b_re",
    "l0_s5_b_im", "l0_s5_c_re", "l0_s5_c_im", "l0_s5_lam_re",
    "l0_s5_lam_im", "l0_s5_log_dt", "l0_s5_d", "l0_s5_w_glu",
    "l0_s5_w_out", "l0_norm2_g", "l0_moe_w_router", "l0_moe_b_router",
    "l0_moe_w_gate_up", "l0_moe_b_gate_up", "l0_moe_w_down", "l0_moe_b_down",
    "l1_norm1_g", "l1_ada_w", "l1_ada_b", "l1_moba_w_qkv",
    "l1_moba_w_o", "l1_norm2_g", "l1_moe_w_router", "l1_moe_b_router",
    "l1_moe_w_gate_up", "l1_moe_b_gate_up", "l1_moe_w_down", "l1_moe_b_down",
    "final_norm_g",
)


def kernel(**inputs):
    inp = {k: np.asarray(inputs[k]) for k in _INPUT_NAMES}
    cores = list(range(8))
    nc = build_fused()
    res = run_bass_kernel_spmd(nc, [prep_fused(inp, c) for c in cores], core_ids=cores).results
    out = np.zeros((2, 8192, 1024), np.float32)
    for c in cores:
        b, j = divmod(c, 4)
        out[b, j * 2048:(j + 1) * 2048] = np.asarray(res[c]["out"])
    return out
```
